# Optimizing a Trainium2 kernel written in Bass

```python
import math
import jax, jax.numpy as jnp
from jax import lax
import numpy as np

D_MODEL = 1024
BATCH = 8
SEQ = 4096
DEPTH = 2

GRID_W = 64
CTX_LEN = 256
EPS = 1e-6
ROPE_THETA = 10000.0
Q_BLOCK = 128
N_MOD = 6

MLA_HEADS = 8
MLA_NOPE = 64
MLA_ROPE = 32
MLA_V = 64
MLA_Q_RANK = 384
MLA_KV_RANK = 256
MLA_SCALE = (MLA_NOPE + MLA_ROPE) ** -0.5

DIFF_HEADS = 4
DIFF_HD = 64
DIFF_SCALE = DIFF_HD ** -0.5

AB_WIDTHS = (MLA_Q_RANK, MLA_KV_RANK, MLA_ROPE, 2 * DIFF_HEADS * DIFF_HD, 2 * DIFF_HEADS * DIFF_HD, DIFF_HEADS * 2 * DIFF_HD)
AB_IN = sum(AB_WIDTHS)
AB_SPLITS = [int(v) for v in np.cumsum(AB_WIDTHS)[:-1]]
AB_OUT = MLA_HEADS * MLA_V + DIFF_HEADS * 2 * DIFF_HD

NA_HEADS = 16
NA_HD = D_MODEL // NA_HEADS
NA_KH = 8
NA_KW = 16
NA_SCALE = NA_HD ** -0.5

D_FF = ((8 * D_MODEL // 3 + 127) // 128) * 128
N_EXPERTS = 8
TOP_K = 2

N_EVEN = (DEPTH + 1) // 2
N_ODD = DEPTH // 2

kernel_name = "hybrid_mla_diff_natten_moe_dit"


def rmsnorm(x, g):
    xf = x.astype(jnp.float32)
    y = xf * lax.rsqrt(jnp.mean(xf * xf, axis=-1, keepdims=True) + EPS)
    return (y * g.astype(jnp.float32)).astype(x.dtype)


def modulate(h, shift, scale):
    return h * (1 + scale) + shift


def softmax32(s):
    return jax.nn.softmax(s.astype(jnp.float32), axis=-1)


def axis_rope_tables(pos, dim):
    inv = ROPE_THETA ** (-jnp.arange(0, dim, 2, dtype=jnp.float32) / dim)
    ang = pos.astype(jnp.float32)[:, None] * inv[None, :]
    ang = jnp.concatenate([ang, ang], axis=-1)[:, None, :]
    return jnp.cos(ang), jnp.sin(ang)


def rotate_half(x):
    x1, x2 = jnp.split(x, 2, axis=-1)
    return jnp.concatenate([-x2, x1], axis=-1)


def axial_rope(x, row, col):
    d = x.shape[-1]
    xf = x.astype(jnp.float32)
    xr, xc = xf[..., : d // 2], xf[..., d // 2:]
    cr, sr = axis_rope_tables(row, d // 2)
    cc, scol = axis_rope_tables(col, d // 2)
    out = jnp.concatenate([xr * cr + rotate_half(xr) * sr, xc * cc + rotate_half(xc) * scol], axis=-1)
    return out.astype(x.dtype)


def sweep_query_blocks(fn, *qs):
    B, L = qs[0].shape[:2]
    nb = L // Q_BLOCK
    blocks = tuple(jnp.moveaxis(q.reshape((B, nb, Q_BLOCK) + q.shape[2:]), 1, 0) for q in qs)
    out = lax.map(lambda qb: fn(*qb), blocks)
    out = jnp.moveaxis(out, 0, 1)
    return out.reshape((B, L) + out.shape[3:])


def mla_attend(q_nope, q_rope, k_nope, k_rope, v):
    s = jnp.einsum('bqhd,bkhd->bhqk', q_nope, k_nope) + jnp.einsum('bqhd,bkd->bhqk', q_rope, k_rope)
    p = softmax32(s * MLA_SCALE).astype(v.dtype)
    return jnp.einsum('bhqk,bkhd->bqhd', p, v)


def diff_attend(q1, q2, k1, k2, v, lam, subln_g, lam_init):
    p1 = softmax32(jnp.einsum('bqhd,bkhd->bhqk', q1, k1) * DIFF_SCALE)
    p2 = softmax32(jnp.einsum('bqhd,bkhd->bhqk', q2, k2) * DIFF_SCALE)
    p = (p1 - lam * p2).astype(v.dtype)
    o = jnp.einsum('bhqk,bkhd->bqhd', p, v)
    return rmsnorm(o, subln_g) * (1 - lam_init)


def ab_project(h, w_in, q_norm, kv_norm, w_uq, w_ukv, row, col):
    B, n, _ = h.shape
    c_q, c_kv, k_rope, dq, dk, dv = jnp.split(h @ w_in, AB_SPLITS, axis=-1)
    q = (rmsnorm(c_q, q_norm) @ w_uq).reshape(B, n, MLA_HEADS, MLA_NOPE + MLA_ROPE)
    kv = (rmsnorm(c_kv, kv_norm) @ w_ukv).reshape(B, n, MLA_HEADS, MLA_NOPE + MLA_V)
    q_nope, q_rope = q[..., :MLA_NOPE], q[..., MLA_NOPE:]
    k_nope, v_a = kv[..., :MLA_NOPE], kv[..., MLA_NOPE:]
    k_rope = k_rope[:, :, None, :]
    dq = dq.reshape(B, n, 2 * DIFF_HEADS, DIFF_HD)
    dk = dk.reshape(B, n, 2 * DIFF_HEADS, DIFF_HD)
    if row is not None:
        q_rope, k_rope, dq, dk = [axial_rope(t, row, col) for t in (q_rope, k_rope, dq, dk)]
    dq = dq.reshape(B, n, DIFF_HEADS, 2, DIFF_HD)
    dk = dk.reshape(B, n, DIFF_HEADS, 2, DIFF_HD)
    dv = dv.reshape(B, n, DIFF_HEADS, 2 * DIFF_HD)
    queries = (q_nope, q_rope, dq[..., 0, :], dq[..., 1, :])
    keys = (k_nope, k_rope[:, :, 0, :], dk[..., 0, :], dk[..., 1, :], v_a, dv)
    return queries, keys


def mixer_ab(hx, hc, row, col, w_in, q_norm, kv_norm, w_uq, w_ukv, lam_vecs, subln, w_out, lam_init, ctx_out):
    B, L, _ = hx.shape
    qx, kx = ab_project(hx, w_in, q_norm, kv_norm, w_uq, w_ukv, row, col)
    qc, kc = ab_project(hc, w_in, q_norm, kv_norm, w_uq, w_ukv, None, None)
    lam = (jnp.exp(jnp.sum(lam_vecs[0].astype(jnp.float32) * lam_vecs[1].astype(jnp.float32)))
           - jnp.exp(jnp.sum(lam_vecs[2].astype(jnp.float32) * lam_vecs[3].astype(jnp.float32))) + lam_init)
    keys_all = tuple(jnp.concatenate([a, b], axis=1) for a, b in zip(kc, kx))

    def attend(q, k):
        qn, qr, q1, q2 = q
        kn, kr, k1, k2, va, vb = k
        oa = mla_attend(qn, qr, kn, kr, va)
        ob = diff_attend(q1, q2, k1, k2, vb, lam, subln, lam_init)
        return jnp.concatenate([oa.reshape(oa.shape[:2] + (-1,)), ob.reshape(ob.shape[:2] + (-1,))], axis=-1)

    ox = sweep_query_blocks(lambda *qb: attend(qb, keys_all), *qx)
    yx = ox @ w_out
    yc = attend(qc, kc) @ w_out if ctx_out else None
    return yx, yc


def mixer_na(hx, hc, w_qkv, b_qkv, rpb, w_out, b_out, ctx_out):
    B, L, _ = hx.shape
    rows = L // GRID_W
    kh = min(NA_KH, rows)

    def qkv(h):
        z = h @ w_qkv + b_qkv
        return [t.reshape(B, h.shape[1], NA_HEADS, NA_HD) for t in jnp.split(z, 3, axis=-1)]

    qc, kc, vc = qkv(hc)
    qx, kx, vx = qkv(hx)
    grid = lambda t: t.reshape(B, rows, GRID_W, NA_HEADS, NA_HD)
    kg, vg = grid(kx), grid(vx)
    cols = np.arange(GRID_W)
    col_start = np.clip(cols - NA_KW // 2, 0, GRID_W - NA_KW)
    col_idx = col_start[:, None] + np.arange(NA_KW)[None, :]
    dcol = col_idx - cols[:, None] + (NA_KW - 1)
    rpb_c = rpb[:, :, dcol]

    def row_block(args):
        r, q_r = args
        rs = jnp.clip(r - kh // 2, 0, rows - kh)
        k_rows = lax.dynamic_slice_in_dim(kg, rs, kh, axis=1)
        v_rows = lax.dynamic_slice_in_dim(vg, rs, kh, axis=1)
        k_win = k_rows[:, :, col_idx]
        v_win = v_rows[:, :, col_idx]
        drow = rs + jnp.arange(kh) - r + (NA_KH - 1)
        bias = jnp.take(rpb_c, drow, axis=1).transpose(0, 2, 1, 3)
        s_win = jnp.einsum('bqhd,biqjhd->bhqij', q_r, k_win) * NA_SCALE + bias[None].astype(q_r.dtype)
        s_win = s_win.reshape(B, NA_HEADS, GRID_W, kh * NA_KW)
        s_ctx = jnp.einsum('bqhd,bkhd->bhqk', q_r, kc) * NA_SCALE
        p = softmax32(jnp.concatenate([s_win, s_ctx], axis=-1)).astype(vx.dtype)
        p_win = p[..., : kh * NA_KW].reshape(B, NA_HEADS, GRID_W, kh, NA_KW)
        o = jnp.einsum('bhqij,biqjhd->bqhd', p_win, v_win) + jnp.einsum('bhqk,bkhd->bqhd', p[..., kh * NA_KW:], vc)
        return o

    q_rows = jnp.moveaxis(grid(qx), 1, 0)
    o = lax.map(row_block, (jnp.arange(rows, dtype=jnp.int32), q_rows))
    o = jnp.moveaxis(o, 0, 1).reshape(B, L, D_MODEL)
    yx = o @ w_out + b_out
    if ctx_out:
        pc = softmax32(jnp.einsum('bqhd,bkhd->bhqk', qc, kc) * NA_SCALE).astype(vc.dtype)
        yc = jnp.einsum('bhqk,bkhd->bqhd', pc, vc).reshape(B, hc.shape[1], D_MODEL) @ w_out + b_out
    else:
        yc = None
    return yx, yc


def swiglu(h, w1, w3, w2):
    return (jax.nn.silu(h @ w1) * (h @ w3)) @ w2


def moe_swiglu(h, router, w1, w3, w2):
    logits = (h @ router).astype(jnp.float32)
    top_v, top_i = lax.top_k(logits, TOP_K)
    gates = jax.nn.softmax(top_v, axis=-1)
    combine = jnp.sum(jax.nn.one_hot(top_i, N_EXPERTS, dtype=jnp.float32) * gates[..., None], axis=-2).astype(h.dtype)
    y = jnp.zeros_like(h)
    for e in range(N_EXPERTS):
        y = y + combine[..., e:e + 1] * swiglu(h, w1[e], w3[e], w2[e])
    return y


def setup_inputs(seed: int = 0) -> dict:
    key = jax.random.key(seed)
    ks = iter(jax.random.split(key, 40))
    D = D_MODEL

    def nrm(shape, s):
        return jax.random.normal(next(ks), shape, jnp.float32) * s

    def gain(shape):
        return 1.0 + nrm(shape, 0.02)

    return {
        "x": nrm((BATCH, SEQ, D), 1.0),
        "c": nrm((BATCH, D), 1.0),
        "ctx": nrm((BATCH, CTX_LEN, D), 1.0),
        "c_ctx": nrm((D,), 1.0),
        "w_mod": nrm((DEPTH, D, N_MOD * D), 0.5 * D ** -0.5),
        "b_mod": nrm((DEPTH, N_MOD * D), 0.02),
        "norm_g": gain((DEPTH, 4, D)),
        "a_w_in": nrm((N_EVEN, D, AB_IN), D ** -0.5),
        "a_q_norm": gain((N_EVEN, MLA_Q_RANK)),
        "a_kv_norm": gain((N_EVEN, MLA_KV_RANK)),
        "a_w_uq": nrm((N_EVEN, MLA_Q_RANK, MLA_HEADS * (MLA_NOPE + MLA_ROPE)), MLA_Q_RANK ** -0.5),
        "a_w_ukv": nrm((N_EVEN, MLA_KV_RANK, MLA_HEADS * (MLA_NOPE + MLA_V)), MLA_KV_RANK ** -0.5),
        "b_lambda": nrm((N_EVEN, 4, DIFF_HD), 0.1),
        "b_subln": gain((N_EVEN, 2 * DIFF_HD)),
        "ab_w_out": nrm((N_EVEN, AB_OUT, D), AB_OUT ** -0.5),
        "f_w1": nrm((N_EVEN, D, D_FF), D ** -0.5),
        "f_w3": nrm((N_EVEN, D, D_FF), D ** -0.5),
        "f_w2": nrm((N_EVEN, D_FF, D), D_FF ** -0.5),
        "c_w_qkv": nrm((N_ODD, D, 3 * D), D ** -0.5),
        "c_b_qkv": nrm((N_ODD, 3 * D), 0.02),
        "c_rpb": nrm((N_ODD, NA_HEADS, 2 * NA_KH - 1, 2 * NA_KW - 1), 0.05),
        "c_w_out": nrm((N_ODD, D, D), D ** -0.5),
        "c_b_out": nrm((N_ODD, D), 0.02),
        "m_router": nrm((N_ODD, D, N_EXPERTS), D ** -0.5),
        "m_w1": nrm((N_ODD, N_EXPERTS, D, D_FF), D ** -0.5),
        "m_w3": nrm((N_ODD, N_EXPERTS, D, D_FF), D ** -0.5),
        "m_w2": nrm((N_ODD, N_EXPERTS, D_FF, D), D_FF ** -0.5),
    }


def reference(x, c, ctx, c_ctx, w_mod, b_mod, norm_g, a_w_in, a_q_norm, a_kv_norm, a_w_uq, a_w_ukv,
              b_lambda, b_subln, ab_w_out, f_w1, f_w3, f_w2, c_w_qkv, c_b_qkv, c_rpb, c_w_out, c_b_out,
              m_router, m_w1, m_w3, m_w2):
    B, L, D = x.shape
    t = jnp.arange(L, dtype=jnp.int32)
    row, col = t // GRID_W, t % GRID_W
    sc = jax.nn.silu(c)
    scc = jax.nn.silu(c_ctx)
    cs = ctx
    for i in range(DEPTH):
        last = i == DEPTH - 1
        j = i // 2
        mod_x = (sc @ w_mod[i] + b_mod[i]).reshape(B, N_MOD, 1, D)
        mod_c = (scc @ w_mod[i] + b_mod[i]).reshape(1, N_MOD, 1, D)
        g = norm_g[i]
        hx = modulate(rmsnorm(x, g[0]), mod_x[:, 0], mod_x[:, 1])
        hc = modulate(rmsnorm(cs, g[0]), mod_c[:, 0], mod_c[:, 1])
        if i % 2 == 0:
            lam_init = 0.8 - 0.6 * math.exp(-0.3 * i)
            yx, yc = mixer_ab(hx, hc, row, col, a_w_in[j], a_q_norm[j], a_kv_norm[j], a_w_uq[j], a_w_ukv[j],
                              b_lambda[j], b_subln[j], ab_w_out[j], lam_init, not last)
        else:
            yx, yc = mixer_na(hx, hc, c_w_qkv[j], c_b_qkv[j], c_rpb[j], c_w_out[j], c_b_out[j], not last)
        x = x + mod_x[:, 2] * rmsnorm(yx, g[1])
        if not last:
            cs = cs + mod_c[:, 2] * rmsnorm(yc, g[1])
        hx = modulate(rmsnorm(x, g[2]), mod_x[:, 3], mod_x[:, 4])
        if i % 2 == 0:
            fx = swiglu(hx, f_w1[j], f_w3[j], f_w2[j])
        else:
            fx = moe_swiglu(hx, m_router[j], m_w1[j], m_w3[j], m_w2[j])
        x = x + mod_x[:, 5] * rmsnorm(fx, g[3])
        if not last:
            hc = modulate(rmsnorm(cs, g[2]), mod_c[:, 3], mod_c[:, 4])
            if i % 2 == 0:
                fc = swiglu(hc, f_w1[j], f_w3[j], f_w2[j])
            else:
                fc = moe_swiglu(hc, m_router[j], m_w1[j], m_w3[j], m_w2[j])
            cs = cs + mod_c[:, 5] * rmsnorm(fc, g[3])
    return x
```

```python
import math
import types
from contextlib import ExitStack
import numpy as np
import concourse.bass as bass
import concourse.mybir as mybir
from concourse.bass_utils import run_bass_kernel_spmd

F32 = mybir.dt.float32
BF16 = mybir.dt.bfloat16
ALU = mybir.AluOpType
AF = mybir.ActivationFunctionType

D = 1024
SEQ = 4096
CTX = 256
NTOK = SEQ + CTX
NT = NTOK // 128
EPS = 1e-6
DFF = 2816
NJ = DFF // 128
NEXP = 8
GRID_W = 64
MLA_SCALE = 96 ** -0.5
DIFF_SCALE = 64 ** -0.5
NA_SCALE = 64 ** -0.5
LAM_INIT = 0.8 - 0.6 * math.exp(-0.0)
NEG = -30000.0
NSLOT = 23
U32 = mybir.dt.uint32


class Buf:
    __slots__ = ("name", "w", "r", "rd")

    def __init__(self, name):
        self.name = name
        self.w = None
        self.r = {}
        self.rd = []


class Op:
    __slots__ = ("eng", "fn", "deps", "sig", "dma", "sem", "semval", "val", "id", "done", "lat", "cost")


def _freeze(fn):
    if fn.__closure__ is None:
        return fn
    cells = []
    for c in fn.__closure__:
        try:
            cells.append(types.CellType(c.cell_contents))
        except ValueError:
            cells.append(c)
    g = types.FunctionType(fn.__code__, fn.__globals__, fn.__name__, fn.__defaults__, tuple(cells))
    g.__kwdefaults__ = fn.__kwdefaults__
    return g


class Sched:
    ENGS = ("pe", "act", "dve", "pool", "sp")
    NRING = 16

    def __init__(self, nc, sems):
        self.nc = nc
        self.sems = sems
        self.ops = []
        self.allbufs = []
        self.cnt = {e: 0 for e in self.ENGS}
        self.dcnt = {e: 0 for e in self.ENGS}
        self.nid = 0
        self.total = 0

    def buf(self, name=None):
        b = Buf(name or "b")
        self.allbufs.append(b)
        return b

    def add(self, eng, fn, reads=(), writes=(), dma=False, lat=3.0, cost=None):
        op = Op()
        op.lat = lat
        op.cost = cost
        op.eng = eng
        op.fn = _freeze(fn)
        op.dma = dma
        op.sig = False
        op.done = False
        op.id = self.nid
        self.nid += 1
        deps = {}
        for b in reads:
            if b.w is not None:
                deps[b.w.id] = b.w
        for b in writes:
            if b.w is not None:
                deps[b.w.id] = b.w
            for o in b.r.values():
                deps[o.id] = o
            for o in b.rd:
                deps[o.id] = o
        op.deps = [d for d in deps.values() if not d.done]
        for b in writes:
            b.w = op
            b.r = {}
            b.rd = []
        for b in reads:
            if b.w is op:
                continue
            if dma:
                b.rd.append(op)
            else:
                b.r[eng] = op
        self.ops.append(op)
        return op

    def pe(self, fn, reads=(), writes=(), cost=None):
        return self.add("pe", fn, reads, writes, cost=cost)

    def act(self, fn, reads=(), writes=(), cost=None):
        return self.add("act", fn, reads, writes, cost=cost)

    def dve(self, fn, reads=(), writes=(), cost=None):
        return self.add("dve", fn, reads, writes, cost=cost)

    def pool(self, fn, reads=(), writes=(), cost=None):
        return self.add("pool", fn, reads, writes, cost=cost)

    def dma(self, fn, reads=(), writes=(), q="sp", lat=3.0, cost=None):
        return self.add(q, fn, reads, writes, dma=True, lat=lat, cost=cost)

    COST = {"pe": 0.216, "act": 0.6, "dve": 0.6, "pool": 1.4}

    def list_schedule(self, ops):
        import heapq
        pos_ = {op.id: i for i, op in enumerate(ops)}
        nd = [0] * len(ops)
        users = [[] for _ in ops]
        for i, op in enumerate(ops):
            for d in op.deps:
                j = pos_.get(d.id)
                if j is not None:
                    nd[i] += 1
                    users[j].append(i)
        ready = [0.0] * len(ops)
        fin = [0.0] * len(ops)
        heaps = {e: [] for e in self.ENGS}
        for i, op in enumerate(ops):
            if nd[i] == 0:
                heapq.heappush(heaps[op.eng], (0.0, i))
        free = {e: 0.0 for e in self.ENGS}
        order = []
        n = len(ops)
        while len(order) < n:
            best = None
            for e in self.ENGS:
                h = heaps[e]
                if h:
                    st = max(free[e], h[0][0])
                    if best is None or st < best[0] or (st == best[0] and h[0][1] < best[2]):
                        best = (st, e, h[0][1])
            st, e, i = best
            heapq.heappop(heaps[e])
            op = ops[i]
            if op.dma:
                busy = op.cost if op.cost is not None else (1.0 if e == "pool" else 0.06)
                fin[i] = st + busy + op.lat
            else:
                busy = self.COST[e] if op.cost is None else op.cost
                fin[i] = st + busy + 0.1
            free[e] = st + busy
            order.append(op)
            for u in users[i]:
                ready[u] = max(ready[u], fin[i])
                nd[u] -= 1
                if nd[u] == 0:
                    heapq.heappush(heaps[ops[u].eng], (ready[u], u))
        return order

    def emit_phase(self, reorder=False):
        nc = self.nc
        sems = self.sems
        if reorder:
            self.ops = self.list_schedule(self.ops)
        ops = self.ops
        for op in ops:
            for d in op.deps:
                if not d.dma:
                    if op.eng == "pe" and d.eng == "pe" and not op.dma:
                        continue
                    d.sig = True
        for op in ops:
            if op.dma:
                i = self.dcnt[op.eng]
                self.dcnt[op.eng] += 1
                ring = sems["ring"][op.eng]
                op.sem = ring[i % len(ring)]
                op.semval = 16 * (i // len(ring) + 1)
            elif op.sig:
                self.cnt[op.eng] += 1
                op.val = self.cnt[op.eng]
        by_eng = {e: [o for o in ops if o.eng == e] for e in self.ENGS}
        final = {}
        for e in ("sp", "pool", "act"):
            ring = sems["ring"][e]
            n = self.dcnt[e]
            for j, s in enumerate(ring):
                k = (n - 1 - j) // len(ring) + 1 if n > j else 0
                if k > 0:
                    final[(e, j)] = (s, 16 * k)

        def run(e, eng):
            seen = {}
            for op in by_eng[e]:
                need = {}
                for d in op.deps:
                    if d.dma:
                        key = ("d", id(d.sem))
                        v = d.semval
                        s = d.sem
                    else:
                        if e == "pe" and d.eng == "pe" and not op.dma:
                            continue
                        key = ("e", d.eng)
                        v = d.val
                        s = sems["eng"][d.eng]
                    if seen.get(key, 0) < v and (key not in need or need[key][1] < v):
                        need[key] = (s, v)
                if op.dma and op.semval > 16:
                    key = ("d", id(op.sem))
                    v = op.semval - 16
                    if seen.get(key, 0) < v and (key not in need or need[key][1] < v):
                        need[key] = (op.sem, v)
                for key, (s, v) in need.items():
                    eng.wait_ge(s, v)
                    seen[key] = v
                ins = op.fn(eng)
                if op.dma:
                    ins.then_inc(op.sem, 16)
                elif op.sig:
                    ins.then_inc(sems["eng"][e], 1)
            if e == "sp":
                for s, v in final.values():
                    eng.wait_ge(s, v)

        with nc.Block() as block:
            @block.sync
            def _(eng):
                run("sp", eng)

            @block.tensor
            def _(eng):
                run("pe", eng)

            @block.scalar
            def _(eng):
                run("act", eng)

            @block.vector
            def _(eng):
                run("dve", eng)

            @block.gpsimd
            def _(eng):
                run("pool", eng)
        nc.all_engine_barrier()
        self.total += len(ops)
        for op in ops:
            op.done = True
        self.ops = []
        for b in self.allbufs:
            b.w = None
            b.r = {}
            b.rd = []
        self.allbufs = []


class TB:
    __slots__ = ("t", "b")

    def __init__(self, t, b):
        self.t = t
        self.b = b


class Rot:
    def __init__(self, items):
        self.items = items
        self.i = 0

    def next(self):
        x = self.items[self.i % len(self.items)]
        self.i += 1
        return x


CHUNKS = [(0, 256, True)] + [(256 + 512 * i, 512, False) for i in range(8)]


def na_variants():
    masks = []
    keyidx = {}
    table = {}
    kl = np.arange(128)
    kr_l, kc = kl // 64, kl % 64
    for i in range(32):
        qr = 2 * i + kr_l
        qc = kc
        rs = np.clip(qr - 4, 0, 56)
        cs = np.clip(qc - 8, 0, 48)
        for kt in range(32):
            kr = 2 * kt + kr_l
            inw = ((kr[:, None] >= rs[None, :]) & (kr[:, None] < rs[None, :] + 8)
                   & (kc[:, None] >= cs[None, :]) & (kc[:, None] < cs[None, :] + 16))
            if not inw.any():
                continue
            m = np.where(inw, 0.0, NEG).astype(np.float32)
            key = m.tobytes()
            if key not in keyidx:
                keyidx[key] = len(masks)
                masks.append(m)
            table[(i, kt)] = keyidx[key]
    return table, np.stack(masks)


NA_TABLE, NA_MASKS = na_variants()
NVAR = NA_MASKS.shape[0]
ND = 7
NA_COMBOS = []
NA_CTAB = {}
for (_i, _kt), _var in sorted(NA_TABLE.items()):
    _c = (_var, _kt - _i + 3)
    if _c not in NA_COMBOS:
        NA_COMBOS.append(_c)
    NA_CTAB[(_i, _kt)] = NA_COMBOS.index(_c)
NCOMBO = len(NA_COMBOS)
NA_CLASSES = []
NA_CLS_OF_I = {}
for _i in range(32):
    _cl = tuple(NA_CTAB[(_i, _kt)] for _kt in range(32) if (_i, _kt) in NA_CTAB)
    if _cl not in NA_CLASSES:
        NA_CLASSES.append(_cl)
    NA_CLS_OF_I[_i] = NA_CLASSES.index(_cl)
NCLS = len(NA_CLASSES)


def build_program(upto=99, debug=False):
    nc = bass.Bass("TRN2", target_bir_lowering=False)

    def din(name, shape, dt=F32):
        return nc.dram_tensor(name, list(shape), dt, kind="ExternalInput").ap()

    def dint(name, shape, dt):
        return nc.dram_tensor(name, list(shape), dt, kind=("ExternalOutput" if debug else "Internal")).ap()

    x_d = din("x", [SEQ, D])
    ctx_d = din("ctx", [CTX, D])
    cc_d = din("cc", [128, 8, 2])
    wmod_d = din("w_mod", [2, D, 6 * D])
    bmod_d = din("b_mod", [2, 6 * D])
    ng_d = din("norm_g", [2, 4 * D])
    win_d = din("a_w_in", [D, 2208])
    winp_d = din("a_w_in_p", [D, 1056])
    qn_d = din("a_q_norm_t", [128, 3])
    kvn_d = din("a_kv_norm_t", [128, 2])
    wuq_d = din("a_w_uq", [384, 768])
    wuqp_d = din("a_w_uq_p", [384, 256])
    wukv_d = din("a_w_ukv_r", [256, 1024])
    lam_d = din("b_lambda", [1, 256])
    subln_d = din("b_subln_t", [128, 1])
    wo_d = din("ab_w_out", [D, D])
    fw1_d = din("f_w1r", [NJ, 128, 8 * 128])
    fw3_d = din("f_w3r", [NJ, 128, 8 * 128])
    fw2_d = din("f_w2", [1, DFF, D])
    wqkv_d = din("c_w_qkv", [D, 3 * D])
    bqk_d = din("c_b_qk_t", [128, 16])
    bv_d = din("c_b_v", [1, D])
    rpbg_d = din("rpbg", [16, ND, 128, 128])
    nmask_d = din("nmask", [NVAR, 128, 128])
    cwo_d = din("c_w_out", [D, D])
    cbo_d = din("c_b_out", [1, D])
    rt_d = din("m_router", [D, NEXP])
    mw1_d = din("m_w1r", [NEXP * 11 * 128, 2048])
    mw3_d = din("m_w3r", [NEXP * 11 * 128, 2048])
    mw2_d = din("m_w2r", [NEXP * 11 * 128, 2048])
    rconst_d = din("rconst", [128, 161])
    cosA_d = din("cosA", [128, SEQ])
    sinA_d = din("sinA", [128, SEQ])
    cosB_d = din("cosB", [128, SEQ])
    sinB_d = din("sinB", [128, SEQ])
    ident_d = din("ident", [128, 128])
    out_d = nc.dram_tensor("out", [SEQ, D], F32, kind="ExternalOutput").ap()

    modrows_d = dint("modrows", [2, 2, 6, D], F32)
    qA_d = dint("qA", [8, 96, NTOK], BF16)
    kAn_d = dint("kAn", [8 * 64, NTOK], BF16)
    kAr_d = dint("kAr", [32, NTOK], BF16)
    vA_d = dint("vA", [NTOK, 512], BF16)
    qB_d = dint("qB", [4, 128, NTOK], BF16)
    kB_d = dint("kB", [4, 128, NTOK], BF16)
    vB_d = dint("vB", [NTOK, 512], BF16)
    cs_d = dint("cs", [CTX, D], F32)
    hT_d = dint("hT", [128, 8, NTOK], BF16)
    qC_d = dint("qC", [8, 128, NTOK], BF16)
    kC_d = dint("kC", [8, 128, NTOK], BF16)
    vC_d = dint("vC", [NTOK, D], BF16)
    oT_d = dint("oT", [128, 8, NTOK], BF16)
    comb_d = dint("comb", [128, 32, NEXP], F32)
    h4_d = dint("h4", [SEQ, D], BF16)
    Hs_d = dint("Hs", [NSLOT * 512, D], BF16)
    R_d = dint("R", [NSLOT * 512, D], F32)
    wb_d = [dint(f"wb{i}", [NEXP * 11 * 128, 2048], BF16) for i in range(3)]

    with ExitStack() as ges:
        sems = {"eng": {}, "ring": {}}
        for e in Sched.ENGS:
            sems["eng"][e] = ges.enter_context(nc.semaphore("s_" + e))
        for e in ("sp", "pool", "act"):
            sems["ring"][e] = [ges.enter_context(nc.semaphore(f"r_{e}{i}")) for i in range(Sched.NRING)]
        S = Sched(nc, sems)

        pp = [ges.enter_context(nc.psum_tensor(f"pp{i}", [128, 1024], F32)) for i in range(4)]

        def bank_ap(i):
            return pp[i // 2][:, (i % 2) * 512:(i % 2 + 1) * 512]

        def gsb(name, shape, dt):
            return ges.enter_context(nc.sbuf_tensor(name, list(shape), dt))

        ident_f = gsb("ident_f", [128, 128], F32)
        ident_b = gsb("ident_b", [128, 128], BF16)
        ones_b = gsb("ones_b", [128, 128], BF16)
        ones_f = gsb("ones_f", [128, 128], F32)

        class Phase:
            def __init__(self, reorder=False):
                self.reorder = reorder
                self.es = ExitStack()
                self.n = 0
                self.banks = [TB(bank_ap(i), S.buf(f"bank{i}")) for i in range(8)]
                self.pairs = [TB(pp[i], S.buf(f"pair{i}")) for i in range(4)]
                self.ident_f = TB(ident_f, S.buf("identf"))
                self.ident_b = TB(ident_b, S.buf("identb"))
                self.ones_b = TB(ones_b, S.buf("onesb"))
                self.ones_f = TB(ones_f, S.buf("onesf"))
                self.dram = {}

            def sb(self, shape, dt, name=None):
                self.n += 1
                t = self.es.enter_context(nc.sbuf_tensor(f"{name or 't'}_{S.total}_{self.n}", list(shape), dt))
                return TB(t, S.buf(name))

            def rot(self, k, shape, dt, name=None):
                return Rot([self.sb(shape, dt, (name or "r") + f"_{S.total}_{self.n}_{i}") for i in range(k)])

            def db(self, key):
                if key not in self.dram:
                    self.dram[key] = S.buf(str(key))
                return self.dram[key]

            def close(self):
                S.emit_phase(reorder=self.reorder)
                self.es.close()

        ph = Phase(reorder=True)
        S.dma(lambda e: e.dma_start(out=ident_f[:], in_=ident_d), writes=[ph.ident_f.b])
        S.dma(lambda e: e.dma_start(out=ident_b[:], in_=ident_d), writes=[ph.ident_b.b], q="pool")
        S.dve(lambda e: e.memset(ones_b[:], 1.0), writes=[ph.ones_b.b])
        S.dve(lambda e: e.memset(ones_f[:], 1.0), writes=[ph.ones_f.b])
        cc = ph.sb([128, 8, 2], F32)
        ccs = ph.sb([128, 8, 2], BF16)
        S.dma(lambda e: e.dma_start(out=cc.t[:], in_=cc_d), writes=[cc.b])
        S.act(lambda e: e.activation(out=ccs.t[:], in_=cc.t[:], func=AF.Silu), reads=[cc.b], writes=[ccs.b])
        wblk = ph.rot(3, [128, 8, 512], BF16)
        modsb = [ph.sb([2, 6 * D], F32) for _ in range(2)]
        brow = [ph.sb([2, 6 * D], F32) for _ in range(2)]
        grow = [ph.sb([2, 4 * D], F32) for _ in range(2)]
        drow = [ph.sb([2, 6 * D], F32) for _ in range(2)]
        pbank = Rot(ph.banks)
        for i in range(2):
            S.dma(lambda e, i=i: e.dma_start(out=brow[i].t[:], in_=bmod_d[i:i + 1, :].partition_broadcast(2)), writes=[brow[i].b])
            S.dma(lambda e, i=i: e.dma_start(out=grow[i].t[:], in_=ng_d[i:i + 1, :].partition_broadcast(2)), writes=[grow[i].b])
            wv = wmod_d[i].rearrange("(k p) n -> p k n", p=128)
            for nb in range(12):
                w = wblk.next()
                S.dma(lambda e, w=w, nb=nb, wv=wv: e.dma_start(out=w.t[:], in_=wv[:, :, nb * 512:(nb + 1) * 512]), writes=[w.b], q="pool")
                pb = pbank.next()
                for k in range(8):
                    S.pe(lambda e, w=w, k=k, pb=pb: e.matmul(pb.t[0:2, :], lhsT=ccs.t[:, k, :], rhs=w.t[:, k, :], start=(k == 0), stop=(k == 7)),
                         reads=[ccs.b, w.b], writes=[pb.b])
                S.dve(lambda e, i=i, nb=nb, pb=pb: e.tensor_tensor(out=modsb[i].t[:, nb * 512:(nb + 1) * 512], in0=pb.t[0:2, :],
                                                                     in1=brow[i].t[:, nb * 512:(nb + 1) * 512], op=ALU.add),
                      reads=[pb.b, brow[i].b], writes=[modsb[i].b])
            m, g, dr = modsb[i], grow[i], drow[i]

            def sl(j):
                return slice(j * D, (j + 1) * D)
            S.dve(lambda e, m=m, g=g, dr=dr: e.scalar_tensor_tensor(out=dr.t[:, sl(0)], in0=m.t[:, sl(1)], scalar=1.0, in1=g.t[:, sl(0)], op0=ALU.add, op1=ALU.mult),
                  reads=[m.b, g.b], writes=[dr.b])
            S.dve(lambda e, m=m, dr=dr: e.tensor_copy(out=dr.t[:, sl(1)], in_=m.t[:, sl(0)]), reads=[m.b], writes=[dr.b])
            S.dve(lambda e, m=m, g=g, dr=dr: e.tensor_tensor(out=dr.t[:, sl(2)], in0=m.t[:, sl(2)], in1=g.t[:, sl(1)], op=ALU.mult), reads=[m.b, g.b], writes=[dr.b])
            S.dve(lambda e, m=m, g=g, dr=dr: e.scalar_tensor_tensor(out=dr.t[:, sl(3)], in0=m.t[:, sl(4)], scalar=1.0, in1=g.t[:, sl(2)], op0=ALU.add, op1=ALU.mult),
                  reads=[m.b, g.b], writes=[dr.b])
            S.dve(lambda e, m=m, dr=dr: e.tensor_copy(out=dr.t[:, sl(4)], in_=m.t[:, sl(3)]), reads=[m.b], writes=[dr.b])
            S.dve(lambda e, m=m, g=g, dr=dr: e.tensor_tensor(out=dr.t[:, sl(5)], in0=m.t[:, sl(5)], in1=g.t[:, sl(3)], op=ALU.mult), reads=[m.b, g.b], writes=[dr.b])
            S.dma(lambda e, i=i, dr=dr: e.dma_start(out=modrows_d[i].rearrange("a r d -> a (r d)"), in_=dr.t[:]), reads=[dr.b], writes=[ph.db("modrows")])
        ph.close()

        def load_bc(ph, layer, which, row):
            t = ph.sb([128, D], F32)
            S.dma(lambda e: e.dma_start(out=t.t[:], in_=modrows_d[layer, which, row:row + 1, :].partition_broadcast(128)), writes=[t.b])
            return t

        class NormCtx:
            def __init__(self, ph):
                self.st = ph.rot(4, [128, 8], F32, "st")
                self.tmp = ph.rot(2, [128, D], F32, "ntmp")
                self.hb = ph.rot(2, [128, D], BF16, "hb")

        def rstd_from_ss(st, ncols, dim):
            if ncols == 2:
                S.dve(lambda e: e.tensor_tensor(out=st.t[:, 0:1], in0=st.t[:, 0:1], in1=st.t[:, 1:2], op=ALU.add), reads=[st.b], writes=[st.b])
            S.dve(lambda e: e.tensor_scalar(out=st.t[:, 4:5], in0=st.t[:, 0:1], scalar1=1.0 / dim, scalar2=EPS, op0=ALU.mult, op1=ALU.add), reads=[st.b], writes=[st.b])
            S.act(lambda e: e.activation(out=st.t[:, 5:6], in_=st.t[:, 4:5], func=AF.Sqrt), reads=[st.b], writes=[st.b])
            S.dve(lambda e: e.reciprocal(out=st.t[:, 7:8], in_=st.t[:, 5:6]), reads=[st.b], writes=[st.b])

        def norm_mod_T(ph, nx, xt, A, sh, dst_ap, dst_b, tbank):
            st, tmp, hb = nx.st.next(), nx.tmp.next(), nx.hb.next()
            junk = tmp
            S.dve(lambda e: e.memset(st.t[:], 0.0), writes=[st.b])
            S.act(lambda e: e.activation(out=junk.t[:], in_=xt.t[:], func=AF.Square, accum_out=st.t[:, 0:1]), reads=[xt.b], writes=[junk.b, st.b])
            rstd_from_ss(st, 1, D)
            S.dve(lambda e: e.scalar_tensor_tensor(out=tmp.t[:], in0=xt.t[:], scalar=st.t[:, 7:8], in1=A.t[:], op0=ALU.mult, op1=ALU.mult),
                  reads=[xt.b, st.b, A.b], writes=[tmp.b])
            S.pool(lambda e: e.tensor_tensor(out=hb.t[:], in0=tmp.t[:], in1=sh.t[:], op=ALU.add), reads=[tmp.b, sh.b], writes=[hb.b])
            pv = tbank.t.bitcast(BF16)
            for k in range(8):
                S.pe(lambda e, k=k: e.transpose(out=pv[:, k * 128:(k + 1) * 128], in_=hb.t[:, k * 128:(k + 1) * 128], identity=ph.ident_b.t[:]),
                     reads=[hb.b, ph.ident_b.b], writes=[tbank.b])
            S.act(lambda e: e.activation(out=dst_ap, in_=pv.rearrange("p (k t) -> p k t", k=8), func=AF.Copy), reads=[tbank.b], writes=[dst_b])
            return hb

        def tok_src(t):
            if t < 2:
                return ctx_d[t * 128:(t + 1) * 128, :]
            return x_d[(t - 2) * 128:(t - 1) * 128, :]

        def res_ap(t):
            if t < 2:
                return cs_d[t * 128:(t + 1) * 128, :]
            return out_d[(t - 2) * 128:(t - 1) * 128, :]

        def castload(ph, dst, src_ap, q="pool"):
            S.dma(lambda e: e.dma_start(out=dst.t[:], in_=src_ap), writes=[dst.b], q=q)

        def mm_acc(out_ap, out_b, pairs, reads):
            n = len(pairs)
            for i, (l, r) in enumerate(pairs):
                S.pe(lambda e, l=l, r=r, i=i: e.matmul(out_ap, lhsT=l, rhs=r, start=(i == 0), stop=(i == n - 1)), reads=reads, writes=[out_b])

        if upto >= 1:
            ph = Phase(reorder=True)
            nx = NormCtx(ph)
            A0 = [load_bc(ph, 0, w, 0) for w in (0, 1)]
            SH0 = [load_bc(ph, 0, w, 1) for w in (0, 1)]
            win = ph.sb([128, 8, 2208], BF16)
            winp = ph.sb([128, 8, 1056], BF16)
            wuq = ph.sb([128, 3, 768], BF16)
            wuqp = ph.sb([128, 3, 256], BF16)
            wukv = ph.sb([128, 2, 1024], BF16)
            qn = ph.sb([128, 3], F32)
            kvn = ph.sb([128, 2], F32)
            stg_r = ph.rot(2, [128, 1024], F32, "stg")
            wv = win_d.rearrange("(k p) n -> p k n", p=128)
            for k in range(8):
                S.dma(lambda e, k=k: e.dma_start(out=win.t[:, k, :], in_=wv[:, k, :]), writes=[win.b], q="pool")
            wv2 = winp_d.rearrange("(k p) n -> p k n", p=128)
            for k in range(8):
                S.dma(lambda e, k=k: e.dma_start(out=winp.t[:, k, :], in_=wv2[:, k, :]), writes=[winp.b], q="pool")
            S.dma(lambda e: e.dma_start(out=qn.t[:], in_=qn_d), writes=[qn.b])
            S.dma(lambda e: e.dma_start(out=kvn.t[:], in_=kvn_d), writes=[kvn.b])
            for (dst, src_d, nk, nc_, gn) in ((wuq, wuq_d, 3, 768, qn), (wuqp, wuqp_d, 3, 256, qn), (wukv, wukv_d, 2, 1024, kvn)):
                for k in range(nk):
                    stg = stg_r.next()
                    S.dma(lambda e, stg=stg, k=k, src_d=src_d, nc_=nc_: e.dma_start(out=stg.t[:, 0:nc_], in_=src_d[k * 128:(k + 1) * 128, :]), writes=[stg.b])
                    S.dve(lambda e, stg=stg, k=k, dst=dst, nc_=nc_, gn=gn: e.tensor_scalar(out=dst.t[:, k, :], in0=stg.t[:, 0:nc_], scalar1=gn.t[:, k:k + 1], scalar2=None, op0=ALU.mult),
                          reads=[stg.b, gn.b], writes=[dst.b])
            for k in range(3):
                v = wuqp.t[:, k, :].rearrange("p (h q e) -> p h q e", h=8, q=4)
                for qq in (0, 2):
                    S.dve(lambda e, v=v, qq=qq: e.tensor_scalar(out=v[:, :, qq, :], in0=v[:, :, qq, :], scalar1=-1.0, scalar2=None, op0=ALU.mult),
                          reads=[wuqp.b], writes=[wuqp.b])
            for k in range(8):
                v0 = winp.t[:, k, 0:32].rearrange("p (q e) -> p q e", q=4)
                v1 = winp.t[:, k, 32:1056].rearrange("p (h q e) -> p h q e", h=16, q=4)
                for qq in (0, 2):
                    S.pool(lambda e, v0=v0, qq=qq: e.tensor_scalar(out=v0[:, qq, :], in0=v0[:, qq, :], scalar1=-1.0, scalar2=None, op0=ALU.mult),
                           reads=[winp.b], writes=[winp.b])
                    S.pool(lambda e, v1=v1, qq=qq: e.tensor_scalar(out=v1[:, :, qq, :], in0=v1[:, :, qq, :], scalar1=-1.0, scalar2=None, op0=ALU.mult),
                           reads=[winp.b], writes=[winp.b])

            xt_r = ph.rot(2, [128, D], F32, "xt")
            hT_r = ph.rot(2, [128, 8, 512], BF16, "hT")
            cq_r = ph.rot(1, [128, 5, 512], F32, "cq")
            sq_r = ph.rot(2, [128, 512], BF16, "sq")
            rs_r = ph.rot(2, [128, 512], F32, "rs")
            cn_r = ph.rot(2, [128, 5, 512], BF16, "cn")
            ob_r = ph.rot(4, [128, 512], BF16, "ob")
            t1_r = ph.rot(2, [128, 512], F32, "t1")
            t2_r = ph.rot(2, [128, 512], F32, "t2")
            rope_r = ph.rot(2, [128, 4, 512], F32, "rope")
            bk = Rot(ph.banks[0:7])
            tb = ph.banks[7]

            for (tok0, n, is_ctx) in CHUNKS:
                w = 0 if is_ctx else 1
                which = 1 if is_ctx else 0
                hT = hT_r.next()
                for j in range(n // 128):
                    t = tok0 // 128 + j
                    xt = xt_r.next()
                    S.dma(lambda e, xt=xt, t=t: e.dma_start(out=xt.t[:], in_=tok_src(t)), writes=[xt.b])
                    norm_mod_T(ph, nx, xt, A0[which], SH0[which], hT.t[:, :, j * 128:(j + 1) * 128], hT.b, tb)
                if not is_ctx:
                    rp = rope_r.next()
                    p0 = tok0 - CTX
                    for ii, src in enumerate((cosA_d, sinA_d, cosB_d, sinB_d)):
                        S.dma(lambda e, ii=ii, src=src, rp=rp, p0=p0: e.dma_start(out=rp.t[:, ii, :], in_=src[:, p0:p0 + 512]), writes=[rp.b])
                cq = cq_r.next()
                cn = cn_r.next()
                for (b0, nb, dim) in ((0, 3, 384.0), (3, 2, 256.0)):
                    ssb = bk.next()
                    for bi in range(nb):
                        blk = b0 + bi
                        pb = bk.next()
                        mm_acc(pb.t[:, 0:n], pb.b, [(win.t[:, k, blk * 128:(blk + 1) * 128], hT.t[:, k, 0:n]) for k in range(8)], [win.b, hT.b])
                        sq = sq_r.next()
                        S.act(lambda e, sq=sq, pb=pb: e.activation(out=sq.t[:, 0:n], in_=pb.t[:, 0:n], func=AF.Square), reads=[pb.b], writes=[sq.b])
                        S.act(lambda e, cq=cq, pb=pb, blk=blk: e.activation(out=cq.t[:, blk, 0:n], in_=pb.t[:, 0:n], func=AF.Copy), reads=[pb.b], writes=[cq.b])
                        S.pe(lambda e, sq=sq, ssb=ssb, bi=bi, nb=nb: e.matmul(ssb.t[:, 0:n], lhsT=ph.ones_b.t[:], rhs=sq.t[:, 0:n], start=(bi == 0), stop=(bi == nb - 1)),
                             reads=[sq.b, ph.ones_b.b], writes=[ssb.b])
                    rs = rs_r.next()
                    S.dve(lambda e, rs=rs, ssb=ssb, dim=dim: e.tensor_scalar(out=rs.t[:, 0:n], in0=ssb.t[:, 0:n], scalar1=1.0 / dim, scalar2=EPS, op0=ALU.mult, op1=ALU.add),
                          reads=[ssb.b], writes=[rs.b])
                    S.act(lambda e, rs=rs: e.activation(out=rs.t[:, 0:n], in_=rs.t[:, 0:n], func=AF.Sqrt), reads=[rs.b], writes=[rs.b])
                    S.dve(lambda e, rs=rs: e.reciprocal(out=rs.t[:, 0:n], in_=rs.t[:, 0:n]), reads=[rs.b], writes=[rs.b])
                    for bi in range(nb):
                        blk = b0 + bi
                        S.dve(lambda e, blk=blk, rs=rs, cq=cq, cn=cn: e.tensor_tensor(out=cn.t[:, blk, 0:n], in0=cq.t[:, blk, 0:n], in1=rs.t[:, 0:n], op=ALU.mult),
                              reads=[cq.b, rs.b], writes=[cn.b])

                def rope_out(pa, pb2, lo, hi, ci, dst):
                    t1, t2 = t1_r.next(), t2_r.next()
                    S.dve(lambda e: e.tensor_tensor(out=t1.t[lo:hi, 0:n], in0=pa.t[lo:hi, 0:n], in1=rp.t[lo:hi, ci, 0:n], op=ALU.mult), reads=[pa.b, rp.b], writes=[t1.b])
                    S.dve(lambda e: e.tensor_tensor(out=t2.t[lo:hi, 0:n], in0=pb2.t[lo:hi, 0:n], in1=rp.t[lo:hi, ci + 1, 0:n], op=ALU.mult), reads=[pb2.b, rp.b], writes=[t2.b])
                    S.pool(lambda e: e.tensor_tensor(out=dst.t[lo:hi, 0:n], in0=t1.t[lo:hi, 0:n], in1=t2.t[lo:hi, 0:n], op=ALU.add), reads=[t1.b, t2.b], writes=[dst.b])

                for h in range(8):
                    pa = bk.next()
                    mm_acc(pa.t[0:96, 0:n], pa.b, [(wuq.t[:, k, h * 96:(h + 1) * 96], cn.t[:, k, 0:n]) for k in range(3)], [wuq.b, cn.b])
                    ob = ob_r.next()
                    if is_ctx:
                        S.act(lambda e, ob=ob, pa=pa: e.activation(out=ob.t[0:96, 0:n], in_=pa.t[0:96, 0:n], func=AF.Copy), reads=[pa.b], writes=[ob.b])
                    else:
                        pb2 = bk.next()
                        mm_acc(pb2.t[64:96, 0:n], pb2.b, [(wuqp.t[:, k, h * 32:(h + 1) * 32], cn.t[:, k, 0:n]) for k in range(3)], [wuqp.b, cn.b])
                        S.act(lambda e, ob=ob, pa=pa: e.activation(out=ob.t[0:64, 0:n], in_=pa.t[0:64, 0:n], func=AF.Copy), reads=[pa.b], writes=[ob.b])
                        rope_out(pa, pb2, 64, 96, 0, ob)
                    S.dma(lambda e, ob=ob, h=h: e.dma_start(out=qA_d[h, :, tok0:tok0 + n], in_=ob.t[0:96, 0:n]), reads=[ob.b], writes=[ph.db(("qA", h))])
                pa = bk.next()
                mm_acc(pa.t[64:96, 0:n], pa.b, [(win.t[:, k, 640:672], hT.t[:, k, 0:n]) for k in range(8)], [win.b, hT.b])
                ob = ob_r.next()
                if is_ctx:
                    S.act(lambda e, ob=ob, pa=pa: e.activation(out=ob.t[64:96, 0:n], in_=pa.t[64:96, 0:n], func=AF.Copy), reads=[pa.b], writes=[ob.b])
                else:
                    pb2 = bk.next()
                    mm_acc(pb2.t[64:96, 0:n], pb2.b, [(winp.t[:, k, 0:32], hT.t[:, k, 0:n]) for k in range(8)], [winp.b, hT.b])
                    rope_out(pa, pb2, 64, 96, 0, ob)
                S.dma(lambda e, ob=ob: e.dma_start(out=kAr_d[:, tok0:tok0 + n], in_=ob.t[64:96, 0:n]), reads=[ob.b], writes=[ph.db("kAr")])
                for hp in range(4):
                    pa = bk.next()
                    mm_acc(pa.t[:, 0:n], pa.b, [(wukv.t[:, k, hp * 128:(hp + 1) * 128], cn.t[:, 3 + k, 0:n]) for k in range(2)], [wukv.b, cn.b])
                    ob = ob_r.next()
                    S.act(lambda e, ob=ob, pa=pa: e.activation(out=ob.t[:, 0:n], in_=pa.t[:, 0:n], func=AF.Copy), reads=[pa.b], writes=[ob.b])
                    S.dma(lambda e, ob=ob, hp=hp: e.dma_start(out=kAn_d[hp * 128:(hp + 1) * 128, tok0:tok0 + n], in_=ob.t[:, 0:n]), reads=[ob.b], writes=[ph.db(("kAn", hp))])
                for j in range(n // 128):
                    ts = slice(j * 128, (j + 1) * 128)
                    r0 = tok0 + j * 128
                    pa = bk.next()
                    mm_acc(pa.t[:, :], pa.b, [(cn.t[:, 3 + k, ts], wukv.t[:, k, 512:1024]) for k in range(2)], [wukv.b, cn.b])
                    ob = ob_r.next()
                    S.act(lambda e, ob=ob, pa=pa: e.activation(out=ob.t[:, :], in_=pa.t[:, :], func=AF.Copy), reads=[pa.b], writes=[ob.b])
                    S.dma(lambda e, ob=ob, r0=r0: e.dma_start(out=vA_d[r0:r0 + 128, :], in_=ob.t[:, :]), reads=[ob.b], writes=[ph.db("vA")])
                    pa = bk.next()
                    mm_acc(pa.t[:, :], pa.b, [(hT.t[:, k, ts], win.t[:, k, 1696:2208]) for k in range(8)], [win.b, hT.b])
                    ob = ob_r.next()
                    S.act(lambda e, ob=ob, pa=pa: e.activation(out=ob.t[:, :], in_=pa.t[:, :], func=AF.Copy), reads=[pa.b], writes=[ob.b])
                    S.dma(lambda e, ob=ob, r0=r0: e.dma_start(out=vB_d[r0:r0 + 128, :], in_=ob.t[:, :]), reads=[ob.b], writes=[ph.db("vB")])
                for (c0, cp0, dst_d, nm) in ((672, 32, qB_d, "qB"), (1184, 544, kB_d, "kB")):
                    for h in range(4):
                        pa = bk.next()
                        mm_acc(pa.t[:, 0:n], pa.b, [(win.t[:, k, c0 + h * 128:c0 + (h + 1) * 128], hT.t[:, k, 0:n]) for k in range(8)], [win.b, hT.b])
                        ob = ob_r.next()
                        if is_ctx:
                            S.act(lambda e, ob=ob, pa=pa: e.activation(out=ob.t[:, 0:n], in_=pa.t[:, 0:n], func=AF.Copy), reads=[pa.b], writes=[ob.b])
                        else:
                            pb2 = bk.next()
                            mm_acc(pb2.t[:, 0:n], pb2.b, [(winp.t[:, k, cp0 + h * 128:cp0 + (h + 1) * 128], hT.t[:, k, 0:n]) for k in range(8)], [winp.b, hT.b])
                            rope_out(pa, pb2, 0, 128, 2, ob)
                        S.dma(lambda e, ob=ob, h=h, dst_d=dst_d: e.dma_start(out=dst_d[h, :, tok0:tok0 + n], in_=ob.t[:, 0:n]), reads=[ob.b], writes=[ph.db((nm, h))])
            ph.close()

        pc_jobs = [(i, g4) for g4 in range(22) for i in range(3)]
        pc_srcs = (mw1_d, mw3_d, mw2_d)

        def precast_job(ph, pc_r, after_buf):
            if not pc_jobs:
                return
            i, g4 = pc_jobs.pop(0)
            pc = pc_r.next()
            rows = slice(g4 * 512, (g4 + 1) * 512)
            S.dma(lambda e: e.dma_start(out=pc.t[:], in_=pc_srcs[i][rows, :].rearrange("(a p) n -> p a n", p=128)), reads=[after_buf], writes=[pc.b], q="pool", lat=40.0)
            S.dma(lambda e: e.dma_start(out=wb_d[i][rows, :].rearrange("(a p) n -> p a n", p=128), in_=pc.t[:]), reads=[pc.b], writes=[ph.db(("wb", i, g4))])

        if upto >= 2:
            ph = Phase(reorder=True)
            KT_r = ph.rot(2, [128, NTOK], BF16, "KT")
            QT_r = ph.rot(2, [128, NTOK], BF16, "QT")
            VA_r = ph.rot(2, [128, NT, 128], BF16, "VA")
            QZ_r = [ph.rot(2, [128, NTOK], BF16, "QZ0"), ph.rot(2, [128, NTOK], BF16, "QZ1")]
            for m_ in range(2):
                for qz in QZ_r[m_].items:
                    zl = 64 * (1 - m_)
                    S.pool(lambda e, qz=qz, zl=zl: e.memset(qz.t[zl:zl + 64, :], 0.0), writes=[qz.b])
            PT_r = ph.rot(6, [128, 512], BF16, "PT")
            acc_r = ph.rot(2, [128, 512], F32, "acc")
            pr_r = ph.rot(3, [128, 512], BF16, "pr")
            hl_r = ph.rot(4, [128, 512], BF16, "hl")
            rc_r = ph.rot(2, [128, 512], F32, "rc")
            o1_r = ph.rot(2, [128, 512], F32, "o1")
            o2_r = ph.rot(2, [128, 512], F32, "o2")
            od_r = ph.rot(2, [128, 512], F32, "od")
            sq_r = ph.rot(2, [128, 512], BF16, "sq2")
            rs_r = ph.rot(2, [128, 512], F32, "rs2")
            on_r = ph.rot(3, [128, 512], BF16, "on")
            lamt = ph.sb([128, 256], F32, "lamt")
            lam2 = ph.sb([128, 128], F32, "lam2")
            lst = ph.sb([128, 8], F32, "lst")
            gsub = ph.sb([128, 1], F32, "gsub")
            Sb = Rot(ph.banks[0:4])
            Ob = Rot(ph.banks[4:6])
            Mb = Rot(ph.banks[6:7])
            ssb = ph.banks[7]
            S.pool(lambda e: e.memset(VA_r.items[0].t[:, :, 64:128], 1.0), writes=[VA_r.items[0].b])
            S.pool(lambda e: e.memset(VA_r.items[1].t[:, :, 0:64], 1.0), writes=[VA_r.items[1].b])
            S.dma(lambda e: e.dma_start(out=lamt.t[:], in_=lam_d.partition_broadcast(128)), writes=[lamt.b])
            S.dma(lambda e: e.dma_start(out=gsub.t[:], in_=subln_d), writes=[gsub.b])
            S.dve(lambda e: e.memset(lst.t[:], 0.0), writes=[lst.b])
            S.dve(lambda e: e.tensor_tensor(out=lam2.t[:, 0:64], in0=lamt.t[:, 0:64], in1=lamt.t[:, 64:128], op=ALU.mult), reads=[lamt.b], writes=[lam2.b])
            S.dve(lambda e: e.tensor_tensor(out=lam2.t[:, 64:128], in0=lamt.t[:, 128:192], in1=lamt.t[:, 192:256], op=ALU.mult), reads=[lamt.b], writes=[lam2.b])
            S.dve(lambda e: e.tensor_reduce(out=lst.t[:, 0:2], in_=lam2.t[:].rearrange("p (a d) -> p a d", a=2), axis=mybir.AxisListType.X, op=ALU.add), reads=[lam2.b], writes=[lst.b])
            S.act(lambda e: e.activation(out=lst.t[:, 2:4], in_=lst.t[:, 0:2], func=AF.Exp), reads=[lst.b], writes=[lst.b])
            S.dve(lambda e: e.tensor_tensor(out=lst.t[:, 4:5], in0=lst.t[:, 3:4], in1=lst.t[:, 2:3], op=ALU.subtract), reads=[lst.b], writes=[lst.b])
            S.dve(lambda e: e.tensor_scalar(out=lst.t[:, 5:6], in0=lst.t[:, 4:5], scalar1=-LAM_INIT, scalar2=None, op0=ALU.add), reads=[lst.b], writes=[lst.b])
            S.dve(lambda e: e.tensor_scalar(out=gsub.t[:], in0=gsub.t[:], scalar1=(1.0 - LAM_INIT), scalar2=None, op0=ALU.mult), reads=[gsub.b], writes=[gsub.b])

            pc_r = ph.rot(3, [128, 4, 2048], BF16, "pc")

            def precast_step(after_buf):
                precast_job(ph, pc_r, after_buf)

            def attend(KT, klo, khi, QT, tok0, n, kts, scale, pv_fn):
                nk = len(kts)
                pts = [None] * nk

                def qk(i):
                    kt = kts[i]
                    sb = Sb.next()
                    S.pe(lambda e: e.matmul(sb.t[:, 0:n], lhsT=KT.t[klo:khi, kt * 128:(kt + 1) * 128], rhs=QT.t[klo:khi, tok0:tok0 + n], start=True, stop=True),
                         reads=[KT.b, QT.b], writes=[sb.b], cost=0.22)
                    pt = PT_r.next()
                    S.act(lambda e: e.activation(out=pt.t[:, 0:n], in_=sb.t[:, 0:n], func=AF.Exp, scale=scale), reads=[sb.b], writes=[pt.b], cost=0.5)
                    pts[i] = pt
                LA = 2
                for i in range(min(LA, nk)):
                    qk(i)
                for i in range(nk):
                    if i + LA < nk:
                        qk(i + LA)
                    pv_fn(kts[i], pts[i], i == 0, i == nk - 1)

            def load_vtiles(VA, c0, c1, src):
                for (t0, t1) in ((0, 17), (17, 34)):
                    S.dma(lambda e: e.dma_start(out=VA.t[:, t0:t1, c0:c1], in_=src[t0 * 128:t1 * 128, :].rearrange("(t p) d -> p t d", p=128)), writes=[VA.b])

            def mla_head(h):
                KT, QT, VA = KT_r.next(), QT_r.next(), VA_r.next()
                odd = h % 2
                S.dma(lambda e: e.dma_start(out=KT.t[0:64, :], in_=kAn_d[h * 64:(h + 1) * 64, :]), writes=[KT.b])
                S.dma(lambda e: e.dma_start(out=KT.t[64:96, :], in_=kAr_d[:, :]), writes=[KT.b])
                S.dma(lambda e: e.dma_start(out=QT.t[0:96, :], in_=qA_d[h, :, :]), writes=[QT.b])
                load_vtiles(VA, 64 * odd, 64 * odd + 64, vA_d[:, h * 64:(h + 1) * 64])
                olo, slo = (64, 0) if odd else (0, 64)
                for (tok0, n, is_ctx) in CHUNKS:
                    kts = [0, 1] if is_ctx else list(range(NT))
                    ob_ = Ob.next()

                    def pv(kt, pt, first, last, ob_=ob_, n=n):
                        S.pe(lambda e: e.matmul(ob_.t[:, 0:n], lhsT=VA.t[:, kt, :], rhs=pt.t[:, 0:n], start=first, stop=last), reads=[VA.b, pt.b], writes=[ob_.b], cost=0.38)
                    attend(KT, 0, 96, QT, tok0, n, kts, MLA_SCALE, pv)
                    rc, on = rc_r.next(), on_r.next()
                    S.dve(lambda e: e.reciprocal(out=rc.t[olo:olo + 64, 0:n], in_=ob_.t[slo:slo + 64, 0:n]), reads=[ob_.b], writes=[rc.b], cost=3.4)
                    S.dve(lambda e: e.tensor_tensor(out=on.t[olo:olo + 64, 0:n], in0=ob_.t[olo:olo + 64, 0:n], in1=rc.t[olo:olo + 64, 0:n], op=ALU.mult),
                          reads=[ob_.b, rc.b], writes=[on.b])
                    S.dma(lambda e: e.dma_start(out=oT_d[olo:olo + 64, h // 2, tok0:tok0 + n], in_=on.t[olo:olo + 64, 0:n]), reads=[on.b], writes=[ph.db(("oT", h))])

            def diff_head(h):
                KT, VA = KT_r.next(), VA_r.next()
                QZ = [QZ_r[0].next(), QZ_r[1].next()]
                S.dma(lambda e: e.dma_start(out=KT.t[:, :], in_=kB_d[h, :, :]), writes=[KT.b])
                S.dma(lambda e: e.dma_start(out=QZ[0].t[0:64, :], in_=qB_d[h, 0:64, :]), writes=[QZ[0].b])
                S.dma(lambda e: e.dma_start(out=QZ[1].t[64:128, :], in_=qB_d[h, 64:128, :]), writes=[QZ[1].b])
                load_vtiles(VA, 0, 128, vB_d[:, h * 128:(h + 1) * 128])
                for (tok0, n, is_ctx) in CHUNKS:
                    kts = [0, 1] if is_ctx else list(range(NT))
                    om = []
                    for m in range(2):
                        ob_, mb_, acc = Ob.next(), Mb.next(), acc_r.next()
                        stt_ = {"prev": None, "n": 0}

                        def pv(kt, pt, first, last, ob_=ob_, acc=acc, n=n, stt_=stt_):
                            S.pe(lambda e: e.matmul(ob_.t[:, 0:n], lhsT=VA.t[:, kt, :], rhs=pt.t[:, 0:n], start=first, stop=last), reads=[VA.b, pt.b], writes=[ob_.b], cost=0.38)
                            if stt_["prev"] is None and not last:
                                stt_["prev"] = pt
                                return
                            src = pt
                            if stt_["prev"] is not None:
                                pp_ = stt_["prev"]
                                stt_["prev"] = None
                                pr_ = pr_r.next()
                                S.dve(lambda e: e.tensor_tensor(out=pr_.t[:, 0:n], in0=pp_.t[:, 0:n], in1=pt.t[:, 0:n], op=ALU.add), reads=[pp_.b, pt.b], writes=[pr_.b], cost=0.3)
                                src = pr_
                            if stt_["n"] == 0:
                                S.dve(lambda e: e.tensor_copy(out=acc.t[:, 0:n], in_=src.t[:, 0:n]), reads=[src.b], writes=[acc.b])
                            else:
                                S.dve(lambda e: e.tensor_tensor(out=acc.t[:, 0:n], in0=acc.t[:, 0:n], in1=src.t[:, 0:n], op=ALU.add), reads=[src.b, acc.b], writes=[acc.b])
                            stt_["n"] += 1
                        attend(KT, 0, 128, QZ[m], tok0, n, kts, DIFF_SCALE, pv)
                        hi, lo = hl_r.next(), hl_r.next()
                        S.pool(lambda e: e.tensor_copy(out=hi.t[:, 0:n], in_=acc.t[:, 0:n]), reads=[acc.b], writes=[hi.b])
                        S.pool(lambda e: e.tensor_tensor(out=lo.t[:, 0:n], in0=acc.t[:, 0:n], in1=hi.t[:, 0:n], op=ALU.subtract), reads=[acc.b, hi.b], writes=[lo.b])
                        S.pe(lambda e: e.matmul(mb_.t[:, 0:n], lhsT=ph.ones_b.t[:], rhs=hi.t[:, 0:n], start=True, stop=False), reads=[ph.ones_b.b, hi.b], writes=[mb_.b])
                        S.pe(lambda e: e.matmul(mb_.t[:, 0:n], lhsT=ph.ones_b.t[:], rhs=lo.t[:, 0:n], start=False, stop=True), reads=[ph.ones_b.b, lo.b], writes=[mb_.b])
                        rc = rc_r.next()
                        o_ = (o1_r if m == 0 else o2_r).next()
                        S.act(lambda e: e.activation(out=rc.t[:, 0:n], in_=mb_.t[:, 0:n], func=AF.Ln), reads=[mb_.b], writes=[rc.b])
                        S.act(lambda e: e.activation(out=rc.t[:, 0:n], in_=rc.t[:, 0:n], func=AF.Exp, scale=-1.0), reads=[rc.b], writes=[rc.b])
                        S.dve(lambda e: e.tensor_tensor(out=o_.t[:, 0:n], in0=ob_.t[:, 0:n], in1=rc.t[:, 0:n], op=ALU.mult), reads=[ob_.b, rc.b], writes=[o_.b])
                        om.append(o_)
                    od, sq, rs, on = od_r.next(), sq_r.next(), rs_r.next(), on_r.next()
                    oa, obb = om[0], om[1]
                    S.dve(lambda e: e.scalar_tensor_tensor(out=od.t[:, 0:n], in0=obb.t[:, 0:n], scalar=lst.t[:, 5:6], in1=oa.t[:, 0:n], op0=ALU.mult, op1=ALU.add),
                           reads=[oa.b, obb.b, lst.b], writes=[od.b])
                    S.pool(lambda e: e.tensor_tensor(out=sq.t[:, 0:n], in0=od.t[:, 0:n], in1=od.t[:, 0:n], op=ALU.mult), reads=[od.b], writes=[sq.b])
                    S.pe(lambda e: e.matmul(ssb.t[:, 0:n], lhsT=ph.ones_b.t[:], rhs=sq.t[:, 0:n], start=True, stop=True), reads=[sq.b, ph.ones_b.b], writes=[ssb.b])
                    S.dve(lambda e: e.tensor_scalar(out=rs.t[:, 0:n], in0=ssb.t[:, 0:n], scalar1=1.0 / 128, scalar2=EPS, op0=ALU.mult, op1=ALU.add), reads=[ssb.b], writes=[rs.b])
                    S.act(lambda e: e.activation(out=rs.t[:, 0:n], in_=rs.t[:, 0:n], func=AF.Ln), reads=[rs.b], writes=[rs.b])
                    S.act(lambda e: e.activation(out=rs.t[:, 0:n], in_=rs.t[:, 0:n], func=AF.Exp, scale=-0.5), reads=[rs.b], writes=[rs.b])
                    S.dve(lambda e: e.scalar_tensor_tensor(out=on.t[:, 0:n], in0=od.t[:, 0:n], scalar=gsub.t[:, 0:1], in1=rs.t[:, 0:n], op0=ALU.mult, op1=ALU.mult),
                          reads=[od.b, gsub.b, rs.b], writes=[on.b])
                    S.dma(lambda e: e.dma_start(out=oT_d[:, 4 + h, tok0:tok0 + n], in_=on.t[:, 0:n]), reads=[on.b], writes=[ph.db(("oT", 8 + h))])
                    precast_step(on.b)
                    if (tok0 // 512) % 3 == 1:
                        precast_step(od.b)

            for h in range(8):
                mla_head(h)
            for h in range(4):
                diff_head(h)
            ph.close()

        if upto >= 3:
            ph = Phase(reorder=True)
            wo = ph.sb([128, 8, D], BF16, "wo")
            S.dma(lambda e: e.dma_start(out=wo.t[:], in_=wo_d.rearrange("(c p) n -> p c n", p=128)), writes=[wo.b], q="pool")
            sublayer_tail(S, ph, load_bc, NormCtx, norm_mod_T, rstd_from_ss, 0, tok_src, res_ap, hT_d, oT_d, wo, None, range(NT), None)
            ph.close()

        if upto >= 4:
            ph = Phase(reorder=True)
            groups = [(0, 256)] + [(256 + 1024 * i, 1024) for i in range(4)]
            ffn_phase(S, ph, load_bc, rstd_from_ss, 0, groups, res_ap, hT_d, [(fw1_d, fw3_d, fw2_d[0])], None)
            ph.close()

        if upto >= 5:
            ph = Phase(reorder=True)
            nx = NormCtx(ph)
            A0 = [load_bc(ph, 1, w, 0) for w in (0, 1)]
            SH0 = [load_bc(ph, 1, w, 1) for w in (0, 1)]
            wqkv = ph.sb([128, 8, 3 * D], BF16, "wqkv")
            wv = wqkv_d.rearrange("(k p) n -> p k n", p=128)
            for k in range(8):
                S.dma(lambda e, k=k: e.dma_start(out=wqkv.t[:, k, :], in_=wv[:, k, :]), writes=[wqkv.b], q="pool")
            bqk = ph.sb([128, 16], F32, "bqk")
            bvb = ph.sb([1, D], BF16, "bvb")
            S.dma(lambda e: e.dma_start(out=bqk.t[:], in_=bqk_d), writes=[bqk.b])
            S.dma(lambda e: e.dma_start(out=bvb.t[:], in_=bv_d), writes=[bvb.b], q="pool")
            xt_r = ph.rot(2, [128, D], F32, "xt5")
            hT_r = ph.rot(2, [128, 8, 512], BF16, "hT5")
            ob_r = ph.rot(4, [128, 512], BF16, "ob5")
            bk = Rot(ph.banks[0:7])
            tb = ph.banks[7]

            def p5_chunk(tok0, n, is_ctx):
                which = 1 if is_ctx else 0
                hT = hT_r.next()
                for j in range(n // 128):
                    t = tok0 // 128 + j
                    xt = xt_r.next()
                    S.dma(lambda e: e.dma_start(out=xt.t[:], in_=res_ap(t)), writes=[xt.b])
                    norm_mod_T(ph, nx, xt, A0[which], SH0[which], hT.t[:, :, j * 128:(j + 1) * 128], hT.b, tb)
                for blk in range(16):
                    pa = bk.next()
                    mm_acc(pa.t[:, 0:n], pa.b, [(wqkv.t[:, k, blk * 128:(blk + 1) * 128], hT.t[:, k, 0:n]) for k in range(8)], [wqkv.b, hT.b])
                    ob = ob_r.next()
                    S.act(lambda e: e.activation(out=ob.t[:, 0:n], in_=pa.t[:, 0:n], func=AF.Identity, bias=bqk.t[:, blk:blk + 1]), reads=[pa.b, bqk.b], writes=[ob.b])
                    dst = qC_d[blk] if blk < 8 else kC_d[blk - 8]
                    S.dma(lambda e: e.dma_start(out=dst[:, tok0:tok0 + n], in_=ob.t[:, 0:n]), reads=[ob.b], writes=[ph.db(("qk", blk))])
                for j in range(n // 128):
                    ts = slice(j * 128, (j + 1) * 128)
                    r0 = tok0 + j * 128
                    for half in range(2):
                        c0 = 2048 + half * 512
                        pa = bk.next()
                        mm_acc(pa.t[:, :], pa.b, [(hT.t[:, k, ts], wqkv.t[:, k, c0:c0 + 512]) for k in range(8)]
                               + [(ph.ones_b.t[0:1, 0:128], bvb.t[0:1, half * 512:(half + 1) * 512])], [wqkv.b, hT.b, bvb.b, ph.ones_b.b])
                        ob = ob_r.next()
                        S.act(lambda e: e.activation(out=ob.t[:, :], in_=pa.t[:, :], func=AF.Copy), reads=[pa.b], writes=[ob.b])
                        S.dma(lambda e: e.dma_start(out=vC_d[r0:r0 + 128, half * 512:(half + 1) * 512], in_=ob.t[:, :]), reads=[ob.b], writes=[ph.db("vC")])
            for (tok0, n, is_ctx) in CHUNKS:
                p5_chunk(tok0, n, is_ctx)
            ph.close()

        if upto >= 6:
            ph = Phase(reorder=True)
            KT_r = ph.rot(2, [128, NTOK], BF16, "KT6")
            VA_r = ph.rot(2, [128, NT, 2, 128], BF16, "VA6")
            pc6_r = ph.rot(2, [128, 4, 2048], BF16, "pc6")
            EB_r = ph.rot(2, [128, 2, NCLS, 640], BF16, "EB")
            rp_r = ph.rot(2, [128, ND, 128], F32, "rp")
            eb_t = ph.rot(3, [128, 128], F32, "ebt")
            nm = ph.sb([128, NVAR, 128], F32, "nm")
            PT_r = ph.rot(3, [128, 1024], BF16, "PT6")
            rc_r = ph.rot(2, [128, 128], F32, "rc6")
            on_r = ph.rot(3, [128, 128], BF16, "on6")
            QZ_r = [ph.rot(2, [128, NTOK], BF16, "QZa"), ph.rot(2, [128, NTOK], BF16, "QZb")]
            for m_ in range(2):
                for qz in QZ_r[m_].items:
                    zl = 64 * (1 - m_)
                    S.pool(lambda e, qz=qz, zl=zl: e.memset(qz.t[zl:zl + 64, :], 0.0), writes=[qz.b])
            Sp = Rot(ph.pairs[0:3])
            Ob = Rot(ph.banks[6:8])
            S.dma(lambda e: e.dma_start(out=nm.t[:], in_=nmask_d.rearrange("v k q -> k v q")), writes=[nm.b])
            for va in VA_r.items:
                S.pool(lambda e, va=va: e.memset(va.t[:, :, 0, 64:128], 1.0), writes=[va.b])
                S.pool(lambda e, va=va: e.memset(va.t[:, :, 1, 0:64], 1.0), writes=[va.b])

            def na_pair(hp):
                KT, VA, EB = KT_r.next(), VA_r.next(), EB_r.next()
                QZ = [QZ_r[0].next(), QZ_r[1].next()]
                S.dma(lambda e: e.dma_start(out=KT.t[:, :], in_=kC_d[hp, :, :]), writes=[KT.b])
                S.dma(lambda e: e.dma_start(out=QZ[0].t[0:64, :], in_=qC_d[hp, 0:64, :]), writes=[QZ[0].b])
                S.dma(lambda e: e.dma_start(out=QZ[1].t[64:128, :], in_=qC_d[hp, 64:128, :]), writes=[QZ[1].b])
                for m in range(2):
                    hh = 2 * hp + m
                    c0 = 64 * m
                    for (t0, t1) in ((0, 17), (17, 34)):
                        S.dma(lambda e, m=m, t0=t0, t1=t1, hh=hh, c0=c0: e.dma_start(out=VA.t[:, t0:t1, m, c0:c0 + 64],
                                                                                     in_=vC_d[t0 * 128:t1 * 128, hh * 64:(hh + 1) * 64].rearrange("(t p) d -> p t d", p=128)), writes=[VA.b])
                    rp = rp_r.next()
                    S.dma(lambda e, rp=rp, hh=hh: e.dma_start(out=rp.t[:], in_=rpbg_d[hh].rearrange("d k q -> k d q")), writes=[rp.b])
                    for ci_, cl in enumerate(NA_CLASSES):
                        for j, cmb in enumerate(cl):
                            var, di = NA_COMBOS[cmb]
                            tt = eb_t.next()
                            S.dve(lambda e, tt=tt, var=var, di=di, rp=rp: e.tensor_tensor(out=tt.t[:], in0=rp.t[:, di, :], in1=nm.t[:, var, :], op=ALU.add), reads=[rp.b, nm.b], writes=[tt.b])
                            S.act(lambda e, tt=tt, m=m, ci_=ci_, j=j: e.activation(out=EB.t[:, m, ci_, j * 128:(j + 1) * 128], in_=tt.t[:], func=AF.Exp), reads=[tt.b], writes=[EB.b])
                na_tiles(hp, KT, QZ, VA, EB)

            def na_stage_a(hp, i, m, KT, QZ, EB):
                q0 = (2 + i) * 128
                wk = [kt for kt in range(32) if (i, kt) in NA_CTAB]
                sp = Sp.next()
                slots = []
                tiles_ = [2 + kt for kt in wk] + [0, 1]
                for j, tile in enumerate(tiles_):
                    sl_ = slice(j * 128, (j + 1) * 128)
                    S.pe(lambda e, tile=tile, sl_=sl_: e.matmul(sp.t[:, sl_], lhsT=KT.t[:, tile * 128:(tile + 1) * 128], rhs=QZ[m].t[:, q0:q0 + 128], start=True, stop=True),
                         reads=[KT.b, QZ[m].b], writes=[sp.b], cost=0.06)
                    slots.append((sl_, tile))
                ns = len(slots)
                nw = len(wk)
                pt = PT_r.next()
                S.act(lambda e: e.activation(out=pt.t[:, 0:512], in_=sp.t[:, 0:512], func=AF.Exp, scale=NA_SCALE), reads=[sp.b], writes=[pt.b], cost=0.5)
                if ns > 4:
                    S.act(lambda e: e.activation(out=pt.t[:, 512:ns * 128], in_=sp.t[:, 512:ns * 128], func=AF.Exp, scale=NA_SCALE), reads=[sp.b], writes=[pt.b], cost=0.45)
                cls = NA_CLS_OF_I[i]
                S.dve(lambda e: e.tensor_tensor(out=pt.t[:, 0:nw * 128], in0=pt.t[:, 0:nw * 128], in1=EB.t[:, m, cls, 0:nw * 128], op=ALU.mult), reads=[pt.b, EB.b], writes=[pt.b], cost=0.45)
                return (pt, slots)

            def na_stage_b(hp, i, m, VA, st, on):
                pt, slots = st
                ns = len(slots)
                q0 = (2 + i) * 128
                ob_ = Ob.next()
                for j, (sl_, tile) in enumerate(slots):
                    S.pe(lambda e, j=j, sl_=sl_, tile=tile: e.matmul(ob_.t[:, 0:128], lhsT=VA.t[:, tile, m, :], rhs=pt.t[:, sl_], start=(j == 0), stop=(j == ns - 1)),
                         reads=[VA.b, pt.b], writes=[ob_.b], cost=0.06)
                rc = rc_r.next()
                olo, slo = (64, 0) if m else (0, 64)
                if m == 0:
                    S.dve(lambda e: e.reciprocal(out=rc.t[olo:olo + 64, :], in_=ob_.t[slo:slo + 64, 0:128]), reads=[ob_.b], writes=[rc.b], cost=0.9)
                else:
                    S.act(lambda e: e.activation(out=rc.t[olo:olo + 64, :], in_=ob_.t[slo:slo + 64, 0:128], func=AF.Ln), reads=[ob_.b], writes=[rc.b], cost=0.27)
                    S.act(lambda e: e.activation(out=rc.t[olo:olo + 64, :], in_=rc.t[olo:olo + 64, :], func=AF.Exp, scale=-1.0), reads=[rc.b], writes=[rc.b], cost=0.27)
                S.dve(lambda e: e.tensor_tensor(out=on.t[olo:olo + 64, :], in0=ob_.t[olo:olo + 64, 0:128], in1=rc.t[olo:olo + 64, :], op=ALU.mult), reads=[ob_.b, rc.b], writes=[on.b], cost=0.3)
                if m == 1:
                    S.dma(lambda e: e.dma_start(out=oT_d[:, hp, q0:q0 + 128], in_=on.t[:, :]), reads=[on.b], writes=[ph.db(("oT", hp))])
                    if i % 8 == 7:
                        precast_job(ph, pc6_r, on.b)

            def na_tiles(hp, KT, QZ, VA, EB):
                work = [(i, m) for i in range(32) for m in range(2)]
                ons = {}
                st = na_stage_a(hp, 0, 0, KT, QZ, EB)
                for idx, (i, m) in enumerate(work):
                    nxt = None
                    if idx + 1 < len(work):
                        ni, nm_ = work[idx + 1]
                        nxt = na_stage_a(hp, ni, nm_, KT, QZ, EB)
                    if m == 0:
                        ons[i] = on_r.next()
                    na_stage_b(hp, i, m, VA, st, ons[i])
                    st = nxt

            for hp in range(8):
                na_pair(hp)
            assert not pc_jobs, len(pc_jobs)
            ph.close()

        if upto >= 7:
            ph = Phase(reorder=True)
            wo = ph.sb([128, 8, D], BF16, "wo7")
            S.dma(lambda e: e.dma_start(out=wo.t[:], in_=cwo_d.rearrange("(c p) n -> p c n", p=128)), writes=[wo.b], q="pool")
            cbo = ph.sb([1, D], BF16, "cbo")
            S.dma(lambda e: e.dma_start(out=cbo.t[:], in_=cbo_d), writes=[cbo.b], q="pool")
            rt = ph.sb([128, 8, NEXP], BF16, "rt")
            S.dma(lambda e: e.dma_start(out=rt.t[:], in_=rt_d.rearrange("(k p) n -> p k n", p=128)), writes=[rt.b], q="pool")
            lg_r = ph.rot(2, [128, 8], F32, "lg")
            m8_r = ph.rot(2, [128, 8], F32, "m8")
            w8_r = ph.rot(2, [128, 32], F32, "w8")
            lgb = ph.banks[6]

            def router(t, h2):
                lg, m8, w8 = lg_r.next(), m8_r.next(), w8_r.next()
                for k in range(8):
                    S.pe(lambda e, k=k: e.matmul(lgb.t[:, 0:NEXP], lhsT=h2.t[:, k, :], rhs=rt.t[:, k, :], start=(k == 0), stop=(k == 7)), reads=[h2.b, rt.b], writes=[lgb.b])
                S.dve(lambda e: e.tensor_copy(out=lg.t[:], in_=lgb.t[:, 0:NEXP]), reads=[lgb.b], writes=[lg.b])
                S.dve(lambda e: e.max(out=m8.t[:], in_=lg.t[:]), reads=[lg.b], writes=[m8.b])
                S.dve(lambda e: e.tensor_scalar(out=w8.t[:, 0:8], in0=lg.t[:], scalar1=m8.t[:, 1:2], scalar2=None, op0=ALU.is_ge), reads=[lg.b, m8.b], writes=[w8.b])
                S.dve(lambda e: e.tensor_scalar(out=w8.t[:, 24:25], in0=m8.t[:, 0:1], scalar1=-1.0, scalar2=None, op0=ALU.mult), reads=[m8.b], writes=[w8.b])
                S.act(lambda e: e.activation(out=w8.t[:, 8:16], in_=lg.t[:], func=AF.Exp, bias=w8.t[:, 24:25]), reads=[lg.b, w8.b], writes=[w8.b])
                S.dve(lambda e: e.tensor_tensor(out=w8.t[:, 16:24], in0=w8.t[:, 8:16], in1=w8.t[:, 0:8], op=ALU.mult), reads=[w8.b], writes=[w8.b])
                S.dve(lambda e: e.tensor_reduce(out=w8.t[:, 25:26], in_=w8.t[:, 16:24], axis=mybir.AxisListType.X, op=ALU.add), reads=[w8.b], writes=[w8.b])
                S.dve(lambda e: e.reciprocal(out=w8.t[:, 26:27], in_=w8.t[:, 25:26]), reads=[w8.b], writes=[w8.b])
                S.dve(lambda e: e.tensor_scalar(out=lg.t[:], in0=w8.t[:, 16:24], scalar1=w8.t[:, 26:27], scalar2=None, op0=ALU.mult), reads=[w8.b, lg.b], writes=[lg.b])
                S.dma(lambda e: e.dma_start(out=comb_d[:, t - 2, :], in_=lg.t[:]), reads=[lg.b], writes=[ph.db(("comb", t))])

            sublayer_tail(S, ph, load_bc, NormCtx, norm_mod_T, rstd_from_ss, 1, res_ap, res_ap, hT_d, oT_d, wo, cbo, range(2, NT), router, h4_d=h4_d)
            ph.close()

        if upto >= 8:
            ph = Phase(reorder=True)
            G3 = load_bc(ph, 1, 0, 5)
            rc_ = ph.sb([128, 161], F32, "rconst")
            S.dma(lambda e: e.dma_start(out=rc_.t[:], in_=rconst_d), writes=[rc_.b])
            tri_b = ph.sb([128, 128], BF16, "tri")
            S.dma(lambda e: e.dma_start(out=tri_b.t[:], in_=rconst_d[:, 0:128]), writes=[tri_b.b], q="pool")
            thr = rc_.t[:, 128:136]
            svals = rc_.t[:, 136:136 + NSLOT]
            pcol = rc_.t[:, 160:161]
            comb = ph.sb([128, 32, NEXP], F32, "comb_sb")
            S.dma(lambda e: e.dma_start(out=comb.t[:], in_=comb_d), writes=[comb.b])
            sel = ph.sb([128, 32, NEXP], F32, "sel")
            selb = ph.sb([128, 256], BF16, "selb")
            tot = ph.sb([128, 32, NEXP], F32, "tot")
            tp = ph.sb([128, 32, NEXP], F32, "tp")
            pos = ph.sb([128, 32, NEXP], F32, "pos")
            Mt = ph.sb([128, 32, NEXP], F32, "Mt")
            eq = ph.sb([128, 32, NEXP], F32, "eq")
            sm8 = ph.sb([128, 64], F32, "sm8")
            cmp8 = ph.sb([128, 8, 8], F32, "cmp8")
            r32 = ph.sb([128, 8, 32], F32, "r32")
            pA_u = ph.sb([128, 32], U32, "pA_u")
            pB_u = ph.sb([128, 32], U32, "pB_u")
            es24 = ph.sb([128, 3, NSLOT], F32, "es24")
            idxf = ph.sb([128, NSLOT, 11], F32, "idxf")
            idxw = ph.sb([128, NSLOT, 11], U32, "idxw")
            cumb, totb = ph.banks[0], ph.banks[1]
            S.dve(lambda e: e.tensor_single_scalar(out=sel.t[:], in_=comb.t[:], scalar=0.0, op=ALU.is_gt), reads=[comb.b], writes=[sel.b])
            S.dve(lambda e: e.tensor_copy(out=selb.t[:], in_=sel.t[:].rearrange("p t e -> p (t e)")), reads=[sel.b], writes=[selb.b])
            S.pe(lambda e: e.matmul(cumb.t[:, 0:256], lhsT=tri_b.t[:], rhs=selb.t[:], start=True, stop=True), reads=[tri_b.b, selb.b], writes=[cumb.b])
            S.pe(lambda e: e.matmul(totb.t[:, 0:256], lhsT=ph.ones_b.t[:], rhs=selb.t[:], start=True, stop=True), reads=[ph.ones_b.b, selb.b], writes=[totb.b])
            S.dve(lambda e: e.tensor_copy(out=tot.t[:].rearrange("p t e -> p (t e)"), in_=totb.t[:, 0:256]), reads=[totb.b], writes=[tot.b])
            S.dve(lambda e: e.memset(tp.t[:, 0, :], 0.0), writes=[tp.b])
            for t in range(1, 32):
                S.dve(lambda e, t=t: e.tensor_tensor(out=tp.t[:, t, :], in0=tp.t[:, t - 1, :], in1=tot.t[:, t - 1, :], op=ALU.add), reads=[tp.b, tot.b], writes=[tp.b])
            S.dve(lambda e: e.tensor_tensor(out=sm8.t[:, 0:8], in0=tp.t[:, 31, :], in1=tot.t[:, 31, :], op=ALU.add), reads=[tp.b, tot.b], writes=[sm8.b])
            for ex in range(8):
                S.dve(lambda e, ex=ex: e.tensor_scalar(out=cmp8.t[:, ex, :], in0=thr, scalar1=sm8.t[:, ex:ex + 1], scalar2=None, op0=ALU.is_lt), reads=[rc_.b, sm8.b], writes=[cmp8.b])
            S.dve(lambda e: e.tensor_reduce(out=sm8.t[:, 8:16], in_=cmp8.t[:], axis=mybir.AxisListType.X, op=ALU.add), reads=[cmp8.b], writes=[sm8.b])
            S.dve(lambda e: e.memset(sm8.t[:, 16:17], 0.0), writes=[sm8.b])
            for ex in range(1, 8):
                S.dve(lambda e, ex=ex: e.tensor_tensor(out=sm8.t[:, 16 + ex:17 + ex], in0=sm8.t[:, 15 + ex:16 + ex], in1=sm8.t[:, 7 + ex:8 + ex], op=ALU.add), reads=[sm8.b], writes=[sm8.b])
            S.dve(lambda e: e.tensor_scalar(out=sm8.t[:, 24:32], in0=sm8.t[:, 16:24], scalar1=512.0, scalar2=-1.0, op0=ALU.mult, op1=ALU.add), reads=[sm8.b], writes=[sm8.b])
            S.dve(lambda e: e.tensor_tensor(out=sm8.t[:, 32:40], in0=sm8.t[:, 16:24], in1=sm8.t[:, 8:16], op=ALU.add), reads=[sm8.b], writes=[sm8.b])
            cumv = cumb.t[:, 0:256].rearrange("p (t e) -> p t e", e=NEXP)
            for ex in range(8):
                S.dve(lambda e, ex=ex: e.scalar_tensor_tensor(out=pos.t[:, :, ex], in0=cumv[:, :, ex], scalar=sm8.t[:, 24 + ex:25 + ex], in1=tp.t[:, :, ex], op0=ALU.add, op1=ALU.add),
                      reads=[cumb.b, sm8.b, tp.b], writes=[pos.b])
            S.dve(lambda e: e.scalar_tensor_tensor(out=Mt.t[:], in0=pos.t[:], scalar=1.0, in1=sel.t[:], op0=ALU.add, op1=ALU.mult), reads=[pos.b, sel.b], writes=[Mt.b])
            S.dve(lambda e: e.tensor_reduce(out=r32.t[:, 0, :], in_=Mt.t[:], axis=mybir.AxisListType.X, op=ALU.max), reads=[Mt.b], writes=[r32.b])
            S.dve(lambda e: e.tensor_reduce(out=r32.t[:, 1, :], in_=Mt.t[:], axis=mybir.AxisListType.X, op=ALU.add), reads=[Mt.b], writes=[r32.b])
            S.dve(lambda e: e.tensor_scalar(out=r32.t[:, 3, :], in0=r32.t[:, 0, :], scalar1=-1.0, scalar2=None, op0=ALU.add), reads=[r32.b], writes=[r32.b])
            S.dve(lambda e: e.scalar_tensor_tensor(out=r32.t[:, 2, :], in0=r32.t[:, 1, :], scalar=-1.0, in1=r32.t[:, 0, :], op0=ALU.add, op1=ALU.subtract), reads=[r32.b], writes=[r32.b])
            S.dve(lambda e: e.tensor_copy(out=pA_u.t[:], in_=r32.t[:, 2, :]), reads=[r32.b], writes=[pA_u.b])
            S.dve(lambda e: e.tensor_copy(out=pB_u.t[:], in_=r32.t[:, 3, :]), reads=[r32.b], writes=[pB_u.b])
            for ex in range(8):
                S.dve(lambda e, ex=ex: e.tensor_tensor(out=eq.t[:, :, ex], in0=Mt.t[:, :, ex], in1=r32.t[:, 0, :], op=ALU.is_equal), reads=[Mt.b, r32.b], writes=[eq.b])
            S.dve(lambda e: e.tensor_tensor(out=eq.t[:], in0=eq.t[:], in1=comb.t[:], op=ALU.mult), reads=[eq.b, comb.b], writes=[eq.b])
            S.dve(lambda e: e.tensor_reduce(out=r32.t[:, 4, :], in_=eq.t[:], axis=mybir.AxisListType.X, op=ALU.add), reads=[eq.b], writes=[r32.b])
            S.dve(lambda e: e.tensor_reduce(out=r32.t[:, 5, :], in_=comb.t[:], axis=mybir.AxisListType.X, op=ALU.add), reads=[comb.b], writes=[r32.b])
            S.dve(lambda e: e.tensor_tensor(out=r32.t[:, 6, :], in0=r32.t[:, 5, :], in1=r32.t[:, 4, :], op=ALU.subtract), reads=[r32.b], writes=[r32.b])
            S.dve(lambda e: e.memset(es24.t[:, 0, :], 0.0), writes=[es24.b])
            for ex in range(8):
                S.dve(lambda e, ex=ex: e.scalar_tensor_tensor(out=es24.t[:, 0, :], in0=svals, scalar=sm8.t[:, 32 + ex:33 + ex], in1=es24.t[:, 0, :], op0=ALU.is_ge, op1=ALU.add),
                      reads=[rc_.b, sm8.b, es24.b], writes=[es24.b])
            S.dve(lambda e: e.tensor_scalar(out=es24.t[:, 1, :], in0=es24.t[:, 0, :], scalar1=7.0, scalar2=None, op0=ALU.min), reads=[es24.b], writes=[es24.b])
            S.dve(lambda e: e.tensor_scalar(out=es24.t[:, 2, :], in0=es24.t[:, 1, :], scalar1=1408.0, scalar2=pcol, op0=ALU.mult, op1=ALU.add), reads=[es24.b, rc_.b], writes=[es24.b])
            for jp in range(11):
                S.dve(lambda e, jp=jp: e.tensor_scalar(out=idxf.t[:, :, jp], in0=es24.t[:, 2, :], scalar1=float(jp * 128), scalar2=None, op0=ALU.add), reads=[es24.b], writes=[idxf.b])
            S.dve(lambda e: e.tensor_copy(out=idxw.t[:], in_=idxf.t[:]), reads=[idxf.b], writes=[idxw.b])

            zt = ph.sb([128, 4, D], BF16, "zt")
            S.pool(lambda e: e.memset(zt.t[:], 0.0), writes=[zt.b])
            zb = []
            for sl in range(NSLOT):
                bz = S.buf("z")
                zb.append(bz)
                S.dma(lambda e, sl=sl: e.dma_start(out=Hs_d[sl * 512:(sl + 1) * 512, :].rearrange("(a p) d -> p a d", p=128), in_=zt.t[:]), reads=[zt.b], writes=[bz])
            h4_r = ph.rot(3, [128, D], BF16, "h4t")
            scb = []
            for t in range(32):
                h4t = h4_r.next()
                S.dma(lambda e, t=t, h4t=h4t: e.dma_start(out=h4t.t[:], in_=h4_d[t * 128:(t + 1) * 128, :]), writes=[h4t.b])
                for pu in (pA_u, pB_u):
                    bs = S.buf("sc")
                    scb.append(bs)
                    S.dma(lambda e, t=t, h4t=h4t, pu=pu: e.indirect_dma_start(out=Hs_d[:, :], out_offset=bass.IndirectOffsetOnAxis(ap=pu.t[:, t:t + 1], axis=0), in_=h4t.t[:, :], in_offset=None),
                          reads=[h4t.b, pu.b] + zb, writes=[bs], q="pool")

            hs_r = ph.rot(1, [128, 4, D], BF16, "hs")
            hT_r = ph.rot(2, [128, 8, 512], BF16, "hTs")
            w1_r = ph.rot(3, [128, 2, 8, 128], BF16, "w1p")
            w3_r = ph.rot(3, [128, 2, 8, 128], BF16, "w3p")
            W2 = ph.sb([128, NJ, D], BF16, "W2s")
            W2b = [S.buf(f"W2_{jp}") for jp in range(11)]
            act = ph.sb([128, NJ, 512], BF16, "acts")
            sg_r = ph.rot(3, [128, 512], BF16, "sgs")
            ys_r = ph.rot(2, [128, D], F32, "ys")
            ub = Rot(ph.banks[0:4])
            yp_r = Rot(ph.pairs[2:4])
            rb = []

            def gather_w(wi, s_, jp, out_ap, out_b, extra_reads=()):
                S.dma(lambda e: e.indirect_dma_start(out=out_ap, out_offset=None, in_=wb_d[wi][:, :], in_offset=bass.IndirectOffsetOnAxis(ap=idxw.t[:, s_, jp:jp + 1], axis=0)),
                      reads=[idxw.b] + list(extra_reads), writes=[out_b], q="pool", cost=3.0, lat=6.0)

            def slot(s_):
                hs, hT = hs_r.next(), hT_r.next()
                S.dma(lambda e: e.dma_start(out=hs.t[:], in_=Hs_d[s_ * 512:(s_ + 1) * 512, :].rearrange("(a p) d -> p a d", p=128)), reads=scb, writes=[hs.b])
                for a_ in range(4):
                    tbk = ub.next()
                    pv = tbk.t.bitcast(BF16)
                    for k in range(8):
                        S.pe(lambda e, k=k, a_=a_: e.transpose(out=pv[:, k * 128:(k + 1) * 128], in_=hs.t[:, a_, k * 128:(k + 1) * 128], identity=ph.ident_b.t[:]),
                             reads=[hs.b, ph.ident_b.b], writes=[tbk.b])
                    S.dve(lambda e, a_=a_: e.tensor_copy(out=hT.t[:, :, a_ * 128:(a_ + 1) * 128], in_=pv.rearrange("p (k t) -> p k t", k=8)), reads=[tbk.b], writes=[hT.b])
                for jp in range(11):
                    w1, w3 = w1_r.next(), w3_r.next()
                    gather_w(0, s_, jp, w1.t[:].rearrange("p a k n -> p (a k n)"), w1.b)
                    gather_w(1, s_, jp, w3.t[:].rearrange("p a k n -> p (a k n)"), w3.b)
                    for jj in range(2):
                        j = 2 * jp + jj
                        u1, u3 = ub.next(), ub.next()
                        for k in range(8):
                            S.pe(lambda e, k=k, jj=jj: e.matmul(u1.t[:, :], lhsT=w1.t[:, jj, k, :], rhs=hT.t[:, k, :], start=(k == 0), stop=(k == 7)), reads=[w1.b, hT.b], writes=[u1.b])
                        for k in range(8):
                            S.pe(lambda e, k=k, jj=jj: e.matmul(u3.t[:, :], lhsT=w3.t[:, jj, k, :], rhs=hT.t[:, k, :], start=(k == 0), stop=(k == 7)), reads=[w3.b, hT.b], writes=[u3.b])
                        sg = sg_r.next()
                        S.act(lambda e: e.activation(out=sg.t[:], in_=u1.t[:], func=AF.Silu), reads=[u1.b], writes=[sg.b])
                        S.dve(lambda e, j=j: e.tensor_tensor(out=act.t[:, j, :], in0=u3.t[:], in1=sg.t[:], op=ALU.mult), reads=[sg.b, u3.b], writes=[act.b])
                    gather_w(2, s_, jp, W2.t[:, 2 * jp:2 * jp + 2, :].rearrange("p a n -> p (a n)"), W2b[jp])
                for a_ in range(4):
                    yp = yp_r.next()
                    for half in range(2):
                        for j in range(NJ):
                            S.pe(lambda e, j=j, a_=a_, half=half: e.matmul(yp.t[:, half * 512:(half + 1) * 512], lhsT=act.t[:, j, a_ * 128:(a_ + 1) * 128],
                                                                           rhs=W2.t[:, j, half * 512:(half + 1) * 512], start=(j == 0), stop=(j == NJ - 1)),
                                 reads=[act.b, W2b[j // 2]], writes=[yp.b])
                    ys = ys_r.next()
                    S.dve(lambda e: e.tensor_copy(out=ys.t[:, 0:512], in_=yp.t[:, 0:512]), reads=[yp.b], writes=[ys.b])
                    S.act(lambda e: e.activation(out=ys.t[:, 512:1024], in_=yp.t[:, 512:1024], func=AF.Copy), reads=[yp.b], writes=[ys.b])
                    br = S.buf("R")
                    rb.append(br)
                    r0 = s_ * 512 + a_ * 128
                    S.dma(lambda e: e.dma_start(out=R_d[r0:r0 + 128, :], in_=ys.t[:]), reads=[ys.b], writes=[br])
            for s_ in range(NSLOT):
                slot(s_)

            ra_r = ph.rot(2, [128, D], F32, "ra")
            rbb_r = ph.rot(2, [128, D], F32, "rbb")
            xt_r = ph.rot(2, [128, D], F32, "xt9")
            jk_r = ph.rot(1, [128, D], F32, "jk9")
            st_r = ph.rot(3, [128, 8], F32, "st9")

            def combine(t):
                ra, rbt, xt, jk, st = ra_r.next(), rbb_r.next(), xt_r.next(), jk_r.next(), st_r.next()
                S.dma(lambda e: e.indirect_dma_start(out=ra.t[:, :], out_offset=None, in_=R_d[:, :], in_offset=bass.IndirectOffsetOnAxis(ap=pA_u.t[:, t:t + 1], axis=0)),
                      reads=[pA_u.b] + rb, writes=[ra.b], q="pool")
                S.dma(lambda e: e.indirect_dma_start(out=rbt.t[:, :], out_offset=None, in_=R_d[:, :], in_offset=bass.IndirectOffsetOnAxis(ap=pB_u.t[:, t:t + 1], axis=0)),
                      reads=[pB_u.b] + rb, writes=[rbt.b], q="pool")
                S.dma(lambda e: e.dma_start(out=xt.t[:], in_=res_ap(t + 2)), writes=[xt.b])
                S.dve(lambda e: e.tensor_scalar(out=ra.t[:], in0=ra.t[:], scalar1=r32.t[:, 6, t:t + 1], scalar2=None, op0=ALU.mult), reads=[ra.b, r32.b], writes=[ra.b])
                S.dve(lambda e: e.scalar_tensor_tensor(out=ra.t[:], in0=rbt.t[:], scalar=r32.t[:, 4, t:t + 1], in1=ra.t[:], op0=ALU.mult, op1=ALU.add), reads=[ra.b, rbt.b, r32.b], writes=[ra.b])
                S.dve(lambda e: e.memset(st.t[:], 0.0), writes=[st.b])
                S.act(lambda e: e.activation(out=jk.t[:], in_=ra.t[:], func=AF.Square, accum_out=st.t[:, 0:1]), reads=[ra.b], writes=[jk.b, st.b])
                rstd_from_ss(st, 1, D)
                S.dve(lambda e: e.scalar_tensor_tensor(out=jk.t[:], in0=ra.t[:], scalar=st.t[:, 7:8], in1=G3.t[:], op0=ALU.mult, op1=ALU.mult), reads=[ra.b, st.b, G3.b], writes=[jk.b])
                S.pool(lambda e: e.tensor_tensor(out=xt.t[:], in0=jk.t[:], in1=xt.t[:], op=ALU.add), reads=[jk.b, xt.b], writes=[xt.b])
                S.dma(lambda e: e.dma_start(out=res_ap(t + 2), in_=xt.t[:]), reads=[xt.b], writes=[ph.db(("res", t + 2))])
            for t in range(32):
                combine(t)
            ph.close()

        print("total ops", S.total)
    return nc


def sublayer_tail(S, ph, load_bc, NormCtx, norm_mod_T, rstd_from_ss, layer, src_fn, res_ap, hT_d, oT_d, wo, bias, tiles, post_fn, h4_d=None):
    nx = NormCtx(ph)
    G1 = [load_bc(ph, layer, w, 2) for w in (0, 1)]
    A2 = [load_bc(ph, layer, w, 3) for w in (0, 1)]
    SH2 = [load_bc(ph, layer, w, 4) for w in (0, 1)]
    ot_r = ph.rot(3, [128, 8, 128], BF16, "ot")
    xt_r = ph.rot(2, [128, D], F32, "xt3")
    x1_r = ph.rot(2, [128, D], F32, "x1")
    tm_r = ph.rot(2, [128, D], F32, "tm3")
    jk_r = ph.rot(2, [128, 512], F32, "jk3")
    st_r = ph.rot(3, [128, 8], F32, "st3")
    h2_r = ph.rot(3, [128, 8, 128], BF16, "h2")
    pr = Rot(ph.pairs[0:2])
    tbank = ph.banks[7]

    def one(t):
        which = 1 if t < 2 else 0
        yp, ot = pr.next(), ot_r.next()
        S.dma(lambda e: e.dma_start(out=ot.t[:], in_=oT_d[:, :, t * 128:(t + 1) * 128]), writes=[ot.b])
        for half in range(2):
            hs = slice(half * 512, (half + 1) * 512)
            prs = [(ot.t[:, c, :], wo.t[:, c, hs]) for c in range(8)]
            rd = [ot.b, wo.b]
            if bias is not None:
                prs.append((ph.ones_b.t[0:1, 0:128], bias.t[0:1, hs]))
                rd = rd + [bias.b, ph.ones_b.b]
            n = len(prs)
            for i, (l, r) in enumerate(prs):
                S.pe(lambda e, l=l, r=r, i=i: e.matmul(yp.t[:, hs], lhsT=l, rhs=r, start=(i == 0), stop=(i == n - 1)), reads=rd, writes=[yp.b])
        xt = xt_r.next()
        S.dma(lambda e: e.dma_start(out=xt.t[:], in_=src_fn(t)), reads=[ph.db(("res", t))], writes=[xt.b])
        st, tm, x1 = st_r.next(), tm_r.next(), x1_r.next()
        S.dve(lambda e: e.memset(st.t[:], 0.0), writes=[st.b])
        for half in range(2):
            jk = jk_r.next()
            S.act(lambda e, jk=jk, half=half: e.activation(out=jk.t[:], in_=yp.t[:, half * 512:(half + 1) * 512], func=AF.Square, accum_out=st.t[:, half:half + 1]),
                  reads=[yp.b], writes=[jk.b, st.b])
        rstd_from_ss(st, 2, D)
        for half in range(2):
            hs = slice(half * 512, (half + 1) * 512)
            S.dve(lambda e, hs=hs: e.scalar_tensor_tensor(out=tm.t[:, hs], in0=yp.t[:, hs], scalar=st.t[:, 7:8], in1=G1[which].t[:, hs], op0=ALU.mult, op1=ALU.mult),
                  reads=[yp.b, st.b, G1[which].b], writes=[tm.b])
        S.pool(lambda e: e.tensor_tensor(out=x1.t[:], in0=tm.t[:], in1=xt.t[:], op=ALU.add), reads=[tm.b, xt.b], writes=[x1.b])
        S.dma(lambda e: e.dma_start(out=res_ap(t), in_=x1.t[:]), reads=[x1.b], writes=[ph.db(("res", t))])
        h2 = h2_r.next()
        hb = norm_mod_T(ph, nx, x1, A2[which], SH2[which], h2.t[:], h2.b, tbank)
        if h4_d is None:
            S.dma(lambda e: e.dma_start(out=hT_d[:, :, t * 128:(t + 1) * 128], in_=h2.t[:]), reads=[h2.b], writes=[ph.db(("hT", t))])
        else:
            S.dma(lambda e: e.dma_start(out=h4_d[(t - 2) * 128:(t - 1) * 128, :], in_=hb.t[:]), reads=[hb.b], writes=[ph.db(("h4", t))])
        if post_fn is not None:
            post_fn(t, h2)
    for t in tiles:
        one(t)


def ffn_phase(S, ph, load_bc, rstd_from_ss, layer, groups, res_ap, hT_d, experts, comb):
    G3 = [load_bc(ph, layer, w, 5) for w in (0, 1)]
    hT = ph.sb([128, 8, 1024], BF16, "hTg")
    act = ph.sb([128, NJ, 1024], BF16, "act")
    W2 = ph.sb([128, NJ, D], BF16, "W2")
    w1_r = ph.rot(4, [128, 8, 128], BF16, "w1c")
    w3_r = ph.rot(4, [128, 8, 128], BF16, "w3c")
    sg_r = ph.rot(3, [128, 512], BF16, "sg")
    xt_r = ph.rot(2, [128, D], F32, "xt4")
    tm_r = ph.rot(2, [128, D], F32, "tm4")
    jk_r = ph.rot(2, [128, 512], F32, "jk4")
    st_r = ph.rot(3, [128, 8], F32, "st4")
    ne = len(experts)
    yacc = ph.sb([128, 8, D], F32, "yacc") if ne > 1 else None
    ub = Rot(ph.banks[0:4])
    yp_r = Rot(ph.pairs[2:4])
    for (tok0, n) in groups:
        S.dma(lambda e, tok0=tok0, n=n: e.dma_start(out=hT.t[:, :, 0:n], in_=hT_d[:, :, tok0:tok0 + n]), writes=[hT.b])
        nsub = [(s0, min(512, n - s0)) for s0 in range(0, n, 512)]
        for ei, (w1_d, w3_d, w2_d) in enumerate(experts):
            w1v, w3v = w1_d, w3_d
            w2v = w2_d.rearrange("(j p) n -> p j n", p=128)
            for j0 in range(0, NJ, 2):
                S.dma(lambda e, j0=j0, w2v=w2v: e.dma_start(out=W2.t[:, j0:j0 + 2, :], in_=w2v[:, j0:j0 + 2, :]), writes=[W2.b], q="pool")
            for j in range(NJ):
                w1, w3 = w1_r.next(), w3_r.next()
                S.dma(lambda e, w1=w1, j=j, w1v=w1v: e.dma_start(out=w1.t[:].rearrange("p k n -> p (k n)"), in_=w1v[j]), writes=[w1.b], q="pool")
                S.dma(lambda e, w3=w3, j=j, w3v=w3v: e.dma_start(out=w3.t[:].rearrange("p k n -> p (k n)"), in_=w3v[j]), writes=[w3.b], q="pool")
                for (s0, sn) in nsub:
                    u1, u3 = ub.next(), ub.next()
                    for k in range(8):
                        S.pe(lambda e, u1=u1, w1=w1, k=k, s0=s0, sn=sn: e.matmul(u1.t[:, 0:sn], lhsT=w1.t[:, k, :], rhs=hT.t[:, k, s0:s0 + sn], start=(k == 0), stop=(k == 7)),
                             reads=[w1.b, hT.b], writes=[u1.b])
                    for k in range(8):
                        S.pe(lambda e, u3=u3, w3=w3, k=k, s0=s0, sn=sn: e.matmul(u3.t[:, 0:sn], lhsT=w3.t[:, k, :], rhs=hT.t[:, k, s0:s0 + sn], start=(k == 0), stop=(k == 7)),
                             reads=[w3.b, hT.b], writes=[u3.b])
                    sg = sg_r.next()
                    S.act(lambda e, sg=sg, u1=u1, sn=sn: e.activation(out=sg.t[:, 0:sn], in_=u1.t[:, 0:sn], func=AF.Silu), reads=[u1.b], writes=[sg.b])
                    S.dve(lambda e, sg=sg, u3=u3, j=j, s0=s0, sn=sn: e.tensor_tensor(out=act.t[:, j, s0:s0 + sn], in0=u3.t[:, 0:sn], in1=sg.t[:, 0:sn], op=ALU.mult),
                          reads=[sg.b, u3.b], writes=[act.b])
            for tl in range(n // 128):
                t = tok0 // 128 + tl
                which = 1 if t < 2 else 0
                yp = yp_r.next()
                for half in range(2):
                    for j in range(NJ):
                        S.pe(lambda e, yp=yp, j=j, tl=tl, half=half: e.matmul(yp.t[:, half * 512:(half + 1) * 512], lhsT=act.t[:, j, tl * 128:(tl + 1) * 128],
                                                                                rhs=W2.t[:, j, half * 512:(half + 1) * 512], start=(j == 0), stop=(j == NJ - 1)),
                             reads=[act.b, W2.b], writes=[yp.b])
                if ne > 1:
                    cap = comb.t[:, t - 2, ei:ei + 1]
                    if ei == 0:
                        S.dve(lambda e, yp=yp, tl=tl, cap=cap: e.tensor_scalar(out=yacc.t[:, tl, :], in0=yp.t[:, :], scalar1=cap, scalar2=None, op0=ALU.mult),
                              reads=[yp.b, comb.b], writes=[yacc.b])
                    else:
                        S.dve(lambda e, yp=yp, tl=tl, cap=cap: e.scalar_tensor_tensor(out=yacc.t[:, tl, :], in0=yp.t[:, :], scalar=cap, in1=yacc.t[:, tl, :], op0=ALU.mult, op1=ALU.add),
                              reads=[yp.b, comb.b, yacc.b], writes=[yacc.b])
                    if ei < ne - 1:
                        continue
                xt, st, tm = xt_r.next(), st_r.next(), tm_r.next()
                S.dma(lambda e, xt=xt, t=t: e.dma_start(out=xt.t[:], in_=res_ap(t)), reads=[ph.db(("res", t))], writes=[xt.b])
                S.dve(lambda e, st=st: e.memset(st.t[:], 0.0), writes=[st.b])
                for half in range(2):
                    hs = slice(half * 512, (half + 1) * 512)
                    jk = jk_r.next()
                    if ne > 1:
                        S.act(lambda e, jk=jk, st=st, half=half, hs=hs, tl=tl: e.activation(out=jk.t[:], in_=yacc.t[:, tl, hs], func=AF.Square, accum_out=st.t[:, half:half + 1]),
                              reads=[yacc.b], writes=[jk.b, st.b])
                    else:
                        S.act(lambda e, jk=jk, st=st, half=half, hs=hs, yp=yp: e.activation(out=jk.t[:], in_=yp.t[:, hs], func=AF.Square, accum_out=st.t[:, half:half + 1]),
                              reads=[yp.b], writes=[jk.b, st.b])
                rstd_from_ss(st, 2, D)
                for half in range(2):
                    hs = slice(half * 512, (half + 1) * 512)
                    if ne > 1:
                        S.dve(lambda e, tm=tm, st=st, hs=hs, tl=tl, which=which: e.scalar_tensor_tensor(out=tm.t[:, hs], in0=yacc.t[:, tl, hs], scalar=st.t[:, 7:8], in1=G3[which].t[:, hs], op0=ALU.mult, op1=ALU.mult),
                              reads=[yacc.b, st.b, G3[which].b], writes=[tm.b])
                    else:
                        S.dve(lambda e, tm=tm, st=st, hs=hs, yp=yp, which=which: e.scalar_tensor_tensor(out=tm.t[:, hs], in0=yp.t[:, hs], scalar=st.t[:, 7:8], in1=G3[which].t[:, hs], op0=ALU.mult, op1=ALU.mult),
                              reads=[yp.b, st.b, G3[which].b], writes=[tm.b])
                S.pool(lambda e, tm=tm, xt=xt: e.tensor_tensor(out=xt.t[:], in0=tm.t[:], in1=xt.t[:], op=ALU.add), reads=[tm.b, xt.b], writes=[xt.b])
                S.dma(lambda e, xt=xt, t=t: e.dma_start(out=res_ap(t), in_=xt.t[:]), reads=[xt.b], writes=[ph.db(("res", t))])


def _rot_perm(d):
    q = d // 4
    idx = np.arange(d)
    src = np.where((idx % (d // 2)) < q, idx + q, idx - q)
    return src


def _rope_tables():
    t = np.arange(SEQ)
    row = (t // GRID_W).astype(np.float32)
    col = (t % GRID_W).astype(np.float32)

    def tab(d, nrep, base):
        half = d // 2
        inv = (10000.0 ** (-np.arange(0, half, 2, dtype=np.float32) / half)).astype(np.float32)
        c = np.zeros((128, SEQ), np.float32)
        s = np.zeros((128, SEQ), np.float32)
        for j in range(d):
            pos = row if j < half else col
            f = inv[(j % half) % (half // 2)]
            ang = (pos * f).astype(np.float32)
            for r in range(nrep):
                c[base + r * d + j] = np.cos(ang)
                s[base + r * d + j] = np.sin(ang)
        return c, s
    cA, sA = tab(32, 1, 64)
    cB, sB = tab(64, 2, 0)
    return cA, sA, cB, sB


def _prep_shared(inp):
    f = lambda a: np.ascontiguousarray(a, dtype=np.float32)
    sh = {}
    sh["w_mod"] = f(inp["w_mod"])
    sh["b_mod"] = f(inp["b_mod"])
    sh["norm_g"] = f(inp["norm_g"].reshape(2, 4 * D))
    w_in = inp["a_w_in"][0]
    sh["a_w_in"] = f(w_in)
    p32 = _rot_perm(32)
    p64 = _rot_perm(64)
    cols = [640 + p32]
    for h in range(8):
        cols.append(672 + h * 64 + p64)
    for h in range(8):
        cols.append(1184 + h * 64 + p64)
    sh["a_w_in_p"] = f(w_in[:, np.concatenate(cols)])
    sh["a_q_norm_t"] = f(inp["a_q_norm"][0].reshape(3, 128).T)
    sh["a_kv_norm_t"] = f(inp["a_kv_norm"][0].reshape(2, 128).T)
    w_uq = inp["a_w_uq"][0]
    sh["a_w_uq"] = f(w_uq)
    sh["a_w_uq_p"] = f(w_uq[:, np.concatenate([h * 96 + 64 + p32 for h in range(8)])])
    w_ukv = inp["a_w_ukv"][0]
    kc = np.concatenate([h * 128 + np.arange(64) for h in range(8)])
    vc = np.concatenate([h * 128 + 64 + np.arange(64) for h in range(8)])
    sh["a_w_ukv_r"] = f(w_ukv[:, np.concatenate([kc, vc])])
    sh["b_lambda"] = f(inp["b_lambda"][0].reshape(1, 256))
    sh["b_subln_t"] = f(inp["b_subln"][0].reshape(128, 1))
    sh["ab_w_out"] = f(inp["ab_w_out"][0])
    for nm_, src in (("f_w1r", inp["f_w1"][0]), ("f_w3r", inp["f_w3"][0])):
        sh[nm_] = f(np.asarray(src).reshape(8, 128, NJ, 128).transpose(2, 1, 0, 3).reshape(NJ, 128, 1024))
    sh["f_w2"] = f(inp["f_w2"])
    sh["c_w_qkv"] = f(inp["c_w_qkv"][0])
    sh["c_b_qk_t"] = f(inp["c_b_qkv"][0][0:2048].reshape(16, 128).T)
    sh["c_b_v"] = f(inp["c_b_qkv"][0][2048:3072].reshape(1, D))
    rpb = inp["c_rpb"][0]
    kl = np.arange(128)
    r_l, c_l = kl // 64, kl % 64
    g = np.zeros((16, ND, 128, 128), np.float32)
    for di, dd in enumerate(range(-3, 4)):
        dr = np.clip(2 * dd + r_l[:, None] - r_l[None, :] + 7, 0, 14)
        dc = np.clip(c_l[:, None] - c_l[None, :] + 15, 0, 30)
        g[:, di] = rpb[:, dr, dc]
    sh["rpbg"] = g
    sh["nmask"] = f(NA_MASKS)
    sh["c_w_out"] = f(inp["c_w_out"][0])
    sh["c_b_out"] = f(inp["c_b_out"][0].reshape(1, D))
    sh["m_router"] = f(inp["m_router"][0])
    for nm_, src in (("m_w1r", inp["m_w1"][0]), ("m_w3r", inp["m_w3"][0])):
        sh[nm_] = f(np.asarray(src).reshape(NEXP, 8, 128, 11, 2, 128).transpose(0, 3, 2, 4, 1, 5).reshape(NEXP * 11 * 128, 2048))
    sh["m_w2r"] = f(np.asarray(inp["m_w2"][0]).reshape(NEXP, 11, 2, 128, 1024).transpose(0, 1, 3, 2, 4).reshape(NEXP * 11 * 128, 2048))
    rconst = np.zeros((128, 161), np.float32)
    rconst[:, 0:128] = np.triu(np.ones((128, 128), np.float32))
    rconst[:, 128:136] = np.arange(8, dtype=np.float32)[None, :] * 512.0
    rconst[:, 136:160] = np.arange(24, dtype=np.float32)[None, :]
    rconst[:, 160] = np.arange(128, dtype=np.float32)
    sh["rconst"] = rconst
    cA, sA, cB, sB = _rope_tables()
    sh["cosA"], sh["sinA"], sh["cosB"], sh["sinB"] = cA, sA, cB, sB
    sh["ident"] = np.eye(128, dtype=np.float32)
    return sh


def make_in_maps(inp, cores):
    sh = _prep_shared(inp)
    maps = []
    for b in cores:
        m = dict(sh)
        m["x"] = np.ascontiguousarray(inp["x"][b], dtype=np.float32)
        m["ctx"] = np.ascontiguousarray(inp["ctx"][b], dtype=np.float32)
        ccv = np.stack([inp["c"][b], inp["c_ctx"]], axis=-1).astype(np.float32)
        m["cc"] = np.ascontiguousarray(ccv.reshape(8, 128, 2).transpose(1, 0, 2))
        maps.append(m)
    return maps


_NC_CACHE = {}


def kernel(**inputs):
    if "nc" not in _NC_CACHE:
        _NC_CACHE["nc"] = build_program()
    nc = _NC_CACHE["nc"]
    in_maps = make_in_maps(inputs, list(range(8)))
    res = run_bass_kernel_spmd(nc, in_maps, core_ids=list(range(8)))
    return np.stack([np.asarray(r["out"], dtype=np.float32) for r in res.results], axis=0)
```

```python
import math
import types
from contextlib import ExitStack
import numpy as np
import concourse.bass as bass
import concourse.mybir as mybir
from concourse.bass_utils import run_bass_kernel_spmd

F32 = mybir.dt.float32
BF16 = mybir.dt.bfloat16
ALU = mybir.AluOpType
AF = mybir.ActivationFunctionType

D = 1024
SEQ = 4096
CTX = 256
NTOK = SEQ + CTX
NT = NTOK // 128
EPS = 1e-6
DFF = 2816
NJ = DFF // 128
NEXP = 8
GRID_W = 64
MLA_SCALE = 96 ** -0.5
DIFF_SCALE = 64 ** -0.5
NA_SCALE = 64 ** -0.5
LAM_INIT = 0.8 - 0.6 * math.exp(-0.0)
NEG = -30000.0
NSLOT = 23
U32 = mybir.dt.uint32


class Buf:
    __slots__ = ("name", "w", "r", "rd")

    def __init__(self, name):
        self.name = name
        self.w = None
        self.r = {}
        self.rd = []


class Op:
    __slots__ = ("eng", "fn", "deps", "sig", "dma", "sem", "semval", "val", "id", "done", "lat", "cost")


def _freeze(fn):
    if fn.__closure__ is None:
        return fn
    cells = []
    for c in fn.__closure__:
        try:
            cells.append(types.CellType(c.cell_contents))
        except ValueError:
            cells.append(c)
    g = types.FunctionType(fn.__code__, fn.__globals__, fn.__name__, fn.__defaults__, tuple(cells))
    g.__kwdefaults__ = fn.__kwdefaults__
    return g


class Sched:
    ENGS = ("pe", "act", "dve", "pool", "sp")
    NRING = 16

    def __init__(self, nc, sems):
        self.nc = nc
        self.sems = sems
        self.ops = []
        self.allbufs = []
        self.cnt = {e: 0 for e in self.ENGS}
        self.dcnt = {e: 0 for e in self.ENGS}
        self.nid = 0
        self.total = 0

    def buf(self, name=None):
        b = Buf(name or "b")
        self.allbufs.append(b)
        return b

    def add(self, eng, fn, reads=(), writes=(), dma=False, lat=3.0, cost=None):
        op = Op()
        op.lat = lat
        op.cost = cost
        op.eng = eng
        op.fn = _freeze(fn)
        op.dma = dma
        op.sig = False
        op.done = False
        op.id = self.nid
        self.nid += 1
        deps = {}
        for b in reads:
            if b.w is not None:
                deps[b.w.id] = b.w
        for b in writes:
            if b.w is not None:
                deps[b.w.id] = b.w
            for o in b.r.values():
                deps[o.id] = o
            for o in b.rd:
                deps[o.id] = o
        op.deps = [d for d in deps.values() if not d.done]
        for b in writes:
            b.w = op
            b.r = {}
            b.rd = []
        for b in reads:
            if b.w is op:
                continue
            if dma:
                b.rd.append(op)
            else:
                b.r[eng] = op
        self.ops.append(op)
        return op

    def pe(self, fn, reads=(), writes=(), cost=None):
        return self.add("pe", fn, reads, writes, cost=cost)

    def act(self, fn, reads=(), writes=(), cost=None):
        return self.add("act", fn, reads, writes, cost=cost)

    def dve(self, fn, reads=(), writes=(), cost=None):
        return self.add("dve", fn, reads, writes, cost=cost)

    def pool(self, fn, reads=(), writes=(), cost=None):
        return self.add("pool", fn, reads, writes, cost=cost)

    def dma(self, fn, reads=(), writes=(), q="sp", lat=3.0, cost=None):
        return self.add(q, fn, reads, writes, dma=True, lat=lat, cost=cost)

    COST = {"pe": 0.2, "act": 0.75, "dve": 0.75, "pool": 1.6}

    def list_schedule(self, ops):
        import heapq
        pos_ = {op.id: i for i, op in enumerate(ops)}
        nd = [0] * len(ops)
        users = [[] for _ in ops]
        for i, op in enumerate(ops):
            for d in op.deps:
                j = pos_.get(d.id)
                if j is not None:
                    nd[i] += 1
                    users[j].append(i)
        ready = [0.0] * len(ops)
        fin = [0.0] * len(ops)
        heaps = {e: [] for e in self.ENGS}
        for i, op in enumerate(ops):
            if nd[i] == 0:
                heapq.heappush(heaps[op.eng], (0.0, i))
        free = {e: 0.0 for e in self.ENGS}
        order = []
        n = len(ops)
        while len(order) < n:
            best = None
            for e in self.ENGS:
                h = heaps[e]
                if h:
                    st = max(free[e], h[0][0])
                    if best is None or st < best[0] or (st == best[0] and h[0][1] < best[2]):
                        best = (st, e, h[0][1])
            st, e, i = best
            heapq.heappop(heaps[e])
            op = ops[i]
            if op.dma:
                busy = op.cost if op.cost is not None else (1.0 if e == "pool" else 0.06)
                fin[i] = st + busy + op.lat
            else:
                busy = self.COST[e] if op.cost is None else op.cost
                fin[i] = st + busy + 0.1
            free[e] = st + busy
            order.append(op)
            for u in users[i]:
                ready[u] = max(ready[u], fin[i])
                nd[u] -= 1
                if nd[u] == 0:
                    heapq.heappush(heaps[ops[u].eng], (ready[u], u))
        return order

    def emit_phase(self, reorder=False):
        nc = self.nc
        sems = self.sems
        if reorder:
            self.ops = self.list_schedule(self.ops)
        ops = self.ops
        for op in ops:
            for d in op.deps:
                if not d.dma:
                    if op.eng == "pe" and d.eng == "pe" and not op.dma:
                        continue
                    d.sig = True
        for op in ops:
            if op.dma:
                i = self.dcnt[op.eng]
                self.dcnt[op.eng] += 1
                ring = sems["ring"][op.eng]
                op.sem = ring[i % len(ring)]
                op.semval = 16 * (i // len(ring) + 1)
            elif op.sig:
                self.cnt[op.eng] += 1
                op.val = self.cnt[op.eng]
        by_eng = {e: [o for o in ops if o.eng == e] for e in self.ENGS}
        final = {}
        for e in ("sp", "pool", "act"):
            ring = sems["ring"][e]
            n = self.dcnt[e]
            for j, s in enumerate(ring):
                k = (n - 1 - j) // len(ring) + 1 if n > j else 0
                if k > 0:
                    final[(e, j)] = (s, 16 * k)

        def run(e, eng):
            seen = {}
            for op in by_eng[e]:
                need = {}
                for d in op.deps:
                    if d.dma:
                        key = ("d", id(d.sem))
                        v = d.semval
                        s = d.sem
                    else:
                        if e == "pe" and d.eng == "pe" and not op.dma:
                            continue
                        key = ("e", d.eng)
                        v = d.val
                        s = sems["eng"][d.eng]
                    if seen.get(key, 0) < v and (key not in need or need[key][1] < v):
                        need[key] = (s, v)
                if op.dma and op.semval > 16:
                    key = ("d", id(op.sem))
                    v = op.semval - 16
                    if seen.get(key, 0) < v and (key not in need or need[key][1] < v):
                        need[key] = (op.sem, v)
                for key, (s, v) in need.items():
                    eng.wait_ge(s, v)
                    seen[key] = v
                ins = op.fn(eng)
                if op.dma:
                    ins.then_inc(op.sem, 16)
                elif op.sig:
                    ins.then_inc(sems["eng"][e], 1)
            if e == "sp":
                for s, v in final.values():
                    eng.wait_ge(s, v)

        with nc.Block() as block:
            @block.sync
            def _(eng):
                run("sp", eng)

            @block.tensor
            def _(eng):
                run("pe", eng)

            @block.scalar
            def _(eng):
                run("act", eng)

            @block.vector
            def _(eng):
                run("dve", eng)

            @block.gpsimd
            def _(eng):
                run("pool", eng)
        nc.all_engine_barrier()
        self.total += len(ops)
        for op in ops:
            op.done = True
        self.ops = []
        for b in self.allbufs:
            b.w = None
            b.r = {}
            b.rd = []
        self.allbufs = []


class TB:
    __slots__ = ("t", "b")

    def __init__(self, t, b):
        self.t = t
        self.b = b


class Rot:
    def __init__(self, items):
        self.items = items
        self.i = 0

    def next(self):
        x = self.items[self.i % len(self.items)]
        self.i += 1
        return x


CHUNKS = [(0, 256, True)] + [(256 + 512 * i, 512, False) for i in range(8)]


def na_variants():
    masks = []
    keyidx = {}
    table = {}
    kl = np.arange(128)
    kr_l, kc = kl // 64, kl % 64
    for i in range(32):
        qr = 2 * i + kr_l
        qc = kc
        rs = np.clip(qr - 4, 0, 56)
        cs = np.clip(qc - 8, 0, 48)
        for kt in range(32):
            kr = 2 * kt + kr_l
            inw = ((kr[:, None] >= rs[None, :]) & (kr[:, None] < rs[None, :] + 8)
                   & (kc[:, None] >= cs[None, :]) & (kc[:, None] < cs[None, :] + 16))
            if not inw.any():
                continue
            m = np.where(inw, 0.0, NEG).astype(np.float32)
            key = m.tobytes()
            if key not in keyidx:
                keyidx[key] = len(masks)
                masks.append(m)
            table[(i, kt)] = keyidx[key]
    return table, np.stack(masks)


NA_TABLE, NA_MASKS = na_variants()
NVAR = NA_MASKS.shape[0]
ND = 7
NA_COMBOS = []
NA_CTAB = {}
for (_i, _kt), _var in sorted(NA_TABLE.items()):
    _c = (_var, _kt - _i + 3)
    if _c not in NA_COMBOS:
        NA_COMBOS.append(_c)
    NA_CTAB[(_i, _kt)] = NA_COMBOS.index(_c)
NCOMBO = len(NA_COMBOS)
NA_CLASSES = []
NA_CLS_OF_I = {}
for _i in range(32):
    _cl = tuple(NA_CTAB[(_i, _kt)] for _kt in range(32) if (_i, _kt) in NA_CTAB)
    if _cl not in NA_CLASSES:
        NA_CLASSES.append(_cl)
    NA_CLS_OF_I[_i] = NA_CLASSES.index(_cl)
NCLS = len(NA_CLASSES)


def build_program(upto=99, debug=False):
    nc = bass.Bass("TRN2", target_bir_lowering=False)

    def din(name, shape, dt=F32):
        return nc.dram_tensor(name, list(shape), dt, kind="ExternalInput").ap()

    def dint(name, shape, dt):
        return nc.dram_tensor(name, list(shape), dt, kind=("ExternalOutput" if debug else "Internal")).ap()

    x_d = din("x", [SEQ, D])
    ctx_d = din("ctx", [CTX, D])
    cc_d = din("cc", [128, 8, 2])
    wmod_d = din("w_mod", [2, D, 6 * D])
    bmod_d = din("b_mod", [2, 6 * D])
    ng_d = din("norm_g", [2, 4 * D])
    win_d = din("a_w_in", [D, 2208])
    winp_d = din("a_w_in_p", [D, 1056])
    qn_d = din("a_q_norm_t", [128, 3])
    kvn_d = din("a_kv_norm_t", [128, 2])
    wuq_d = din("a_w_uq", [384, 768])
    wuqp_d = din("a_w_uq_p", [384, 256])
    wukv_d = din("a_w_ukv_r", [256, 1024])
    lam_d = din("b_lambda", [1, 256])
    subln_d = din("b_subln_t", [128, 1])
    wo_d = din("ab_w_out", [D, D])
    fw1_d = din("f_w1r", [NJ, 128, 8 * 128])
    fw3_d = din("f_w3r", [NJ, 128, 8 * 128])
    fw2_d = din("f_w2", [1, DFF, D])
    wqkv_d = din("c_w_qkv", [D, 3 * D])
    bqk_d = din("c_b_qk_t", [128, 16])
    bv_d = din("c_b_v", [1, D])
    rpbg_d = din("rpbg", [16, ND, 128, 128])
    nmask_d = din("nmask", [NVAR, 128, 128])
    cwo_d = din("c_w_out", [D, D])
    cbo_d = din("c_b_out", [1, D])
    rt_d = din("m_router", [D, NEXP])
    mw1_d = din("m_w1r", [NEXP * 11 * 128, 2048])
    mw3_d = din("m_w3r", [NEXP * 11 * 128, 2048])
    mw2_d = din("m_w2r", [NEXP * 11 * 128, 2048])
    rconst_d = din("rconst", [128, 161])
    cosA_d = din("cosA", [128, SEQ])
    sinA_d = din("sinA", [128, SEQ])
    cosB_d = din("cosB", [128, SEQ])
    sinB_d = din("sinB", [128, SEQ])
    ident_d = din("ident", [128, 128])
    out_d = nc.dram_tensor("out", [SEQ, D], F32, kind="ExternalOutput").ap()

    modrows_d = dint("modrows", [2, 2, 6, D], F32)
    qA_d = dint("qA", [8, 96, NTOK], BF16)
    kAn_d = dint("kAn", [8 * 64, NTOK], BF16)
    kAr_d = dint("kAr", [32, NTOK], BF16)
    vA_d = dint("vA", [NTOK, 512], BF16)
    qB_d = dint("qB", [4, 128, NTOK], BF16)
    kB_d = dint("kB", [4, 128, NTOK], BF16)
    vB_d = dint("vB", [NTOK, 512], BF16)
    cs_d = dint("cs", [CTX, D], F32)
    hT_d = dint("hT", [128, 8, NTOK], BF16)
    qC_d = dint("qC", [8, 128, NTOK], BF16)
    kC_d = dint("kC", [8, 128, NTOK], BF16)
    vC_d = dint("vC", [NTOK, D], BF16)
    oT_d = dint("oT", [128, 8, NTOK], BF16)
    comb_d = dint("comb", [128, 32, NEXP], F32)
    h4_d = dint("h4", [SEQ, D], BF16)
    Hs_d = dint("Hs", [NSLOT * 512, D], BF16)
    R_d = dint("R", [NSLOT * 512, D], F32)
    wb_d = [dint(f"wb{i}", [NEXP * 11 * 128, 2048], BF16) for i in range(3)]

    with ExitStack() as ges:
        sems = {"eng": {}, "ring": {}}
        for e in Sched.ENGS:
            sems["eng"][e] = ges.enter_context(nc.semaphore("s_" + e))
        for e in ("sp", "pool", "act"):
            sems["ring"][e] = [ges.enter_context(nc.semaphore(f"r_{e}{i}")) for i in range(Sched.NRING)]
        S = Sched(nc, sems)

        pp = [ges.enter_context(nc.psum_tensor(f"pp{i}", [128, 1024], F32)) for i in range(4)]

        def bank_ap(i):
            return pp[i // 2][:, (i % 2) * 512:(i % 2 + 1) * 512]

        def gsb(name, shape, dt):
            return ges.enter_context(nc.sbuf_tensor(name, list(shape), dt))

        ident_f = gsb("ident_f", [128, 128], F32)
        ident_b = gsb("ident_b", [128, 128], BF16)
        ones_b = gsb("ones_b", [128, 128], BF16)
        ones_f = gsb("ones_f", [128, 128], F32)

        class Phase:
            def __init__(self, reorder=False):
                self.reorder = reorder
                self.es = ExitStack()
                self.n = 0
                self.banks = [TB(bank_ap(i), S.buf(f"bank{i}")) for i in range(8)]
                self.pairs = [TB(pp[i], S.buf(f"pair{i}")) for i in range(4)]
                self.ident_f = TB(ident_f, S.buf("identf"))
                self.ident_b = TB(ident_b, S.buf("identb"))
                self.ones_b = TB(ones_b, S.buf("onesb"))
                self.ones_f = TB(ones_f, S.buf("onesf"))
                self.dram = {}

            def sb(self, shape, dt, name=None):
                self.n += 1
                t = self.es.enter_context(nc.sbuf_tensor(f"{name or 't'}_{S.total}_{self.n}", list(shape), dt))
                return TB(t, S.buf(name))

            def rot(self, k, shape, dt, name=None):
                return Rot([self.sb(shape, dt, (name or "r") + f"_{S.total}_{self.n}_{i}") for i in range(k)])

            def db(self, key):
                if key not in self.dram:
                    self.dram[key] = S.buf(str(key))
                return self.dram[key]

            def close(self):
                S.emit_phase(reorder=self.reorder)
                self.es.close()

        ph = Phase(reorder=True)
        S.dma(lambda e: e.dma_start(out=ident_f[:], in_=ident_d), writes=[ph.ident_f.b])
        S.dma(lambda e: e.dma_start(out=ident_b[:], in_=ident_d), writes=[ph.ident_b.b], q="pool")
        S.dve(lambda e: e.memset(ones_b[:], 1.0), writes=[ph.ones_b.b])
        S.dve(lambda e: e.memset(ones_f[:], 1.0), writes=[ph.ones_f.b])
        cc = ph.sb([128, 8, 2], F32)
        ccs = ph.sb([128, 8, 2], BF16)
        S.dma(lambda e: e.dma_start(out=cc.t[:], in_=cc_d), writes=[cc.b])
        S.act(lambda e: e.activation(out=ccs.t[:], in_=cc.t[:], func=AF.Silu), reads=[cc.b], writes=[ccs.b])
        wblk = ph.rot(3, [128, 8, 512], BF16)
        modsb = [ph.sb([2, 6 * D], F32) for _ in range(2)]
        brow = [ph.sb([2, 6 * D], F32) for _ in range(2)]
        grow = [ph.sb([2, 4 * D], F32) for _ in range(2)]
        drow = [ph.sb([2, 6 * D], F32) for _ in range(2)]
        pbank = Rot(ph.banks)
        for i in range(2):
            S.dma(lambda e, i=i: e.dma_start(out=brow[i].t[:], in_=bmod_d[i:i + 1, :].partition_broadcast(2)), writes=[brow[i].b])
            S.dma(lambda e, i=i: e.dma_start(out=grow[i].t[:], in_=ng_d[i:i + 1, :].partition_broadcast(2)), writes=[grow[i].b])
            wv = wmod_d[i].rearrange("(k p) n -> p k n", p=128)
            for nb in range(12):
                w = wblk.next()
                S.dma(lambda e, w=w, nb=nb, wv=wv: e.dma_start(out=w.t[:], in_=wv[:, :, nb * 512:(nb + 1) * 512]), writes=[w.b], q="pool")
                pb = pbank.next()
                for k in range(8):
                    S.pe(lambda e, w=w, k=k, pb=pb: e.matmul(pb.t[0:2, :], lhsT=ccs.t[:, k, :], rhs=w.t[:, k, :], start=(k == 0), stop=(k == 7)),
                         reads=[ccs.b, w.b], writes=[pb.b])
                S.dve(lambda e, i=i, nb=nb, pb=pb: e.tensor_tensor(out=modsb[i].t[:, nb * 512:(nb + 1) * 512], in0=pb.t[0:2, :],
                                                                     in1=brow[i].t[:, nb * 512:(nb + 1) * 512], op=ALU.add),
                      reads=[pb.b, brow[i].b], writes=[modsb[i].b])
            m, g, dr = modsb[i], grow[i], drow[i]

            def sl(j):
                return slice(j * D, (j + 1) * D)
            S.dve(lambda e, m=m, g=g, dr=dr: e.scalar_tensor_tensor(out=dr.t[:, sl(0)], in0=m.t[:, sl(1)], scalar=1.0, in1=g.t[:, sl(0)], op0=ALU.add, op1=ALU.mult),
                  reads=[m.b, g.b], writes=[dr.b])
            S.dve(lambda e, m=m, dr=dr: e.tensor_copy(out=dr.t[:, sl(1)], in_=m.t[:, sl(0)]), reads=[m.b], writes=[dr.b])
            S.dve(lambda e, m=m, g=g, dr=dr: e.tensor_tensor(out=dr.t[:, sl(2)], in0=m.t[:, sl(2)], in1=g.t[:, sl(1)], op=ALU.mult), reads=[m.b, g.b], writes=[dr.b])
            S.dve(lambda e, m=m, g=g, dr=dr: e.scalar_tensor_tensor(out=dr.t[:, sl(3)], in0=m.t[:, sl(4)], scalar=1.0, in1=g.t[:, sl(2)], op0=ALU.add, op1=ALU.mult),
                  reads=[m.b, g.b], writes=[dr.b])
            S.dve(lambda e, m=m, dr=dr: e.tensor_copy(out=dr.t[:, sl(4)], in_=m.t[:, sl(3)]), reads=[m.b], writes=[dr.b])
            S.dve(lambda e, m=m, g=g, dr=dr: e.tensor_tensor(out=dr.t[:, sl(5)], in0=m.t[:, sl(5)], in1=g.t[:, sl(3)], op=ALU.mult), reads=[m.b, g.b], writes=[dr.b])
            S.dma(lambda e, i=i, dr=dr: e.dma_start(out=modrows_d[i].rearrange("a r d -> a (r d)"), in_=dr.t[:]), reads=[dr.b], writes=[ph.db("modrows")])
        ph.close()

        def load_bc(ph, layer, which, row):
            t = ph.sb([128, D], F32)
            S.dma(lambda e: e.dma_start(out=t.t[:], in_=modrows_d[layer, which, row:row + 1, :].partition_broadcast(128)), writes=[t.b])
            return t

        class NormCtx:
            def __init__(self, ph):
                self.st = ph.rot(4, [128, 8], F32, "st")
                self.tmp = ph.rot(2, [128, D], F32, "ntmp")
                self.hb = ph.rot(2, [128, D], BF16, "hb")

        def rstd_from_ss(st, ncols, dim):
            if ncols == 2:
                S.dve(lambda e: e.tensor_tensor(out=st.t[:, 0:1], in0=st.t[:, 0:1], in1=st.t[:, 1:2], op=ALU.add), reads=[st.b], writes=[st.b])
            S.dve(lambda e: e.tensor_scalar(out=st.t[:, 4:5], in0=st.t[:, 0:1], scalar1=1.0 / dim, scalar2=EPS, op0=ALU.mult, op1=ALU.add), reads=[st.b], writes=[st.b])
            S.act(lambda e: e.activation(out=st.t[:, 5:6], in_=st.t[:, 4:5], func=AF.Sqrt), reads=[st.b], writes=[st.b])
            S.dve(lambda e: e.reciprocal(out=st.t[:, 7:8], in_=st.t[:, 5:6]), reads=[st.b], writes=[st.b])

        def norm_mod_T(ph, nx, xt, A, sh, dst_ap, dst_b, tbank):
            st, tmp, hb = nx.st.next(), nx.tmp.next(), nx.hb.next()
            junk = tmp
            S.dve(lambda e: e.memset(st.t[:], 0.0), writes=[st.b])
            S.act(lambda e: e.activation(out=junk.t[:], in_=xt.t[:], func=AF.Square, accum_out=st.t[:, 0:1]), reads=[xt.b], writes=[junk.b, st.b])
            rstd_from_ss(st, 1, D)
            S.dve(lambda e: e.scalar_tensor_tensor(out=tmp.t[:], in0=xt.t[:], scalar=st.t[:, 7:8], in1=A.t[:], op0=ALU.mult, op1=ALU.mult),
                  reads=[xt.b, st.b, A.b], writes=[tmp.b])
            S.pool(lambda e: e.tensor_tensor(out=hb.t[:], in0=tmp.t[:], in1=sh.t[:], op=ALU.add), reads=[tmp.b, sh.b], writes=[hb.b])
            pv = tbank.t.bitcast(BF16)
            for k in range(8):
                S.pe(lambda e, k=k: e.transpose(out=pv[:, k * 128:(k + 1) * 128], in_=hb.t[:, k * 128:(k + 1) * 128], identity=ph.ident_b.t[:]),
                     reads=[hb.b, ph.ident_b.b], writes=[tbank.b])
            S.act(lambda e: e.activation(out=dst_ap, in_=pv.rearrange("p (k t) -> p k t", k=8), func=AF.Copy), reads=[tbank.b], writes=[dst_b])
            return hb

        def tok_src(t):
            if t < 2:
                return ctx_d[t * 128:(t + 1) * 128, :]
            return x_d[(t - 2) * 128:(t - 1) * 128, :]

        def res_ap(t):
            if t < 2:
                return cs_d[t * 128:(t + 1) * 128, :]
            return out_d[(t - 2) * 128:(t - 1) * 128, :]

        def castload(ph, dst, src_ap, q="pool"):
            S.dma(lambda e: e.dma_start(out=dst.t[:], in_=src_ap), writes=[dst.b], q=q)

        def mm_acc(out_ap, out_b, pairs, reads):
            n = len(pairs)
            for i, (l, r) in enumerate(pairs):
                S.pe(lambda e, l=l, r=r, i=i: e.matmul(out_ap, lhsT=l, rhs=r, start=(i == 0), stop=(i == n - 1)), reads=reads, writes=[out_b])

        if upto >= 1:
            ph = Phase(reorder=True)
            nx = NormCtx(ph)
            A0 = [load_bc(ph, 0, w, 0) for w in (0, 1)]
            SH0 = [load_bc(ph, 0, w, 1) for w in (0, 1)]
            win = ph.sb([128, 8, 2208], BF16)
            winp = ph.sb([128, 8, 1056], BF16)
            wuq = ph.sb([128, 3, 768], BF16)
            wuqp = ph.sb([128, 3, 256], BF16)
            wukv = ph.sb([128, 2, 1024], BF16)
            qn = ph.sb([128, 3], F32)
            kvn = ph.sb([128, 2], F32)
            stg_r = ph.rot(2, [128, 1024], F32, "stg")
            wv = win_d.rearrange("(k p) n -> p k n", p=128)
            for k in range(8):
                S.dma(lambda e, k=k: e.dma_start(out=win.t[:, k, :], in_=wv[:, k, :]), writes=[win.b], q="pool")
            wv2 = winp_d.rearrange("(k p) n -> p k n", p=128)
            for k in range(8):
                S.dma(lambda e, k=k: e.dma_start(out=winp.t[:, k, :], in_=wv2[:, k, :]), writes=[winp.b], q="pool")
            S.dma(lambda e: e.dma_start(out=qn.t[:], in_=qn_d), writes=[qn.b])
            S.dma(lambda e: e.dma_start(out=kvn.t[:], in_=kvn_d), writes=[kvn.b])
            for (dst, src_d, nk, nc_, gn) in ((wuq, wuq_d, 3, 768, qn), (wuqp, wuqp_d, 3, 256, qn), (wukv, wukv_d, 2, 1024, kvn)):
                for k in range(nk):
                    stg = stg_r.next()
                    S.dma(lambda e, stg=stg, k=k, src_d=src_d, nc_=nc_: e.dma_start(out=stg.t[:, 0:nc_], in_=src_d[k * 128:(k + 1) * 128, :]), writes=[stg.b])
                    S.dve(lambda e, stg=stg, k=k, dst=dst, nc_=nc_, gn=gn: e.tensor_scalar(out=dst.t[:, k, :], in0=stg.t[:, 0:nc_], scalar1=gn.t[:, k:k + 1], scalar2=None, op0=ALU.mult),
                          reads=[stg.b, gn.b], writes=[dst.b])
            for k in range(3):
                v = wuqp.t[:, k, :].rearrange("p (h q e) -> p h q e", h=8, q=4)
                for qq in (0, 2):
                    S.dve(lambda e, v=v, qq=qq: e.tensor_scalar(out=v[:, :, qq, :], in0=v[:, :, qq, :], scalar1=-1.0, scalar2=None, op0=ALU.mult),
                          reads=[wuqp.b], writes=[wuqp.b])
            for k in range(8):
                v0 = winp.t[:, k, 0:32].rearrange("p (q e) -> p q e", q=4)
                v1 = winp.t[:, k, 32:1056].rearrange("p (h q e) -> p h q e", h=16, q=4)
                for qq in (0, 2):
                    S.pool(lambda e, v0=v0, qq=qq: e.tensor_scalar(out=v0[:, qq, :], in0=v0[:, qq, :], scalar1=-1.0, scalar2=None, op0=ALU.mult),
                           reads=[winp.b], writes=[winp.b])
                    S.pool(lambda e, v1=v1, qq=qq: e.tensor_scalar(out=v1[:, :, qq, :], in0=v1[:, :, qq, :], scalar1=-1.0, scalar2=None, op0=ALU.mult),
                           reads=[winp.b], writes=[winp.b])

            xt_r = ph.rot(2, [128, D], F32, "xt")
            hT_r = ph.rot(2, [128, 8, 512], BF16, "hT")
            cq_r = ph.rot(1, [128, 5, 512], F32, "cq")
            sq_r = ph.rot(2, [128, 512], BF16, "sq")
            rs_r = ph.rot(2, [128, 512], F32, "rs")
            cn_r = ph.rot(2, [128, 5, 512], BF16, "cn")
            ob_r = ph.rot(4, [128, 512], BF16, "ob")
            t1_r = ph.rot(2, [128, 512], F32, "t1")
            t2_r = ph.rot(2, [128, 512], F32, "t2")
            rope_r = ph.rot(2, [128, 4, 512], F32, "rope")
            bk = Rot(ph.banks[0:7])
            tb = ph.banks[7]

            for (tok0, n, is_ctx) in CHUNKS:
                w = 0 if is_ctx else 1
                which = 1 if is_ctx else 0
                hT = hT_r.next()
                for j in range(n // 128):
                    t = tok0 // 128 + j
                    xt = xt_r.next()
                    S.dma(lambda e, xt=xt, t=t: e.dma_start(out=xt.t[:], in_=tok_src(t)), writes=[xt.b])
                    norm_mod_T(ph, nx, xt, A0[which], SH0[which], hT.t[:, :, j * 128:(j + 1) * 128], hT.b, tb)
                if not is_ctx:
                    rp = rope_r.next()
                    p0 = tok0 - CTX
                    for ii, src in enumerate((cosA_d, sinA_d, cosB_d, sinB_d)):
                        S.dma(lambda e, ii=ii, src=src, rp=rp, p0=p0: e.dma_start(out=rp.t[:, ii, :], in_=src[:, p0:p0 + 512]), writes=[rp.b])
                cq = cq_r.next()
                cn = cn_r.next()
                for (b0, nb, dim) in ((0, 3, 384.0), (3, 2, 256.0)):
                    ssb = bk.next()
                    for bi in range(nb):
                        blk = b0 + bi
                        pb = bk.next()
                        mm_acc(pb.t[:, 0:n], pb.b, [(win.t[:, k, blk * 128:(blk + 1) * 128], hT.t[:, k, 0:n]) for k in range(8)], [win.b, hT.b])
                        sq = sq_r.next()
                        S.act(lambda e, sq=sq, pb=pb: e.activation(out=sq.t[:, 0:n], in_=pb.t[:, 0:n], func=AF.Square), reads=[pb.b], writes=[sq.b])
                        S.act(lambda e, cq=cq, pb=pb, blk=blk: e.activation(out=cq.t[:, blk, 0:n], in_=pb.t[:, 0:n], func=AF.Copy), reads=[pb.b], writes=[cq.b])
                        S.pe(lambda e, sq=sq, ssb=ssb, bi=bi, nb=nb: e.matmul(ssb.t[:, 0:n], lhsT=ph.ones_b.t[:], rhs=sq.t[:, 0:n], start=(bi == 0), stop=(bi == nb - 1)),
                             reads=[sq.b, ph.ones_b.b], writes=[ssb.b])
                    rs = rs_r.next()
                    S.dve(lambda e, rs=rs, ssb=ssb, dim=dim: e.tensor_scalar(out=rs.t[:, 0:n], in0=ssb.t[:, 0:n], scalar1=1.0 / dim, scalar2=EPS, op0=ALU.mult, op1=ALU.add),
                          reads=[ssb.b], writes=[rs.b])
                    S.act(lambda e, rs=rs: e.activation(out=rs.t[:, 0:n], in_=rs.t[:, 0:n], func=AF.Sqrt), reads=[rs.b], writes=[rs.b])
                    S.dve(lambda e, rs=rs: e.reciprocal(out=rs.t[:, 0:n], in_=rs.t[:, 0:n]), reads=[rs.b], writes=[rs.b])
                    for bi in range(nb):
                        blk = b0 + bi
                        S.dve(lambda e, blk=blk, rs=rs, cq=cq, cn=cn: e.tensor_tensor(out=cn.t[:, blk, 0:n], in0=cq.t[:, blk, 0:n], in1=rs.t[:, 0:n], op=ALU.mult),
                              reads=[cq.b, rs.b], writes=[cn.b])

                def rope_out(pa, pb2, lo, hi, ci, dst):
                    t1, t2 = t1_r.next(), t2_r.next()
                    S.dve(lambda e: e.tensor_tensor(out=t1.t[lo:hi, 0:n], in0=pa.t[lo:hi, 0:n], in1=rp.t[lo:hi, ci, 0:n], op=ALU.mult), reads=[pa.b, rp.b], writes=[t1.b])
                    S.dve(lambda e: e.tensor_tensor(out=t2.t[lo:hi, 0:n], in0=pb2.t[lo:hi, 0:n], in1=rp.t[lo:hi, ci + 1, 0:n], op=ALU.mult), reads=[pb2.b, rp.b], writes=[t2.b])
                    S.pool(lambda e: e.tensor_tensor(out=dst.t[lo:hi, 0:n], in0=t1.t[lo:hi, 0:n], in1=t2.t[lo:hi, 0:n], op=ALU.add), reads=[t1.b, t2.b], writes=[dst.b])

                for h in range(8):
                    pa = bk.next()
                    mm_acc(pa.t[0:96, 0:n], pa.b, [(wuq.t[:, k, h * 96:(h + 1) * 96], cn.t[:, k, 0:n]) for k in range(3)], [wuq.b, cn.b])
                    ob = ob_r.next()
                    if is_ctx:
                        S.act(lambda e, ob=ob, pa=pa: e.activation(out=ob.t[0:96, 0:n], in_=pa.t[0:96, 0:n], func=AF.Copy), reads=[pa.b], writes=[ob.b])
                    else:
                        pb2 = bk.next()
                        mm_acc(pb2.t[64:96, 0:n], pb2.b, [(wuqp.t[:, k, h * 32:(h + 1) * 32], cn.t[:, k, 0:n]) for k in range(3)], [wuqp.b, cn.b])
                        S.act(lambda e, ob=ob, pa=pa: e.activation(out=ob.t[0:64, 0:n], in_=pa.t[0:64, 0:n], func=AF.Copy), reads=[pa.b], writes=[ob.b])
                        rope_out(pa, pb2, 64, 96, 0, ob)
                    S.dma(lambda e, ob=ob, h=h: e.dma_start(out=qA_d[h, :, tok0:tok0 + n], in_=ob.t[0:96, 0:n]), reads=[ob.b], writes=[ph.db(("qA", h))])
                pa = bk.next()
                mm_acc(pa.t[64:96, 0:n], pa.b, [(win.t[:, k, 640:672], hT.t[:, k, 0:n]) for k in range(8)], [win.b, hT.b])
                ob = ob_r.next()
                if is_ctx:
                    S.act(lambda e, ob=ob, pa=pa: e.activation(out=ob.t[64:96, 0:n], in_=pa.t[64:96, 0:n], func=AF.Copy), reads=[pa.b], writes=[ob.b])
                else:
                    pb2 = bk.next()
                    mm_acc(pb2.t[64:96, 0:n], pb2.b, [(winp.t[:, k, 0:32], hT.t[:, k, 0:n]) for k in range(8)], [winp.b, hT.b])
                    rope_out(pa, pb2, 64, 96, 0, ob)
                S.dma(lambda e, ob=ob: e.dma_start(out=kAr_d[:, tok0:tok0 + n], in_=ob.t[64:96, 0:n]), reads=[ob.b], writes=[ph.db("kAr")])
                for hp in range(4):
                    pa = bk.next()
                    mm_acc(pa.t[:, 0:n], pa.b, [(wukv.t[:, k, hp * 128:(hp + 1) * 128], cn.t[:, 3 + k, 0:n]) for k in range(2)], [wukv.b, cn.b])
                    ob = ob_r.next()
                    S.act(lambda e, ob=ob, pa=pa: e.activation(out=ob.t[:, 0:n], in_=pa.t[:, 0:n], func=AF.Copy), reads=[pa.b], writes=[ob.b])
                    S.dma(lambda e, ob=ob, hp=hp: e.dma_start(out=kAn_d[hp * 128:(hp + 1) * 128, tok0:tok0 + n], in_=ob.t[:, 0:n]), reads=[ob.b], writes=[ph.db(("kAn", hp))])
                for j in range(n // 128):
                    ts = slice(j * 128, (j + 1) * 128)
                    r0 = tok0 + j * 128
                    pa = bk.next()
                    mm_acc(pa.t[:, :], pa.b, [(cn.t[:, 3 + k, ts], wukv.t[:, k, 512:1024]) for k in range(2)], [wukv.b, cn.b])
                    ob = ob_r.next()
                    S.act(lambda e, ob=ob, pa=pa: e.activation(out=ob.t[:, :], in_=pa.t[:, :], func=AF.Copy), reads=[pa.b], writes=[ob.b])
                    S.dma(lambda e, ob=ob, r0=r0: e.dma_start(out=vA_d[r0:r0 + 128, :], in_=ob.t[:, :]), reads=[ob.b], writes=[ph.db("vA")])
                    pa = bk.next()
                    mm_acc(pa.t[:, :], pa.b, [(hT.t[:, k, ts], win.t[:, k, 1696:2208]) for k in range(8)], [win.b, hT.b])
                    ob = ob_r.next()
                    S.act(lambda e, ob=ob, pa=pa: e.activation(out=ob.t[:, :], in_=pa.t[:, :], func=AF.Copy), reads=[pa.b], writes=[ob.b])
                    S.dma(lambda e, ob=ob, r0=r0: e.dma_start(out=vB_d[r0:r0 + 128, :], in_=ob.t[:, :]), reads=[ob.b], writes=[ph.db("vB")])
                for (c0, cp0, dst_d, nm) in ((672, 32, qB_d, "qB"), (1184, 544, kB_d, "kB")):
                    for h in range(4):
                        pa = bk.next()
                        mm_acc(pa.t[:, 0:n], pa.b, [(win.t[:, k, c0 + h * 128:c0 + (h + 1) * 128], hT.t[:, k, 0:n]) for k in range(8)], [win.b, hT.b])
                        ob = ob_r.next()
                        if is_ctx:
                            S.act(lambda e, ob=ob, pa=pa: e.activation(out=ob.t[:, 0:n], in_=pa.t[:, 0:n], func=AF.Copy), reads=[pa.b], writes=[ob.b])
                        else:
                            pb2 = bk.next()
                            mm_acc(pb2.t[:, 0:n], pb2.b, [(winp.t[:, k, cp0 + h * 128:cp0 + (h + 1) * 128], hT.t[:, k, 0:n]) for k in range(8)], [winp.b, hT.b])
                            rope_out(pa, pb2, 0, 128, 2, ob)
                        S.dma(lambda e, ob=ob, h=h, dst_d=dst_d: e.dma_start(out=dst_d[h, :, tok0:tok0 + n], in_=ob.t[:, 0:n]), reads=[ob.b], writes=[ph.db((nm, h))])
            ph.close()

        pc_jobs = [(i, g4) for g4 in range(22) for i in range(3)]
        pc_srcs = (mw1_d, mw3_d, mw2_d)

        def precast_job(ph, pc_r, after_buf):
            if not pc_jobs:
                return
            i, g4 = pc_jobs.pop(0)
            pc = pc_r.next()
            rows = slice(g4 * 512, (g4 + 1) * 512)
            S.dma(lambda e: e.dma_start(out=pc.t[:], in_=pc_srcs[i][rows, :].rearrange("(a p) n -> p a n", p=128)), reads=[after_buf], writes=[pc.b], q="pool", lat=40.0)
            S.dma(lambda e: e.dma_start(out=wb_d[i][rows, :].rearrange("(a p) n -> p a n", p=128), in_=pc.t[:]), reads=[pc.b], writes=[ph.db(("wb", i, g4))])

        if upto >= 2:
            ph = Phase(reorder=True)
            KT_r = ph.rot(2, [128, NTOK], BF16, "KT")
            QT_r = ph.rot(2, [128, NTOK], BF16, "QT")
            VA_r = ph.rot(2, [128, NT, 128], BF16, "VA")
            QZ_r = [ph.rot(2, [128, NTOK], BF16, "QZ0"), ph.rot(2, [128, NTOK], BF16, "QZ1")]
            for m_ in range(2):
                for qz in QZ_r[m_].items:
                    zl = 64 * (1 - m_)
                    S.pool(lambda e, qz=qz, zl=zl: e.memset(qz.t[zl:zl + 64, :], 0.0), writes=[qz.b])
            PT_r = ph.rot(6, [128, 512], BF16, "PT")
            acc_r = ph.rot(2, [128, 512], F32, "acc")
            pr_r = ph.rot(3, [128, 512], BF16, "pr")
            hl_r = ph.rot(4, [128, 512], BF16, "hl")
            rc_r = ph.rot(2, [128, 512], F32, "rc")
            o1_r = ph.rot(2, [128, 512], F32, "o1")
            o2_r = ph.rot(2, [128, 512], F32, "o2")
            od_r = ph.rot(2, [128, 512], F32, "od")
            sq_r = ph.rot(2, [128, 512], BF16, "sq2")
            rs_r = ph.rot(2, [128, 512], F32, "rs2")
            on_r = ph.rot(3, [128, 512], BF16, "on")
            lamt = ph.sb([128, 256], F32, "lamt")
            lam2 = ph.sb([128, 128], F32, "lam2")
            lst = ph.sb([128, 8], F32, "lst")
            gsub = ph.sb([128, 1], F32, "gsub")
            Sb = Rot(ph.banks[0:4])
            Ob = Rot(ph.banks[4:6])
            Mb = Rot(ph.banks[6:7])
            ssb = ph.banks[7]
            S.pool(lambda e: e.memset(VA_r.items[0].t[:, :, 64:128], 1.0), writes=[VA_r.items[0].b])
            S.pool(lambda e: e.memset(VA_r.items[1].t[:, :, 0:64], 1.0), writes=[VA_r.items[1].b])
            S.dma(lambda e: e.dma_start(out=lamt.t[:], in_=lam_d.partition_broadcast(128)), writes=[lamt.b])
            S.dma(lambda e: e.dma_start(out=gsub.t[:], in_=subln_d), writes=[gsub.b])
            S.dve(lambda e: e.memset(lst.t[:], 0.0), writes=[lst.b])
            S.dve(lambda e: e.tensor_tensor(out=lam2.t[:, 0:64], in0=lamt.t[:, 0:64], in1=lamt.t[:, 64:128], op=ALU.mult), reads=[lamt.b], writes=[lam2.b])
            S.dve(lambda e: e.tensor_tensor(out=lam2.t[:, 64:128], in0=lamt.t[:, 128:192], in1=lamt.t[:, 192:256], op=ALU.mult), reads=[lamt.b], writes=[lam2.b])
            S.dve(lambda e: e.tensor_reduce(out=lst.t[:, 0:2], in_=lam2.t[:].rearrange("p (a d) -> p a d", a=2), axis=mybir.AxisListType.X, op=ALU.add), reads=[lam2.b], writes=[lst.b])
            S.act(lambda e: e.activation(out=lst.t[:, 2:4], in_=lst.t[:, 0:2], func=AF.Exp), reads=[lst.b], writes=[lst.b])
            S.dve(lambda e: e.tensor_tensor(out=lst.t[:, 4:5], in0=lst.t[:, 3:4], in1=lst.t[:, 2:3], op=ALU.subtract), reads=[lst.b], writes=[lst.b])
            S.dve(lambda e: e.tensor_scalar(out=lst.t[:, 5:6], in0=lst.t[:, 4:5], scalar1=-LAM_INIT, scalar2=None, op0=ALU.add), reads=[lst.b], writes=[lst.b])
            S.dve(lambda e: e.tensor_scalar(out=gsub.t[:], in0=gsub.t[:], scalar1=(1.0 - LAM_INIT), scalar2=None, op0=ALU.mult), reads=[gsub.b], writes=[gsub.b])

            pc_r = ph.rot(3, [128, 4, 2048], BF16, "pc")

            def precast_step(after_buf):
                precast_job(ph, pc_r, after_buf)

            def attend(KT, klo, khi, QT, tok0, n, kts, scale, pv_fn):
                nk = len(kts)
                pts = [None] * nk

                def qk(i):
                    kt = kts[i]
                    sb = Sb.next()
                    S.pe(lambda e: e.matmul(sb.t[:, 0:n], lhsT=KT.t[klo:khi, kt * 128:(kt + 1) * 128], rhs=QT.t[klo:khi, tok0:tok0 + n], start=True, stop=True),
                         reads=[KT.b, QT.b], writes=[sb.b], cost=0.22)
                    pt = PT_r.next()
                    S.act(lambda e: e.activation(out=pt.t[:, 0:n], in_=sb.t[:, 0:n], func=AF.Exp, scale=scale), reads=[sb.b], writes=[pt.b], cost=0.5)
                    pts[i] = pt
                LA = 2
                for i in range(min(LA, nk)):
                    qk(i)
                for i in range(nk):
                    if i + LA < nk:
                        qk(i + LA)
                    pv_fn(kts[i], pts[i], i == 0, i == nk - 1)

            def load_vtiles(VA, c0, c1, src):
                for (t0, t1) in ((0, 17), (17, 34)):
                    S.dma(lambda e: e.dma_start(out=VA.t[:, t0:t1, c0:c1], in_=src[t0 * 128:t1 * 128, :].rearrange("(t p) d -> p t d", p=128)), writes=[VA.b])

            def mla_head(h):
                KT, QT, VA = KT_r.next(), QT_r.next(), VA_r.next()
                odd = h % 2
                S.dma(lambda e: e.dma_start(out=KT.t[0:64, :], in_=kAn_d[h * 64:(h + 1) * 64, :]), writes=[KT.b])
                S.dma(lambda e: e.dma_start(out=KT.t[64:96, :], in_=kAr_d[:, :]), writes=[KT.b])
                S.dma(lambda e: e.dma_start(out=QT.t[0:96, :], in_=qA_d[h, :, :]), writes=[QT.b])
                load_vtiles(VA, 64 * odd, 64 * odd + 64, vA_d[:, h * 64:(h + 1) * 64])
                olo, slo = (64, 0) if odd else (0, 64)
                for (tok0, n, is_ctx) in CHUNKS:
                    kts = [0, 1] if is_ctx else list(range(NT))
                    ob_ = Ob.next()

                    def pv(kt, pt, first, last, ob_=ob_, n=n):
                        S.pe(lambda e: e.matmul(ob_.t[:, 0:n], lhsT=VA.t[:, kt, :], rhs=pt.t[:, 0:n], start=first, stop=last), reads=[VA.b, pt.b], writes=[ob_.b], cost=0.38)
                    attend(KT, 0, 96, QT, tok0, n, kts, MLA_SCALE, pv)
                    rc, on = rc_r.next(), on_r.next()
                    S.dve(lambda e: e.reciprocal(out=rc.t[olo:olo + 64, 0:n], in_=ob_.t[slo:slo + 64, 0:n]), reads=[ob_.b], writes=[rc.b], cost=3.4)
                    S.dve(lambda e: e.tensor_tensor(out=on.t[olo:olo + 64, 0:n], in0=ob_.t[olo:olo + 64, 0:n], in1=rc.t[olo:olo + 64, 0:n], op=ALU.mult),
                          reads=[ob_.b, rc.b], writes=[on.b])
                    S.dma(lambda e: e.dma_start(out=oT_d[olo:olo + 64, h // 2, tok0:tok0 + n], in_=on.t[olo:olo + 64, 0:n]), reads=[on.b], writes=[ph.db(("oT", h))])

            def diff_head(h):
                KT, VA = KT_r.next(), VA_r.next()
                QZ = [QZ_r[0].next(), QZ_r[1].next()]
                S.dma(lambda e: e.dma_start(out=KT.t[:, :], in_=kB_d[h, :, :]), writes=[KT.b])
                S.dma(lambda e: e.dma_start(out=QZ[0].t[0:64, :], in_=qB_d[h, 0:64, :]), writes=[QZ[0].b])
                S.dma(lambda e: e.dma_start(out=QZ[1].t[64:128, :], in_=qB_d[h, 64:128, :]), writes=[QZ[1].b])
                load_vtiles(VA, 0, 128, vB_d[:, h * 128:(h + 1) * 128])
                for (tok0, n, is_ctx) in CHUNKS:
                    kts = [0, 1] if is_ctx else list(range(NT))
                    om = []
                    for m in range(2):
                        ob_, mb_, acc = Ob.next(), Mb.next(), acc_r.next()
                        stt_ = {"prev": None, "n": 0}

                        def pv(kt, pt, first, last, ob_=ob_, acc=acc, n=n, stt_=stt_):
                            S.pe(lambda e: e.matmul(ob_.t[:, 0:n], lhsT=VA.t[:, kt, :], rhs=pt.t[:, 0:n], start=first, stop=last), reads=[VA.b, pt.b], writes=[ob_.b], cost=0.38)
                            if stt_["prev"] is None and not last:
                                stt_["prev"] = pt
                                return
                            src = pt
                            if stt_["prev"] is not None:
                                pp_ = stt_["prev"]
                                stt_["prev"] = None
                                pr_ = pr_r.next()
                                S.dve(lambda e: e.tensor_tensor(out=pr_.t[:, 0:n], in0=pp_.t[:, 0:n], in1=pt.t[:, 0:n], op=ALU.add), reads=[pp_.b, pt.b], writes=[pr_.b], cost=0.3)
                                src = pr_
                            if stt_["n"] == 0:
                                S.dve(lambda e: e.tensor_copy(out=acc.t[:, 0:n], in_=src.t[:, 0:n]), reads=[src.b], writes=[acc.b])
                            else:
                                S.dve(lambda e: e.tensor_tensor(out=acc.t[:, 0:n], in0=acc.t[:, 0:n], in1=src.t[:, 0:n], op=ALU.add), reads=[src.b, acc.b], writes=[acc.b])
                            stt_["n"] += 1
                        attend(KT, 0, 128, QZ[m], tok0, n, kts, DIFF_SCALE, pv)
                        hi, lo = hl_r.next(), hl_r.next()
                        S.pool(lambda e: e.tensor_copy(out=hi.t[:, 0:n], in_=acc.t[:, 0:n]), reads=[acc.b], writes=[hi.b])
                        S.pool(lambda e: e.tensor_tensor(out=lo.t[:, 0:n], in0=acc.t[:, 0:n], in1=hi.t[:, 0:n], op=ALU.subtract), reads=[acc.b, hi.b], writes=[lo.b])
                        S.pe(lambda e: e.matmul(mb_.t[:, 0:n], lhsT=ph.ones_b.t[:], rhs=hi.t[:, 0:n], start=True, stop=False), reads=[ph.ones_b.b, hi.b], writes=[mb_.b])
                        S.pe(lambda e: e.matmul(mb_.t[:, 0:n], lhsT=ph.ones_b.t[:], rhs=lo.t[:, 0:n], start=False, stop=True), reads=[ph.ones_b.b, lo.b], writes=[mb_.b])
                        rc = rc_r.next()
                        o_ = (o1_r if m == 0 else o2_r).next()
                        S.act(lambda e: e.activation(out=rc.t[:, 0:n], in_=mb_.t[:, 0:n], func=AF.Ln), reads=[mb_.b], writes=[rc.b])
                        S.act(lambda e: e.activation(out=rc.t[:, 0:n], in_=rc.t[:, 0:n], func=AF.Exp, scale=-1.0), reads=[rc.b], writes=[rc.b])
                        S.dve(lambda e: e.tensor_tensor(out=o_.t[:, 0:n], in0=ob_.t[:, 0:n], in1=rc.t[:, 0:n], op=ALU.mult), reads=[ob_.b, rc.b], writes=[o_.b])
                        om.append(o_)
                    od, sq, rs, on = od_r.next(), sq_r.next(), rs_r.next(), on_r.next()
                    oa, obb = om[0], om[1]
                    S.dve(lambda e: e.scalar_tensor_tensor(out=od.t[:, 0:n], in0=obb.t[:, 0:n], scalar=lst.t[:, 5:6], in1=oa.t[:, 0:n], op0=ALU.mult, op1=ALU.add),
                           reads=[oa.b, obb.b, lst.b], writes=[od.b])
                    S.pool(lambda e: e.tensor_tensor(out=sq.t[:, 0:n], in0=od.t[:, 0:n], in1=od.t[:, 0:n], op=ALU.mult), reads=[od.b], writes=[sq.b])
                    S.pe(lambda e: e.matmul(ssb.t[:, 0:n], lhsT=ph.ones_b.t[:], rhs=sq.t[:, 0:n], start=True, stop=True), reads=[sq.b, ph.ones_b.b], writes=[ssb.b])
                    S.dve(lambda e: e.tensor_scalar(out=rs.t[:, 0:n], in0=ssb.t[:, 0:n], scalar1=1.0 / 128, scalar2=EPS, op0=ALU.mult, op1=ALU.add), reads=[ssb.b], writes=[rs.b])
                    S.act(lambda e: e.activation(out=rs.t[:, 0:n], in_=rs.t[:, 0:n], func=AF.Ln), reads=[rs.b], writes=[rs.b])
                    S.act(lambda e: e.activation(out=rs.t[:, 0:n], in_=rs.t[:, 0:n], func=AF.Exp, scale=-0.5), reads=[rs.b], writes=[rs.b])
                    S.dve(lambda e: e.scalar_tensor_tensor(out=on.t[:, 0:n], in0=od.t[:, 0:n], scalar=gsub.t[:, 0:1], in1=rs.t[:, 0:n], op0=ALU.mult, op1=ALU.mult),
                          reads=[od.b, gsub.b, rs.b], writes=[on.b])
                    S.dma(lambda e: e.dma_start(out=oT_d[:, 4 + h, tok0:tok0 + n], in_=on.t[:, 0:n]), reads=[on.b], writes=[ph.db(("oT", 8 + h))])
                    precast_step(on.b)
                    if (tok0 // 512) % 3 == 1:
                        precast_step(od.b)

            for h in range(8):
                mla_head(h)
            for h in range(4):
                diff_head(h)
            ph.close()

        if upto >= 3:
            ph = Phase(reorder=True)
            wo = ph.sb([128, 8, D], BF16, "wo")
            S.dma(lambda e: e.dma_start(out=wo.t[:], in_=wo_d.rearrange("(c p) n -> p c n", p=128)), writes=[wo.b], q="pool")
            sublayer_tail(S, ph, load_bc, NormCtx, norm_mod_T, rstd_from_ss, 0, tok_src, res_ap, hT_d, oT_d, wo, None, range(NT), None)
            ph.close()

        if upto >= 4:
            ph = Phase(reorder=True)
            groups = [(0, 256)] + [(256 + 1024 * i, 1024) for i in range(4)]
            ffn_phase(S, ph, load_bc, rstd_from_ss, 0, groups, res_ap, hT_d, [(fw1_d, fw3_d, fw2_d[0])], None)
            ph.close()

        if upto >= 5:
            ph = Phase(reorder=True)
            nx = NormCtx(ph)
            A0 = [load_bc(ph, 1, w, 0) for w in (0, 1)]
            SH0 = [load_bc(ph, 1, w, 1) for w in (0, 1)]
            wqkv = ph.sb([128, 8, 3 * D], BF16, "wqkv")
            wv = wqkv_d.rearrange("(k p) n -> p k n", p=128)
            for k in range(8):
                S.dma(lambda e, k=k: e.dma_start(out=wqkv.t[:, k, :], in_=wv[:, k, :]), writes=[wqkv.b], q="pool")
            bqk = ph.sb([128, 16], F32, "bqk")
            bvb = ph.sb([1, D], BF16, "bvb")
            S.dma(lambda e: e.dma_start(out=bqk.t[:], in_=bqk_d), writes=[bqk.b])
            S.dma(lambda e: e.dma_start(out=bvb.t[:], in_=bv_d), writes=[bvb.b], q="pool")
            xt_r = ph.rot(2, [128, D], F32, "xt5")
            hT_r = ph.rot(2, [128, 8, 512], BF16, "hT5")
            ob_r = ph.rot(4, [128, 512], BF16, "ob5")
            bk = Rot(ph.banks[0:7])
            tb = ph.banks[7]

            def p5_chunk(tok0, n, is_ctx):
                which = 1 if is_ctx else 0
                hT = hT_r.next()
                for j in range(n // 128):
                    t = tok0 // 128 + j
                    xt = xt_r.next()
                    S.dma(lambda e: e.dma_start(out=xt.t[:], in_=res_ap(t)), writes=[xt.b])
                    norm_mod_T(ph, nx, xt, A0[which], SH0[which], hT.t[:, :, j * 128:(j + 1) * 128], hT.b, tb)
                for blk in range(16):
                    pa = bk.next()
                    mm_acc(pa.t[:, 0:n], pa.b, [(wqkv.t[:, k, blk * 128:(blk + 1) * 128], hT.t[:, k, 0:n]) for k in range(8)], [wqkv.b, hT.b])
                    ob = ob_r.next()
                    S.act(lambda e: e.activation(out=ob.t[:, 0:n], in_=pa.t[:, 0:n], func=AF.Identity, bias=bqk.t[:, blk:blk + 1]), reads=[pa.b, bqk.b], writes=[ob.b])
                    dst = qC_d[blk] if blk < 8 else kC_d[blk - 8]
                    S.dma(lambda e: e.dma_start(out=dst[:, tok0:tok0 + n], in_=ob.t[:, 0:n]), reads=[ob.b], writes=[ph.db(("qk", blk))])
                for j in range(n // 128):
                    ts = slice(j * 128, (j + 1) * 128)
                    r0 = tok0 + j * 128
                    for half in range(2):
                        c0 = 2048 + half * 512
                        pa = bk.next()
                        mm_acc(pa.t[:, :], pa.b, [(hT.t[:, k, ts], wqkv.t[:, k, c0:c0 + 512]) for k in range(8)]
                               + [(ph.ones_b.t[0:1, 0:128], bvb.t[0:1, half * 512:(half + 1) * 512])], [wqkv.b, hT.b, bvb.b, ph.ones_b.b])
                        ob = ob_r.next()
                        S.act(lambda e: e.activation(out=ob.t[:, :], in_=pa.t[:, :], func=AF.Copy), reads=[pa.b], writes=[ob.b])
                        S.dma(lambda e: e.dma_start(out=vC_d[r0:r0 + 128, half * 512:(half + 1) * 512], in_=ob.t[:, :]), reads=[ob.b], writes=[ph.db("vC")])
            for (tok0, n, is_ctx) in CHUNKS:
                p5_chunk(tok0, n, is_ctx)
            ph.close()

        if upto >= 6:
            ph = Phase(reorder=True)
            KT_r = ph.rot(2, [128, NTOK], BF16, "KT6")
            VA_r = ph.rot(2, [128, NT, 2, 128], BF16, "VA6")
            pc6_r = ph.rot(2, [128, 4, 2048], BF16, "pc6")
            EB_r = ph.rot(2, [128, 2, NCLS, 640], BF16, "EB")
            rp_r = ph.rot(2, [128, ND, 128], F32, "rp")
            eb_t = ph.rot(3, [128, 128], F32, "ebt")
            nm = ph.sb([128, NVAR, 128], F32, "nm")
            PT_r = ph.rot(3, [128, 1024], BF16, "PT6")
            rc_r = ph.rot(2, [128, 128], F32, "rc6")
            on_r = ph.rot(3, [128, 128], BF16, "on6")
            QZ_r = [ph.rot(2, [128, NTOK], BF16, "QZa"), ph.rot(2, [128, NTOK], BF16, "QZb")]
            for m_ in range(2):
                for qz in QZ_r[m_].items:
                    zl = 64 * (1 - m_)
                    S.pool(lambda e, qz=qz, zl=zl: e.memset(qz.t[zl:zl + 64, :], 0.0), writes=[qz.b])
            Sp = Rot(ph.pairs[0:3])
            Ob = Rot(ph.banks[6:8])
            S.dma(lambda e: e.dma_start(out=nm.t[:], in_=nmask_d.rearrange("v k q -> k v q")), writes=[nm.b])
            for va in VA_r.items:
                S.pool(lambda e, va=va: e.memset(va.t[:, :, 0, 64:128], 1.0), writes=[va.b])
                S.pool(lambda e, va=va: e.memset(va.t[:, :, 1, 0:64], 1.0), writes=[va.b])

            def na_pair(hp):
                KT, VA, EB = KT_r.next(), VA_r.next(), EB_r.next()
                QZ = [QZ_r[0].next(), QZ_r[1].next()]
                S.dma(lambda e: e.dma_start(out=KT.t[:, :], in_=kC_d[hp, :, :]), writes=[KT.b])
                S.dma(lambda e: e.dma_start(out=QZ[0].t[0:64, :], in_=qC_d[hp, 0:64, :]), writes=[QZ[0].b])
                S.dma(lambda e: e.dma_start(out=QZ[1].t[64:128, :], in_=qC_d[hp, 64:128, :]), writes=[QZ[1].b])
                for m in range(2):
                    hh = 2 * hp + m
                    c0 = 64 * m
                    for (t0, t1) in ((0, 17), (17, 34)):
                        S.dma(lambda e, m=m, t0=t0, t1=t1, hh=hh, c0=c0: e.dma_start(out=VA.t[:, t0:t1, m, c0:c0 + 64],
                                                                                     in_=vC_d[t0 * 128:t1 * 128, hh * 64:(hh + 1) * 64].rearrange("(t p) d -> p t d", p=128)), writes=[VA.b])
                    rp = rp_r.next()
                    S.dma(lambda e, rp=rp, hh=hh: e.dma_start(out=rp.t[:], in_=rpbg_d[hh].rearrange("d k q -> k d q")), writes=[rp.b])
                    for ci_, cl in enumerate(NA_CLASSES):
                        for j, cmb in enumerate(cl):
                            var, di = NA_COMBOS[cmb]
                            tt = eb_t.next()
                            S.dve(lambda e, tt=tt, var=var, di=di, rp=rp: e.tensor_tensor(out=tt.t[:], in0=rp.t[:, di, :], in1=nm.t[:, var, :], op=ALU.add), reads=[rp.b, nm.b], writes=[tt.b])
                            S.act(lambda e, tt=tt, m=m, ci_=ci_, j=j: e.activation(out=EB.t[:, m, ci_, j * 128:(j + 1) * 128], in_=tt.t[:], func=AF.Exp), reads=[tt.b], writes=[EB.b])
                na_tiles(hp, KT, QZ, VA, EB)

            def na_stage_a(hp, i, m, KT, QZ, EB):
                q0 = (2 + i) * 128
                wk = [kt for kt in range(32) if (i, kt) in NA_CTAB]
                sp = Sp.next()
                slots = []
                tiles_ = [2 + kt for kt in wk] + [0, 1]
                for j, tile in enumerate(tiles_):
                    sl_ = slice(j * 128, (j + 1) * 128)
                    S.pe(lambda e, tile=tile, sl_=sl_: e.matmul(sp.t[:, sl_], lhsT=KT.t[:, tile * 128:(tile + 1) * 128], rhs=QZ[m].t[:, q0:q0 + 128], start=True, stop=True),
                         reads=[KT.b, QZ[m].b], writes=[sp.b], cost=0.06)
                    slots.append((sl_, tile))
                ns = len(slots)
                nw = len(wk)
                pt = PT_r.next()
                S.act(lambda e: e.activation(out=pt.t[:, 0:512], in_=sp.t[:, 0:512], func=AF.Exp, scale=NA_SCALE), reads=[sp.b], writes=[pt.b], cost=0.5)
                if ns > 4:
                    S.act(lambda e: e.activation(out=pt.t[:, 512:ns * 128], in_=sp.t[:, 512:ns * 128], func=AF.Exp, scale=NA_SCALE), reads=[sp.b], writes=[pt.b], cost=0.45)
                cls = NA_CLS_OF_I[i]
                S.dve(lambda e: e.tensor_tensor(out=pt.t[:, 0:nw * 128], in0=pt.t[:, 0:nw * 128], in1=EB.t[:, m, cls, 0:nw * 128], op=ALU.mult), reads=[pt.b, EB.b], writes=[pt.b], cost=0.45)
                return (pt, slots)

            def na_stage_b(hp, i, m, VA, st, on):
                pt, slots = st
                ns = len(slots)
                q0 = (2 + i) * 128
                ob_ = Ob.next()
                for j, (sl_, tile) in enumerate(slots):
                    S.pe(lambda e, j=j, sl_=sl_, tile=tile: e.matmul(ob_.t[:, 0:128], lhsT=VA.t[:, tile, m, :], rhs=pt.t[:, sl_], start=(j == 0), stop=(j == ns - 1)),
                         reads=[VA.b, pt.b], writes=[ob_.b], cost=0.06)
                rc = rc_r.next()
                olo, slo = (64, 0) if m else (0, 64)
                if m == 0:
                    S.dve(lambda e: e.reciprocal(out=rc.t[olo:olo + 64, :], in_=ob_.t[slo:slo + 64, 0:128]), reads=[ob_.b], writes=[rc.b], cost=0.9)
                else:
                    S.act(lambda e: e.activation(out=rc.t[olo:olo + 64, :], in_=ob_.t[slo:slo + 64, 0:128], func=AF.Ln), reads=[ob_.b], writes=[rc.b], cost=0.27)
                    S.act(lambda e: e.activation(out=rc.t[olo:olo + 64, :], in_=rc.t[olo:olo + 64, :], func=AF.Exp, scale=-1.0), reads=[rc.b], writes=[rc.b], cost=0.27)
                S.dve(lambda e: e.tensor_tensor(out=on.t[olo:olo + 64, :], in0=ob_.t[olo:olo + 64, 0:128], in1=rc.t[olo:olo + 64, :], op=ALU.mult), reads=[ob_.b, rc.b], writes=[on.b], cost=0.3)
                if m == 1:
                    S.dma(lambda e: e.dma_start(out=oT_d[:, hp, q0:q0 + 128], in_=on.t[:, :]), reads=[on.b], writes=[ph.db(("oT", hp))])
                    if i % 8 == 7:
                        precast_job(ph, pc6_r, on.b)

            def na_tiles(hp, KT, QZ, VA, EB):
                work = [(i, m) for i in range(32) for m in range(2)]
                ons = {}
                st = na_stage_a(hp, 0, 0, KT, QZ, EB)
                for idx, (i, m) in enumerate(work):
                    nxt = None
                    if idx + 1 < len(work):
                        ni, nm_ = work[idx + 1]
                        nxt = na_stage_a(hp, ni, nm_, KT, QZ, EB)
                    if m == 0:
                        ons[i] = on_r.next()
                    na_stage_b(hp, i, m, VA, st, ons[i])
                    st = nxt

            for hp in range(8):
                na_pair(hp)
            assert not pc_jobs, len(pc_jobs)
            ph.close()

        if upto >= 7:
            ph = Phase(reorder=True)
            wo = ph.sb([128, 8, D], BF16, "wo7")
            S.dma(lambda e: e.dma_start(out=wo.t[:], in_=cwo_d.rearrange("(c p) n -> p c n", p=128)), writes=[wo.b], q="pool")
            cbo = ph.sb([1, D], BF16, "cbo")
            S.dma(lambda e: e.dma_start(out=cbo.t[:], in_=cbo_d), writes=[cbo.b], q="pool")
            rt = ph.sb([128, 8, NEXP], BF16, "rt")
            S.dma(lambda e: e.dma_start(out=rt.t[:], in_=rt_d.rearrange("(k p) n -> p k n", p=128)), writes=[rt.b], q="pool")
            lg_r = ph.rot(2, [128, 8], F32, "lg")
            m8_r = ph.rot(2, [128, 8], F32, "m8")
            w8_r = ph.rot(2, [128, 32], F32, "w8")
            lgb = ph.banks[6]

            def router(t, h2):
                lg, m8, w8 = lg_r.next(), m8_r.next(), w8_r.next()
                for k in range(8):
                    S.pe(lambda e, k=k: e.matmul(lgb.t[:, 0:NEXP], lhsT=h2.t[:, k, :], rhs=rt.t[:, k, :], start=(k == 0), stop=(k == 7)), reads=[h2.b, rt.b], writes=[lgb.b])
                S.dve(lambda e: e.tensor_copy(out=lg.t[:], in_=lgb.t[:, 0:NEXP]), reads=[lgb.b], writes=[lg.b])
                S.dve(lambda e: e.max(out=m8.t[:], in_=lg.t[:]), reads=[lg.b], writes=[m8.b])
                S.dve(lambda e: e.tensor_scalar(out=w8.t[:, 0:8], in0=lg.t[:], scalar1=m8.t[:, 1:2], scalar2=None, op0=ALU.is_ge), reads=[lg.b, m8.b], writes=[w8.b])
                S.dve(lambda e: e.tensor_scalar(out=w8.t[:, 24:25], in0=m8.t[:, 0:1], scalar1=-1.0, scalar2=None, op0=ALU.mult), reads=[m8.b], writes=[w8.b])
                S.act(lambda e: e.activation(out=w8.t[:, 8:16], in_=lg.t[:], func=AF.Exp, bias=w8.t[:, 24:25]), reads=[lg.b, w8.b], writes=[w8.b])
                S.dve(lambda e: e.tensor_tensor(out=w8.t[:, 16:24], in0=w8.t[:, 8:16], in1=w8.t[:, 0:8], op=ALU.mult), reads=[w8.b], writes=[w8.b])
                S.dve(lambda e: e.tensor_reduce(out=w8.t[:, 25:26], in_=w8.t[:, 16:24], axis=mybir.AxisListType.X, op=ALU.add), reads=[w8.b], writes=[w8.b])
                S.dve(lambda e: e.reciprocal(out=w8.t[:, 26:27], in_=w8.t[:, 25:26]), reads=[w8.b], writes=[w8.b])
                S.dve(lambda e: e.tensor_scalar(out=lg.t[:], in0=w8.t[:, 16:24], scalar1=w8.t[:, 26:27], scalar2=None, op0=ALU.mult), reads=[w8.b, lg.b], writes=[lg.b])
                S.dma(lambda e: e.dma_start(out=comb_d[:, t - 2, :], in_=lg.t[:]), reads=[lg.b], writes=[ph.db(("comb", t))])

            sublayer_tail(S, ph, load_bc, NormCtx, norm_mod_T, rstd_from_ss, 1, res_ap, res_ap, hT_d, oT_d, wo, cbo, range(2, NT), router, h4_d=h4_d)
            ph.close()

        if upto >= 8:
            ph = Phase(reorder=True)
            G3 = load_bc(ph, 1, 0, 5)
            rc_ = ph.sb([128, 161], F32, "rconst")
            S.dma(lambda e: e.dma_start(out=rc_.t[:], in_=rconst_d), writes=[rc_.b])
            tri_b = ph.sb([128, 128], BF16, "tri")
            S.dma(lambda e: e.dma_start(out=tri_b.t[:], in_=rconst_d[:, 0:128]), writes=[tri_b.b], q="pool")
            thr = rc_.t[:, 128:136]
            svals = rc_.t[:, 136:136 + NSLOT]
            pcol = rc_.t[:, 160:161]
            comb = ph.sb([128, 32, NEXP], F32, "comb_sb")
            S.dma(lambda e: e.dma_start(out=comb.t[:], in_=comb_d), writes=[comb.b])
            sel = ph.sb([128, 32, NEXP], F32, "sel")
            selb = ph.sb([128, 256], BF16, "selb")
            tot = ph.sb([128, 32, NEXP], F32, "tot")
            tp = ph.sb([128, 32, NEXP], F32, "tp")
            pos = ph.sb([128, 32, NEXP], F32, "pos")
            Mt = ph.sb([128, 32, NEXP], F32, "Mt")
            eq = ph.sb([128, 32, NEXP], F32, "eq")
            sm8 = ph.sb([128, 64], F32, "sm8")
            cmp8 = ph.sb([128, 8, 8], F32, "cmp8")
            r32 = ph.sb([128, 8, 32], F32, "r32")
            pA_u = ph.sb([128, 32], U32, "pA_u")
            pB_u = ph.sb([128, 32], U32, "pB_u")
            es24 = ph.sb([128, 3, NSLOT], F32, "es24")
            idxf = ph.sb([128, NSLOT, 11], F32, "idxf")
            idxw = ph.sb([128, NSLOT, 11], U32, "idxw")
            cumb, totb = ph.banks[0], ph.banks[1]
            S.dve(lambda e: e.tensor_single_scalar(out=sel.t[:], in_=comb.t[:], scalar=0.0, op=ALU.is_gt), reads=[comb.b], writes=[sel.b])
            S.dve(lambda e: e.tensor_copy(out=selb.t[:], in_=sel.t[:].rearrange("p t e -> p (t e)")), reads=[sel.b], writes=[selb.b])
            S.pe(lambda e: e.matmul(cumb.t[:, 0:256], lhsT=tri_b.t[:], rhs=selb.t[:], start=True, stop=True), reads=[tri_b.b, selb.b], writes=[cumb.b])
            S.pe(lambda e: e.matmul(totb.t[:, 0:256], lhsT=ph.ones_b.t[:], rhs=selb.t[:], start=True, stop=True), reads=[ph.ones_b.b, selb.b], writes=[totb.b])
            S.dve(lambda e: e.tensor_copy(out=tot.t[:].rearrange("p t e -> p (t e)"), in_=totb.t[:, 0:256]), reads=[totb.b], writes=[tot.b])
            S.dve(lambda e: e.memset(tp.t[:, 0, :], 0.0), writes=[tp.b])
            for t in range(1, 32):
                S.dve(lambda e, t=t: e.tensor_tensor(out=tp.t[:, t, :], in0=tp.t[:, t - 1, :], in1=tot.t[:, t - 1, :], op=ALU.add), reads=[tp.b, tot.b], writes=[tp.b])
            S.dve(lambda e: e.tensor_tensor(out=sm8.t[:, 0:8], in0=tp.t[:, 31, :], in1=tot.t[:, 31, :], op=ALU.add), reads=[tp.b, tot.b], writes=[sm8.b])
            for ex in range(8):
                S.dve(lambda e, ex=ex: e.tensor_scalar(out=cmp8.t[:, ex, :], in0=thr, scalar1=sm8.t[:, ex:ex + 1], scalar2=None, op0=ALU.is_lt), reads=[rc_.b, sm8.b], writes=[cmp8.b])
            S.dve(lambda e: e.tensor_reduce(out=sm8.t[:, 8:16], in_=cmp8.t[:], axis=mybir.AxisListType.X, op=ALU.add), reads=[cmp8.b], writes=[sm8.b])
            S.dve(lambda e: e.memset(sm8.t[:, 16:17], 0.0), writes=[sm8.b])
            for ex in range(1, 8):
                S.dve(lambda e, ex=ex: e.tensor_tensor(out=sm8.t[:, 16 + ex:17 + ex], in0=sm8.t[:, 15 + ex:16 + ex], in1=sm8.t[:, 7 + ex:8 + ex], op=ALU.add), reads=[sm8.b], writes=[sm8.b])
            S.dve(lambda e: e.tensor_scalar(out=sm8.t[:, 24:32], in0=sm8.t[:, 16:24], scalar1=512.0, scalar2=-1.0, op0=ALU.mult, op1=ALU.add), reads=[sm8.b], writes=[sm8.b])
            S.dve(lambda e: e.tensor_tensor(out=sm8.t[:, 32:40], in0=sm8.t[:, 16:24], in1=sm8.t[:, 8:16], op=ALU.add), reads=[sm8.b], writes=[sm8.b])
            cumv = cumb.t[:, 0:256].rearrange("p (t e) -> p t e", e=NEXP)
            for ex in range(8):
                S.dve(lambda e, ex=ex: e.scalar_tensor_tensor(out=pos.t[:, :, ex], in0=cumv[:, :, ex], scalar=sm8.t[:, 24 + ex:25 + ex], in1=tp.t[:, :, ex], op0=ALU.add, op1=ALU.add),
                      reads=[cumb.b, sm8.b, tp.b], writes=[pos.b])
            S.dve(lambda e: e.scalar_tensor_tensor(out=Mt.t[:], in0=pos.t[:], scalar=1.0, in1=sel.t[:], op0=ALU.add, op1=ALU.mult), reads=[pos.b, sel.b], writes=[Mt.b])
            S.dve(lambda e: e.tensor_reduce(out=r32.t[:, 0, :], in_=Mt.t[:], axis=mybir.AxisListType.X, op=ALU.max), reads=[Mt.b], writes=[r32.b])
            S.dve(lambda e: e.tensor_reduce(out=r32.t[:, 1, :], in_=Mt.t[:], axis=mybir.AxisListType.X, op=ALU.add), reads=[Mt.b], writes=[r32.b])
            S.dve(lambda e: e.tensor_scalar(out=r32.t[:, 3, :], in0=r32.t[:, 0, :], scalar1=-1.0, scalar2=None, op0=ALU.add), reads=[r32.b], writes=[r32.b])
            S.dve(lambda e: e.scalar_tensor_tensor(out=r32.t[:, 2, :], in0=r32.t[:, 1, :], scalar=-1.0, in1=r32.t[:, 0, :], op0=ALU.add, op1=ALU.subtract), reads=[r32.b], writes=[r32.b])
            S.dve(lambda e: e.tensor_copy(out=pA_u.t[:], in_=r32.t[:, 2, :]), reads=[r32.b], writes=[pA_u.b])
            S.dve(lambda e: e.tensor_copy(out=pB_u.t[:], in_=r32.t[:, 3, :]), reads=[r32.b], writes=[pB_u.b])
            for ex in range(8):
                S.dve(lambda e, ex=ex: e.tensor_tensor(out=eq.t[:, :, ex], in0=Mt.t[:, :, ex], in1=r32.t[:, 0, :], op=ALU.is_equal), reads=[Mt.b, r32.b], writes=[eq.b])
            S.dve(lambda e: e.tensor_tensor(out=eq.t[:], in0=eq.t[:], in1=comb.t[:], op=ALU.mult), reads=[eq.b, comb.b], writes=[eq.b])
            S.dve(lambda e: e.tensor_reduce(out=r32.t[:, 4, :], in_=eq.t[:], axis=mybir.AxisListType.X, op=ALU.add), reads=[eq.b], writes=[r32.b])
            S.dve(lambda e: e.tensor_reduce(out=r32.t[:, 5, :], in_=comb.t[:], axis=mybir.AxisListType.X, op=ALU.add), reads=[comb.b], writes=[r32.b])
            S.dve(lambda e: e.tensor_tensor(out=r32.t[:, 6, :], in0=r32.t[:, 5, :], in1=r32.t[:, 4, :], op=ALU.subtract), reads=[r32.b], writes=[r32.b])
            S.dve(lambda e: e.memset(es24.t[:, 0, :], 0.0), writes=[es24.b])
            for ex in range(8):
                S.dve(lambda e, ex=ex: e.scalar_tensor_tensor(out=es24.t[:, 0, :], in0=svals, scalar=sm8.t[:, 32 + ex:33 + ex], in1=es24.t[:, 0, :], op0=ALU.is_ge, op1=ALU.add),
                      reads=[rc_.b, sm8.b, es24.b], writes=[es24.b])
            S.dve(lambda e: e.tensor_scalar(out=es24.t[:, 1, :], in0=es24.t[:, 0, :], scalar1=7.0, scalar2=None, op0=ALU.min), reads=[es24.b], writes=[es24.b])
            S.dve(lambda e: e.tensor_scalar(out=es24.t[:, 2, :], in0=es24.t[:, 1, :], scalar1=1408.0, scalar2=pcol, op0=ALU.mult, op1=ALU.add), reads=[es24.b, rc_.b], writes=[es24.b])
            for jp in range(11):
                S.dve(lambda e, jp=jp: e.tensor_scalar(out=idxf.t[:, :, jp], in0=es24.t[:, 2, :], scalar1=float(jp * 128), scalar2=None, op0=ALU.add), reads=[es24.b], writes=[idxf.b])
            S.dve(lambda e: e.tensor_copy(out=idxw.t[:], in_=idxf.t[:]), reads=[idxf.b], writes=[idxw.b])

            zt = ph.sb([128, 4, D], BF16, "zt")
            S.pool(lambda e: e.memset(zt.t[:], 0.0), writes=[zt.b])
            zb = []
            for sl in range(NSLOT):
                bz = S.buf("z")
                zb.append(bz)
                S.dma(lambda e, sl=sl: e.dma_start(out=Hs_d[sl * 512:(sl + 1) * 512, :].rearrange("(a p) d -> p a d", p=128), in_=zt.t[:]), reads=[zt.b], writes=[bz])
            h4_r = ph.rot(3, [128, D], BF16, "h4t")
            scb = []
            for t in range(32):
                h4t = h4_r.next()
                S.dma(lambda e, t=t, h4t=h4t: e.dma_start(out=h4t.t[:], in_=h4_d[t * 128:(t + 1) * 128, :]), writes=[h4t.b])
                for pu in (pA_u, pB_u):
                    bs = S.buf("sc")
                    scb.append(bs)
                    S.dma(lambda e, t=t, h4t=h4t, pu=pu: e.indirect_dma_start(out=Hs_d[:, :], out_offset=bass.IndirectOffsetOnAxis(ap=pu.t[:, t:t + 1], axis=0), in_=h4t.t[:, :], in_offset=None),
                          reads=[h4t.b, pu.b] + zb, writes=[bs], q="pool")

            hs_r = ph.rot(1, [128, 4, D], BF16, "hs")
            hT_r = ph.rot(2, [128, 8, 512], BF16, "hTs")
            w1_r = ph.rot(3, [128, 2, 8, 128], BF16, "w1p")
            w3_r = ph.rot(3, [128, 2, 8, 128], BF16, "w3p")
            W2 = ph.sb([128, NJ, D], BF16, "W2s")
            W2b = [S.buf(f"W2_{jp}") for jp in range(11)]
            act = ph.sb([128, NJ, 512], BF16, "acts")
            sg_r = ph.rot(3, [128, 512], BF16, "sgs")
            ys_r = ph.rot(2, [128, D], F32, "ys")
            ub = Rot(ph.banks[0:4])
            yp_r = Rot(ph.pairs[2:4])
            rb = []

            def gather_w(wi, s_, jp, out_ap, out_b, extra_reads=()):
                S.dma(lambda e: e.indirect_dma_start(out=out_ap, out_offset=None, in_=wb_d[wi][:, :], in_offset=bass.IndirectOffsetOnAxis(ap=idxw.t[:, s_, jp:jp + 1], axis=0)),
                      reads=[idxw.b] + list(extra_reads), writes=[out_b], q="pool", cost=3.0, lat=6.0)

            def slot(s_):
                hs, hT = hs_r.next(), hT_r.next()
                S.dma(lambda e: e.dma_start(out=hs.t[:], in_=Hs_d[s_ * 512:(s_ + 1) * 512, :].rearrange("(a p) d -> p a d", p=128)), reads=scb, writes=[hs.b])
                for a_ in range(4):
                    tbk = ub.next()
                    pv = tbk.t.bitcast(BF16)
                    for k in range(8):
                        S.pe(lambda e, k=k, a_=a_: e.transpose(out=pv[:, k * 128:(k + 1) * 128], in_=hs.t[:, a_, k * 128:(k + 1) * 128], identity=ph.ident_b.t[:]),
                             reads=[hs.b, ph.ident_b.b], writes=[tbk.b])
                    S.dve(lambda e, a_=a_: e.tensor_copy(out=hT.t[:, :, a_ * 128:(a_ + 1) * 128], in_=pv.rearrange("p (k t) -> p k t", k=8)), reads=[tbk.b], writes=[hT.b])
                for jp in range(11):
                    w1, w3 = w1_r.next(), w3_r.next()
                    gather_w(0, s_, jp, w1.t[:].rearrange("p a k n -> p (a k n)"), w1.b)
                    gather_w(1, s_, jp, w3.t[:].rearrange("p a k n -> p (a k n)"), w3.b)
                    for jj in range(2):
                        j = 2 * jp + jj
                        u1, u3 = ub.next(), ub.next()
                        for k in range(8):
                            S.pe(lambda e, k=k, jj=jj: e.matmul(u1.t[:, :], lhsT=w1.t[:, jj, k, :], rhs=hT.t[:, k, :], start=(k == 0), stop=(k == 7)), reads=[w1.b, hT.b], writes=[u1.b])
                        for k in range(8):
                            S.pe(lambda e, k=k, jj=jj: e.matmul(u3.t[:, :], lhsT=w3.t[:, jj, k, :], rhs=hT.t[:, k, :], start=(k == 0), stop=(k == 7)), reads=[w3.b, hT.b], writes=[u3.b])
                        sg = sg_r.next()
                        S.act(lambda e: e.activation(out=sg.t[:], in_=u1.t[:], func=AF.Silu), reads=[u1.b], writes=[sg.b])
                        S.dve(lambda e, j=j: e.tensor_tensor(out=act.t[:, j, :], in0=u3.t[:], in1=sg.t[:], op=ALU.mult), reads=[sg.b, u3.b], writes=[act.b])
                    gather_w(2, s_, jp, W2.t[:, 2 * jp:2 * jp + 2, :].rearrange("p a n -> p (a n)"), W2b[jp])
                for a_ in range(4):
                    yp = yp_r.next()
                    for half in range(2):
                        for j in range(NJ):
                            S.pe(lambda e, j=j, a_=a_, half=half: e.matmul(yp.t[:, half * 512:(half + 1) * 512], lhsT=act.t[:, j, a_ * 128:(a_ + 1) * 128],
                                                                           rhs=W2.t[:, j, half * 512:(half + 1) * 512], start=(j == 0), stop=(j == NJ - 1)),
                                 reads=[act.b, W2b[j // 2]], writes=[yp.b])
                    ys = ys_r.next()
                    S.dve(lambda e: e.tensor_copy(out=ys.t[:, 0:512], in_=yp.t[:, 0:512]), reads=[yp.b], writes=[ys.b])
                    S.act(lambda e: e.activation(out=ys.t[:, 512:1024], in_=yp.t[:, 512:1024], func=AF.Copy), reads=[yp.b], writes=[ys.b])
                    br = S.buf("R")
                    rb.append(br)
                    r0 = s_ * 512 + a_ * 128
                    S.dma(lambda e: e.dma_start(out=R_d[r0:r0 + 128, :], in_=ys.t[:]), reads=[ys.b], writes=[br])
            for s_ in range(NSLOT):
                slot(s_)

            ra_r = ph.rot(2, [128, D], F32, "ra")
            rbb_r = ph.rot(2, [128, D], F32, "rbb")
            xt_r = ph.rot(2, [128, D], F32, "xt9")
            jk_r = ph.rot(1, [128, D], F32, "jk9")
            st_r = ph.rot(3, [128, 8], F32, "st9")

            def combine(t):
                ra, rbt, xt, jk, st = ra_r.next(), rbb_r.next(), xt_r.next(), jk_r.next(), st_r.next()
                S.dma(lambda e: e.indirect_dma_start(out=ra.t[:, :], out_offset=None, in_=R_d[:, :], in_offset=bass.IndirectOffsetOnAxis(ap=pA_u.t[:, t:t + 1], axis=0)),
                      reads=[pA_u.b] + rb, writes=[ra.b], q="pool")
                S.dma(lambda e: e.indirect_dma_start(out=rbt.t[:, :], out_offset=None, in_=R_d[:, :], in_offset=bass.IndirectOffsetOnAxis(ap=pB_u.t[:, t:t + 1], axis=0)),
                      reads=[pB_u.b] + rb, writes=[rbt.b], q="pool")
                S.dma(lambda e: e.dma_start(out=xt.t[:], in_=res_ap(t + 2)), writes=[xt.b])
                S.dve(lambda e: e.tensor_scalar(out=ra.t[:], in0=ra.t[:], scalar1=r32.t[:, 6, t:t + 1], scalar2=None, op0=ALU.mult), reads=[ra.b, r32.b], writes=[ra.b])
                S.dve(lambda e: e.scalar_tensor_tensor(out=ra.t[:], in0=rbt.t[:], scalar=r32.t[:, 4, t:t + 1], in1=ra.t[:], op0=ALU.mult, op1=ALU.add), reads=[ra.b, rbt.b, r32.b], writes=[ra.b])
                S.dve(lambda e: e.memset(st.t[:], 0.0), writes=[st.b])
                S.act(lambda e: e.activation(out=jk.t[:], in_=ra.t[:], func=AF.Square, accum_out=st.t[:, 0:1]), reads=[ra.b], writes=[jk.b, st.b])
                rstd_from_ss(st, 1, D)
                S.dve(lambda e: e.scalar_tensor_tensor(out=jk.t[:], in0=ra.t[:], scalar=st.t[:, 7:8], in1=G3.t[:], op0=ALU.mult, op1=ALU.mult), reads=[ra.b, st.b, G3.b], writes=[jk.b])
                S.pool(lambda e: e.tensor_tensor(out=xt.t[:], in0=jk.t[:], in1=xt.t[:], op=ALU.add), reads=[jk.b, xt.b], writes=[xt.b])
                S.dma(lambda e: e.dma_start(out=res_ap(t + 2), in_=xt.t[:]), reads=[xt.b], writes=[ph.db(("res", t + 2))])
            for t in range(32):
                combine(t)
            ph.close()

        print("total ops", S.total)
    return nc


def sublayer_tail(S, ph, load_bc, NormCtx, norm_mod_T, rstd_from_ss, layer, src_fn, res_ap, hT_d, oT_d, wo, bias, tiles, post_fn, h4_d=None):
    nx = NormCtx(ph)
    G1 = [load_bc(ph, layer, w, 2) for w in (0, 1)]
    A2 = [load_bc(ph, layer, w, 3) for w in (0, 1)]
    SH2 = [load_bc(ph, layer, w, 4) for w in (0, 1)]
    ot_r = ph.rot(3, [128, 8, 128], BF16, "ot")
    xt_r = ph.rot(2, [128, D], F32, "xt3")
    x1_r = ph.rot(2, [128, D], F32, "x1")
    tm_r = ph.rot(2, [128, D], F32, "tm3")
    jk_r = ph.rot(2, [128, 512], F32, "jk3")
    st_r = ph.rot(3, [128, 8], F32, "st3")
    h2_r = ph.rot(3, [128, 8, 128], BF16, "h2")
    pr = Rot(ph.pairs[0:2])
    tbank = ph.banks[7]

    def one(t):
        which = 1 if t < 2 else 0
        yp, ot = pr.next(), ot_r.next()
        S.dma(lambda e: e.dma_start(out=ot.t[:], in_=oT_d[:, :, t * 128:(t + 1) * 128]), writes=[ot.b])
        for half in range(2):
            hs = slice(half * 512, (half + 1) * 512)
            prs = [(ot.t[:, c, :], wo.t[:, c, hs]) for c in range(8)]
            rd = [ot.b, wo.b]
            if bias is not None:
                prs.append((ph.ones_b.t[0:1, 0:128], bias.t[0:1, hs]))
                rd = rd + [bias.b, ph.ones_b.b]
            n = len(prs)
            for i, (l, r) in enumerate(prs):
                S.pe(lambda e, l=l, r=r, i=i: e.matmul(yp.t[:, hs], lhsT=l, rhs=r, start=(i == 0), stop=(i == n - 1)), reads=rd, writes=[yp.b])
        xt = xt_r.next()
        S.dma(lambda e: e.dma_start(out=xt.t[:], in_=src_fn(t)), reads=[ph.db(("res", t))], writes=[xt.b])
        st, tm, x1 = st_r.next(), tm_r.next(), x1_r.next()
        S.dve(lambda e: e.memset(st.t[:], 0.0), writes=[st.b])
        for half in range(2):
            jk = jk_r.next()
            S.act(lambda e, jk=jk, half=half: e.activation(out=jk.t[:], in_=yp.t[:, half * 512:(half + 1) * 512], func=AF.Square, accum_out=st.t[:, half:half + 1]),
                  reads=[yp.b], writes=[jk.b, st.b])
        rstd_from_ss(st, 2, D)
        for half in range(2):
            hs = slice(half * 512, (half + 1) * 512)
            S.dve(lambda e, hs=hs: e.scalar_tensor_tensor(out=tm.t[:, hs], in0=yp.t[:, hs], scalar=st.t[:, 7:8], in1=G1[which].t[:, hs], op0=ALU.mult, op1=ALU.mult),
                  reads=[yp.b, st.b, G1[which].b], writes=[tm.b])
        S.pool(lambda e: e.tensor_tensor(out=x1.t[:], in0=tm.t[:], in1=xt.t[:], op=ALU.add), reads=[tm.b, xt.b], writes=[x1.b])
        S.dma(lambda e: e.dma_start(out=res_ap(t), in_=x1.t[:]), reads=[x1.b], writes=[ph.db(("res", t))])
        h2 = h2_r.next()
        hb = norm_mod_T(ph, nx, x1, A2[which], SH2[which], h2.t[:], h2.b, tbank)
        if h4_d is None:
            S.dma(lambda e: e.dma_start(out=hT_d[:, :, t * 128:(t + 1) * 128], in_=h2.t[:]), reads=[h2.b], writes=[ph.db(("hT", t))])
        else:
            S.dma(lambda e: e.dma_start(out=h4_d[(t - 2) * 128:(t - 1) * 128, :], in_=hb.t[:]), reads=[hb.b], writes=[ph.db(("h4", t))])
        if post_fn is not None:
            post_fn(t, h2)
    for t in tiles:
        one(t)


def ffn_phase(S, ph, load_bc, rstd_from_ss, layer, groups, res_ap, hT_d, experts, comb):
    G3 = [load_bc(ph, layer, w, 5) for w in (0, 1)]
    hT = ph.sb([128, 8, 1024], BF16, "hTg")
    act = ph.sb([128, NJ, 1024], BF16, "act")
    W2 = ph.sb([128, NJ, D], BF16, "W2")
    w1_r = ph.rot(4, [128, 8, 128], BF16, "w1c")
    w3_r = ph.rot(4, [128, 8, 128], BF16, "w3c")
    sg_r = ph.rot(3, [128, 512], BF16, "sg")
    xt_r = ph.rot(2, [128, D], F32, "xt4")
    tm_r = ph.rot(2, [128, D], F32, "tm4")
    jk_r = ph.rot(2, [128, 512], F32, "jk4")
    st_r = ph.rot(3, [128, 8], F32, "st4")
    ne = len(experts)
    yacc = ph.sb([128, 8, D], F32, "yacc") if ne > 1 else None
    ub = Rot(ph.banks[0:4])
    yp_r = Rot(ph.pairs[2:4])
    for (tok0, n) in groups:
        S.dma(lambda e, tok0=tok0, n=n: e.dma_start(out=hT.t[:, :, 0:n], in_=hT_d[:, :, tok0:tok0 + n]), writes=[hT.b])
        nsub = [(s0, min(512, n - s0)) for s0 in range(0, n, 512)]
        for ei, (w1_d, w3_d, w2_d) in enumerate(experts):
            w1v, w3v = w1_d, w3_d
            w2v = w2_d.rearrange("(j p) n -> p j n", p=128)
            for j0 in range(0, NJ, 2):
                S.dma(lambda e, j0=j0, w2v=w2v: e.dma_start(out=W2.t[:, j0:j0 + 2, :], in_=w2v[:, j0:j0 + 2, :]), writes=[W2.b], q="pool")
            for j in range(NJ):
                w1, w3 = w1_r.next(), w3_r.next()
                S.dma(lambda e, w1=w1, j=j, w1v=w1v: e.dma_start(out=w1.t[:].rearrange("p k n -> p (k n)"), in_=w1v[j]), writes=[w1.b], q="pool")
                S.dma(lambda e, w3=w3, j=j, w3v=w3v: e.dma_start(out=w3.t[:].rearrange("p k n -> p (k n)"), in_=w3v[j]), writes=[w3.b], q="pool")
                for (s0, sn) in nsub:
                    u1, u3 = ub.next(), ub.next()
                    for k in range(8):
                        S.pe(lambda e, u1=u1, w1=w1, k=k, s0=s0, sn=sn: e.matmul(u1.t[:, 0:sn], lhsT=w1.t[:, k, :], rhs=hT.t[:, k, s0:s0 + sn], start=(k == 0), stop=(k == 7)),
                             reads=[w1.b, hT.b], writes=[u1.b])
                    for k in range(8):
                        S.pe(lambda e, u3=u3, w3=w3, k=k, s0=s0, sn=sn: e.matmul(u3.t[:, 0:sn], lhsT=w3.t[:, k, :], rhs=hT.t[:, k, s0:s0 + sn], start=(k == 0), stop=(k == 7)),
                             reads=[w3.b, hT.b], writes=[u3.b])
                    sg = sg_r.next()
                    S.act(lambda e, sg=sg, u1=u1, sn=sn: e.activation(out=sg.t[:, 0:sn], in_=u1.t[:, 0:sn], func=AF.Silu), reads=[u1.b], writes=[sg.b])
                    S.dve(lambda e, sg=sg, u3=u3, j=j, s0=s0, sn=sn: e.tensor_tensor(out=act.t[:, j, s0:s0 + sn], in0=u3.t[:, 0:sn], in1=sg.t[:, 0:sn], op=ALU.mult),
                          reads=[sg.b, u3.b], writes=[act.b])
            for tl in range(n // 128):
                t = tok0 // 128 + tl
                which = 1 if t < 2 else 0
                yp = yp_r.next()
                for half in range(2):
                    for j in range(NJ):
                        S.pe(lambda e, yp=yp, j=j, tl=tl, half=half: e.matmul(yp.t[:, half * 512:(half + 1) * 512], lhsT=act.t[:, j, tl * 128:(tl + 1) * 128],
                                                                                rhs=W2.t[:, j, half * 512:(half + 1) * 512], start=(j == 0), stop=(j == NJ - 1)),
                             reads=[act.b, W2.b], writes=[yp.b])
                if ne > 1:
                    cap = comb.t[:, t - 2, ei:ei + 1]
                    if ei == 0:
                        S.dve(lambda e, yp=yp, tl=tl, cap=cap: e.tensor_scalar(out=yacc.t[:, tl, :], in0=yp.t[:, :], scalar1=cap, scalar2=None, op0=ALU.mult),
                              reads=[yp.b, comb.b], writes=[yacc.b])
                    else:
                        S.dve(lambda e, yp=yp, tl=tl, cap=cap: e.scalar_tensor_tensor(out=yacc.t[:, tl, :], in0=yp.t[:, :], scalar=cap, in1=yacc.t[:, tl, :], op0=ALU.mult, op1=ALU.add),
                              reads=[yp.b, comb.b, yacc.b], writes=[yacc.b])
                    if ei < ne - 1:
                        continue
                xt, st, tm = xt_r.next(), st_r.next(), tm_r.next()
                S.dma(lambda e, xt=xt, t=t: e.dma_start(out=xt.t[:], in_=res_ap(t)), reads=[ph.db(("res", t))], writes=[xt.b])
                S.dve(lambda e, st=st: e.memset(st.t[:], 0.0), writes=[st.b])
                for half in range(2):
                    hs = slice(half * 512, (half + 1) * 512)
                    jk = jk_r.next()
                    if ne > 1:
                        S.act(lambda e, jk=jk, st=st, half=half, hs=hs, tl=tl: e.activation(out=jk.t[:], in_=yacc.t[:, tl, hs], func=AF.Square, accum_out=st.t[:, half:half + 1]),
                              reads=[yacc.b], writes=[jk.b, st.b])
                    else:
                        S.act(lambda e, jk=jk, st=st, half=half, hs=hs, yp=yp: e.activation(out=jk.t[:], in_=yp.t[:, hs], func=AF.Square, accum_out=st.t[:, half:half + 1]),
                              reads=[yp.b], writes=[jk.b, st.b])
                rstd_from_ss(st, 2, D)
                for half in range(2):
                    hs = slice(half * 512, (half + 1) * 512)
                    if ne > 1:
                        S.dve(lambda e, tm=tm, st=st, hs=hs, tl=tl, which=which: e.scalar_tensor_tensor(out=tm.t[:, hs], in0=yacc.t[:, tl, hs], scalar=st.t[:, 7:8], in1=G3[which].t[:, hs], op0=ALU.mult, op1=ALU.mult),
                              reads=[yacc.b, st.b, G3[which].b], writes=[tm.b])
                    else:
                        S.dve(lambda e, tm=tm, st=st, hs=hs, yp=yp, which=which: e.scalar_tensor_tensor(out=tm.t[:, hs], in0=yp.t[:, hs], scalar=st.t[:, 7:8], in1=G3[which].t[:, hs], op0=ALU.mult, op1=ALU.mult),
                              reads=[yp.b, st.b, G3[which].b], writes=[tm.b])
                S.pool(lambda e, tm=tm, xt=xt: e.tensor_tensor(out=xt.t[:], in0=tm.t[:], in1=xt.t[:], op=ALU.add), reads=[tm.b, xt.b], writes=[xt.b])
                S.dma(lambda e, xt=xt, t=t: e.dma_start(out=res_ap(t), in_=xt.t[:]), reads=[xt.b], writes=[ph.db(("res", t))])


def _rot_perm(d):
    q = d // 4
    idx = np.arange(d)
    src = np.where((idx % (d // 2)) < q, idx + q, idx - q)
    return src


def _rope_tables():
    t = np.arange(SEQ)
    row = (t // GRID_W).astype(np.float32)
    col = (t % GRID_W).astype(np.float32)

    def tab(d, nrep, base):
        half = d // 2
        inv = (10000.0 ** (-np.arange(0, half, 2, dtype=np.float32) / half)).astype(np.float32)
        c = np.zeros((128, SEQ), np.float32)
        s = np.zeros((128, SEQ), np.float32)
        for j in range(d):
            pos = row if j < half else col
            f = inv[(j % half) % (half // 2)]
            ang = (pos * f).astype(np.float32)
            for r in range(nrep):
                c[base + r * d + j] = np.cos(ang)
                s[base + r * d + j] = np.sin(ang)
        return c, s
    cA, sA = tab(32, 1, 64)
    cB, sB = tab(64, 2, 0)
    return cA, sA, cB, sB


def _prep_shared(inp):
    f = lambda a: np.ascontiguousarray(a, dtype=np.float32)
    sh = {}
    sh["w_mod"] = f(inp["w_mod"])
    sh["b_mod"] = f(inp["b_mod"])
    sh["norm_g"] = f(inp["norm_g"].reshape(2, 4 * D))
    w_in = inp["a_w_in"][0]
    sh["a_w_in"] = f(w_in)
    p32 = _rot_perm(32)
    p64 = _rot_perm(64)
    cols = [640 + p32]
    for h in range(8):
        cols.append(672 + h * 64 + p64)
    for h in range(8):
        cols.append(1184 + h * 64 + p64)
    sh["a_w_in_p"] = f(w_in[:, np.concatenate(cols)])
    sh["a_q_norm_t"] = f(inp["a_q_norm"][0].reshape(3, 128).T)
    sh["a_kv_norm_t"] = f(inp["a_kv_norm"][0].reshape(2, 128).T)
    w_uq = inp["a_w_uq"][0]
    sh["a_w_uq"] = f(w_uq)
    sh["a_w_uq_p"] = f(w_uq[:, np.concatenate([h * 96 + 64 + p32 for h in range(8)])])
    w_ukv = inp["a_w_ukv"][0]
    kc = np.concatenate([h * 128 + np.arange(64) for h in range(8)])
    vc = np.concatenate([h * 128 + 64 + np.arange(64) for h in range(8)])
    sh["a_w_ukv_r"] = f(w_ukv[:, np.concatenate([kc, vc])])
    sh["b_lambda"] = f(inp["b_lambda"][0].reshape(1, 256))
    sh["b_subln_t"] = f(inp["b_subln"][0].reshape(128, 1))
    sh["ab_w_out"] = f(inp["ab_w_out"][0])
    for nm_, src in (("f_w1r", inp["f_w1"][0]), ("f_w3r", inp["f_w3"][0])):
        sh[nm_] = f(np.asarray(src).reshape(8, 128, NJ, 128).transpose(2, 1, 0, 3).reshape(NJ, 128, 1024))
    sh["f_w2"] = f(inp["f_w2"])
    sh["c_w_qkv"] = f(inp["c_w_qkv"][0])
    sh["c_b_qk_t"] = f(inp["c_b_qkv"][0][0:2048].reshape(16, 128).T)
    sh["c_b_v"] = f(inp["c_b_qkv"][0][2048:3072].reshape(1, D))
    rpb = inp["c_rpb"][0]
    kl = np.arange(128)
    r_l, c_l = kl // 64, kl % 64
    g = np.zeros((16, ND, 128, 128), np.float32)
    for di, dd in enumerate(range(-3, 4)):
        dr = np.clip(2 * dd + r_l[:, None] - r_l[None, :] + 7, 0, 14)
        dc = np.clip(c_l[:, None] - c_l[None, :] + 15, 0, 30)
        g[:, di] = rpb[:, dr, dc]
    sh["rpbg"] = g
    sh["nmask"] = f(NA_MASKS)
    sh["c_w_out"] = f(inp["c_w_out"][0])
    sh["c_b_out"] = f(inp["c_b_out"][0].reshape(1, D))
    sh["m_router"] = f(inp["m_router"][0])
    for nm_, src in (("m_w1r", inp["m_w1"][0]), ("m_w3r", inp["m_w3"][0])):
        sh[nm_] = f(np.asarray(src).reshape(NEXP, 8, 128, 11, 2, 128).transpose(0, 3, 2, 4, 1, 5).reshape(NEXP * 11 * 128, 2048))
    sh["m_w2r"] = f(np.asarray(inp["m_w2"][0]).reshape(NEXP, 11, 2, 128, 1024).transpose(0, 1, 3, 2, 4).reshape(NEXP * 11 * 128, 2048))
    rconst = np.zeros((128, 161), np.float32)
    rconst[:, 0:128] = np.triu(np.ones((128, 128), np.float32))
    rconst[:, 128:136] = np.arange(8, dtype=np.float32)[None, :] * 512.0
    rconst[:, 136:160] = np.arange(24, dtype=np.float32)[None, :]
    rconst[:, 160] = np.arange(128, dtype=np.float32)
    sh["rconst"] = rconst
    cA, sA, cB, sB = _rope_tables()
    sh["cosA"], sh["sinA"], sh["cosB"], sh["sinB"] = cA, sA, cB, sB
    sh["ident"] = np.eye(128, dtype=np.float32)
    return sh


def make_in_maps(inp, cores):
    sh = _prep_shared(inp)
    maps = []
    for b in cores:
        m = dict(sh)
        m["x"] = np.ascontiguousarray(inp["x"][b], dtype=np.float32)
        m["ctx"] = np.ascontiguousarray(inp["ctx"][b], dtype=np.float32)
        ccv = np.stack([inp["c"][b], inp["c_ctx"]], axis=-1).astype(np.float32)
        m["cc"] = np.ascontiguousarray(ccv.reshape(8, 128, 2).transpose(1, 0, 2))
        maps.append(m)
    return maps


_NC_CACHE = {}


def kernel(**inputs):
    if "nc" not in _NC_CACHE:
        _NC_CACHE["nc"] = build_program()
    nc = _NC_CACHE["nc"]
    in_maps = make_in_maps(inputs, list(range(8)))
    res = run_bass_kernel_spmd(nc, in_maps, core_ids=list(range(8)))
    return np.stack([np.asarray(r["out"], dtype=np.float32) for r in res.results], axis=0)
```
